# Optimizing a Trainium2 kernel written in Bass

```python
import math
import jax, jax.numpy as jnp
from jax import lax
import numpy as np

D_MODEL = 1024
BATCH = 8
SEQ = 8192
DEPTH = 1

ROPE_THETA = 10000.0
EPS = 1e-6
Q_BLOCK = 128

MLA_HEADS = 4
MLA_Q_RANK = 256
MLA_KV_RANK = 256
MLA_NOPE_DIM = 128
MLA_ROPE_DIM = 64
MLA_V_DIM = 128

DIFF_HEADS = 4
DIFF_QK_DIM = 64
DIFF_V_DIM = 2 * DIFF_QK_DIM

MIX_WIDTH = MLA_HEADS * MLA_V_DIM + DIFF_HEADS * DIFF_V_DIM
IN_SPLITS = (MLA_Q_RANK, MLA_KV_RANK, MLA_ROPE_DIM,
             DIFF_HEADS * 2 * DIFF_QK_DIM, DIFF_HEADS * 2 * DIFF_QK_DIM, DIFF_HEADS * DIFF_V_DIM)
IN_COLS = sum(IN_SPLITS)

N_GROUPS = 4
EXPERTS_PER_GROUP = 8
N_EXPERTS = N_GROUPS * EXPERTS_PER_GROUP
TOP_K = 2
D_EXPERT = 512
MOE_BLOCK = 128

kernel_name = 'hymba_mla_diffattn_hiermoe_encoder'


def rms_norm(x, g):
    xf = x.astype(jnp.float32)
    y = xf * lax.rsqrt(jnp.mean(xf * xf, axis=-1, keepdims=True) + EPS)
    return (y * g.astype(jnp.float32)).astype(x.dtype)


def rope(x, pos):
    d = x.shape[-1]
    inv = ROPE_THETA ** (-jnp.arange(0, d, 2, dtype=jnp.float32) / d)
    ang = pos.astype(jnp.float32)[:, None] * inv[None, :]
    shape = (1, x.shape[1]) + (1,) * (x.ndim - 3) + (d // 2,)
    cos = jnp.cos(ang).reshape(shape)
    sin = jnp.sin(ang).reshape(shape)
    xf = x.astype(jnp.float32)
    x1, x2 = xf[..., : d // 2], xf[..., d // 2:]
    return jnp.concatenate([x1 * cos - x2 * sin, x2 * cos + x1 * sin], axis=-1).astype(x.dtype)


def to_blocks(t):
    b, s = t.shape[0], t.shape[1]
    return t.reshape((b, s // Q_BLOCK, Q_BLOCK) + t.shape[2:]).swapaxes(0, 1)


def from_blocks(t):
    nb, b, qb = t.shape[0], t.shape[1], t.shape[2]
    return t.swapaxes(0, 1).reshape((b, nb * qb) + t.shape[3:])


def mla_attention(q_nope, q_pe, k_nope, k_pe, v):
    scale = (q_nope.shape[-1] + q_pe.shape[-1]) ** -0.5

    def block(args):
        qn, qr = args
        s = (jnp.einsum('bqhd,bkhd->bhqk', qn, k_nope)
             + jnp.einsum('bqhr,bkr->bhqk', qr, k_pe)).astype(jnp.float32) * scale
        p = jax.nn.softmax(s, axis=-1).astype(v.dtype)
        return jnp.einsum('bhqk,bkhd->bqhd', p, v)

    return from_blocks(lax.map(block, (to_blocks(q_nope), to_blocks(q_pe))))


def diff_attention(q1, q2, k1, k2, v, lam):
    scale = q1.shape[-1] ** -0.5

    def block(args):
        a1, a2 = args
        s1 = jnp.einsum('bqhd,bkhd->bhqk', a1, k1).astype(jnp.float32) * scale
        s2 = jnp.einsum('bqhd,bkhd->bhqk', a2, k2).astype(jnp.float32) * scale
        p = (jax.nn.softmax(s1, axis=-1) - lam * jax.nn.softmax(s2, axis=-1)).astype(v.dtype)
        return jnp.einsum('bhqk,bkhd->bqhd', p, v)

    return from_blocks(lax.map(block, (to_blocks(q1), to_blocks(q2))))


def hier_moe(h, w_rg, b_rg, w_re, b_re, w_gate, w_up, w_down):
    b, s, d = h.shape
    t = b * s
    a = t * TOP_K
    hf = h.reshape(t, d)
    hf32 = hf.astype(jnp.float32)
    g_logits = hf32 @ w_rg.astype(jnp.float32) + b_rg.astype(jnp.float32)
    g_prob, g_idx = lax.top_k(jax.nn.softmax(g_logits, axis=-1), 1)
    e_logits = (hf32 @ w_re.astype(jnp.float32) + b_re.astype(jnp.float32)).reshape(t, N_GROUPS, EXPERTS_PER_GROUP)
    e_sel = jnp.take_along_axis(e_logits, g_idx[:, :, None], axis=1)[:, 0]
    top_l, top_i = lax.top_k(e_sel, TOP_K)
    weights = (g_prob * jax.nn.softmax(top_l, axis=-1)).astype(h.dtype)
    eid = g_idx * EXPERTS_PER_GROUP + top_i

    e_flat = eid.reshape(a).astype(jnp.int32)
    t_flat = jnp.repeat(jnp.arange(t, dtype=jnp.int32), TOP_K)
    w_flat = weights.reshape(a)
    order = jnp.argsort(e_flat)
    e_s, t_s, w_s = e_flat[order], t_flat[order], w_flat[order]
    counts = jnp.zeros((N_EXPERTS,), jnp.int32).at[e_flat].add(1)
    starts = jnp.cumsum(counts) - counts
    padded = ((counts + MOE_BLOCK - 1) // MOE_BLOCK) * MOE_BLOCK
    pends = jnp.cumsum(padded)
    pstarts = pends - padded
    dest = pstarts[e_s] + (jnp.arange(a, dtype=jnp.int32) - starts[e_s])
    rows = a + N_EXPERTS * MOE_BLOCK
    nb = rows // MOE_BLOCK
    row_token = jnp.zeros((rows,), jnp.int32).at[dest].set(t_s)
    row_weight = jnp.zeros((rows,), h.dtype).at[dest].set(w_s)
    block_expert = jnp.minimum(
        jnp.searchsorted(pends, jnp.arange(nb, dtype=jnp.int32) * MOE_BLOCK, side='right'),
        N_EXPERTS - 1).astype(jnp.int32)
    xs = hf[row_token].reshape(nb, MOE_BLOCK, d)

    def expert_block(args):
        xb, e = args
        g = xb @ w_gate[e]
        u = xb @ w_up[e]
        return (jax.nn.silu(g) * u) @ w_down[e]

    ys = lax.map(expert_block, (xs, block_expert)).reshape(rows, d)
    out = jnp.zeros((t, d), h.dtype).at[row_token].add(ys * row_weight[:, None])
    return out.reshape(b, s, d)


def setup_inputs(seed: int = 0) -> dict:
    key = jax.random.key(seed)
    ks = jax.random.split(key, 32)
    f32 = jnp.float32
    L, D = DEPTH, D_MODEL

    def nrm(k, shape, std):
        return jax.random.normal(k, shape, f32) * std

    def gain(k, shape):
        return 1.0 + 0.02 * jax.random.normal(k, shape, f32)

    return {
        'x': nrm(ks[0], (BATCH, SEQ, D), 1.0),
        'c': nrm(ks[1], (BATCH, D), 1.0),
        'w_ada': nrm(ks[2], (L, D, 6 * D), 0.5 * D ** -0.5),
        'b_ada': nrm(ks[3], (L, 6 * D), 0.02),
        'norm1_g': gain(ks[4], (L, D)),
        'w_in': nrm(ks[5], (L, D, IN_COLS), D ** -0.5),
        'q_a_norm_g': gain(ks[6], (L, MLA_Q_RANK)),
        'w_q_up': nrm(ks[7], (L, MLA_Q_RANK, MLA_HEADS * (MLA_NOPE_DIM + MLA_ROPE_DIM)), MLA_Q_RANK ** -0.5),
        'kv_a_norm_g': gain(ks[8], (L, MLA_KV_RANK)),
        'w_kv_up': nrm(ks[9], (L, MLA_KV_RANK, MLA_HEADS * (MLA_NOPE_DIM + MLA_V_DIM)), MLA_KV_RANK ** -0.5),
        'lambda_q1': nrm(ks[10], (L, DIFF_QK_DIM), 0.1),
        'lambda_k1': nrm(ks[11], (L, DIFF_QK_DIM), 0.1),
        'lambda_q2': nrm(ks[12], (L, DIFF_QK_DIM), 0.1),
        'lambda_k2': nrm(ks[13], (L, DIFF_QK_DIM), 0.1),
        'subln_g': gain(ks[14], (L, DIFF_V_DIM)),
        'w_out': nrm(ks[15], (L, MIX_WIDTH, D), MIX_WIDTH ** -0.5),
        'norm2_g': gain(ks[16], (L, D)),
        'w_router_group': nrm(ks[17], (L, D, N_GROUPS), D ** -0.5),
        'b_router_group': nrm(ks[18], (L, N_GROUPS), 0.01),
        'w_router_expert': nrm(ks[19], (L, D, N_EXPERTS), D ** -0.5),
        'b_router_expert': nrm(ks[20], (L, N_EXPERTS), 0.01),
        'w_expert_gate': nrm(ks[21], (L, N_EXPERTS, D, D_EXPERT), D ** -0.5),
        'w_expert_up': nrm(ks[22], (L, N_EXPERTS, D, D_EXPERT), D ** -0.5),
        'w_expert_down': nrm(ks[23], (L, N_EXPERTS, D_EXPERT, D), D_EXPERT ** -0.5),
        'final_norm_g': gain(ks[24], (D,)),
    }


def reference(x, c, w_ada, b_ada, norm1_g, w_in, q_a_norm_g, w_q_up, kv_a_norm_g, w_kv_up,
              lambda_q1, lambda_k1, lambda_q2, lambda_k2, subln_g, w_out, norm2_g,
              w_router_group, b_router_group, w_router_expert, b_router_expert,
              w_expert_gate, w_expert_up, w_expert_down, final_norm_g):
    b, s, _ = x.shape
    pos = jnp.arange(s, dtype=jnp.int32)
    offsets = [int(v) for v in np.cumsum(IN_SPLITS)[:-1]]
    for l in range(DEPTH):
        mod = jax.nn.silu(c) @ w_ada[l] + b_ada[l]
        shift1, scale1, gate1, shift2, scale2, gate2 = jnp.split(mod, 6, axis=-1)

        h = rms_norm(x, norm1_g[l]) * (1.0 + scale1[:, None, :]) + shift1[:, None, :]
        z = h @ w_in[l]
        c_q, c_kv, k_pe, dq, dk, dv = jnp.split(z, offsets, axis=-1)

        q = (rms_norm(c_q, q_a_norm_g[l]) @ w_q_up[l]).reshape(b, s, MLA_HEADS, MLA_NOPE_DIM + MLA_ROPE_DIM)
        q_nope, q_pe = q[..., :MLA_NOPE_DIM], rope(q[..., MLA_NOPE_DIM:], pos)
        kv = (rms_norm(c_kv, kv_a_norm_g[l]) @ w_kv_up[l]).reshape(b, s, MLA_HEADS, MLA_NOPE_DIM + MLA_V_DIM)
        k_nope, v_mla = kv[..., :MLA_NOPE_DIM], kv[..., MLA_NOPE_DIM:]
        k_pe = rope(k_pe[:, :, None, :], pos)[:, :, 0, :]
        mla_out = mla_attention(q_nope, q_pe, k_nope, k_pe, v_mla).reshape(b, s, MLA_HEADS * MLA_V_DIM)

        dq = rope(dq.reshape(b, s, DIFF_HEADS, 2, DIFF_QK_DIM), pos)
        dk = rope(dk.reshape(b, s, DIFF_HEADS, 2, DIFF_QK_DIM), pos)
        dv = dv.reshape(b, s, DIFF_HEADS, DIFF_V_DIM)
        lambda_init = 0.8 - 0.6 * math.exp(-0.3 * l)
        lam = (jnp.exp(jnp.sum(lambda_q1[l].astype(jnp.float32) * lambda_k1[l].astype(jnp.float32)))
               - jnp.exp(jnp.sum(lambda_q2[l].astype(jnp.float32) * lambda_k2[l].astype(jnp.float32)))
               + lambda_init)
        d_out = diff_attention(dq[..., 0, :], dq[..., 1, :], dk[..., 0, :], dk[..., 1, :], dv, lam)
        d_out = (rms_norm(d_out, subln_g[l]) * (1.0 - lambda_init)).reshape(b, s, DIFF_HEADS * DIFF_V_DIM)

        mix = jnp.concatenate([mla_out, d_out], axis=-1) @ w_out[l]
        x = x + gate1[:, None, :] * mix

        h2 = rms_norm(x, norm2_g[l]) * (1.0 + scale2[:, None, :]) + shift2[:, None, :]
        moe = hier_moe(h2, w_router_group[l], b_router_group[l], w_router_expert[l], b_router_expert[l],
                       w_expert_gate[l], w_expert_up[l], w_expert_down[l])
        x = x + gate2[:, None, :] * moe
    return rms_norm(x, final_norm_g)
```

```python
import math
from contextlib import ExitStack

import numpy as np
import concourse.bass as bass
import concourse.mybir as mybir
from concourse.bass_utils import run_bass_kernel_spmd

F32 = mybir.dt.float32
BF16 = mybir.dt.bfloat16
AF = mybir.ActivationFunctionType
ALU = mybir.AluOpType
AX = mybir.AxisListType

D = 1024
EPS = 1e-6
LAMBDA_INIT = 0.8 - 0.6 * math.exp(0.0)
NE = 32
DE = 512

_off = {}
_n = 0
for _name, _w in [("c", 8), ("bada", 32), ("g1", 8), ("g2", 8), ("qg", 2), ("kvg", 2),
                  ("lq1", 64), ("lk1", 64), ("lq2", 64), ("lk2", 64), ("subln", 128),
                  ("brt", 36), ("ident", 128), ("utri", 128), ("pcol", 1)]:
    _off[_name] = (_n, _w)
    _n += _w
NCST = _n


class Eng:
    def __init__(self, e, sem):
        self.e = e
        self.sem = sem
        self.n = 0
        self.seen = {}
        self.dcnt = {}

    def wait(self, *toks):
        for t in toks:
            if t is None:
                continue
            if isinstance(t, list):
                self.wait(*t)
                continue
            sem, v = t
            k = id(sem)
            if self.seen.get(k, 0) >= v:
                continue
            self.e.wait_ge(sem, v)
            self.seen[k] = v

    def inc(self, inst):
        inst.then_inc(self.sem, 1)
        self.n += 1
        return (self.sem, self.n)

    def dma(self, out, in_, sem, deps=(), **kw):
        self.wait(*deps)
        c = self.dcnt.get(id(sem), 0) + 16
        self.dcnt[id(sem)] = c
        self.e.dma_start(out=out, in_=in_, **kw).then_inc(sem, 16)
        return (sem, c)


class Banks:
    def __init__(self, banks):
        self.banks = banks
        self.free = [None] * len(banks)
        self.i = 0

    def get(self):
        i = self.i
        self.i = (i + 1) % len(self.banks)
        return i, self.banks[i], self.free[i]

    def rel(self, i, tok):
        self.free[i] = tok


def build(S, stop=None, dbg=False):
    assert S % 512 == 0
    NCH = S // 512
    NT = S // 128
    BLK = 512
    ROWS = 2 * S + NE * BLK
    NB = ROWS // BLK
    nc = bass.Bass("TRN2", target_bir_lowering=False)
    skind = "ExternalOutput" if dbg else "Internal"

    def dram_in(name, shape, dt=F32):
        return nc.dram_tensor(name, shape, dt, kind="ExternalInput").ap()

    x_d = dram_in("x", [S, D])
    cst_d = dram_in("cst", [128, NCST])
    cst2_d = dram_in("cst2", [128, 3 * D])
    wada_d = dram_in("w_ada", [D, 6 * D])
    win_d = dram_in("w_in_ext", [D, 3200])
    wq_d = dram_in("wq_ext", [256, 1024])
    wkv_d = dram_in("wkv_ext", [256, 1024])
    cos_d = dram_in("cosT", [128, S])
    sin_d = dram_in("sinT", [128, S])
    wout_d = dram_in("w_out", [D, D])
    wr_d = dram_in("w_router", [D, 36])
    weg_d = dram_in("w_eg", [NE, D, DE])
    weu_d = dram_in("w_eu", [NE, D, DE])
    wed_d = dram_in("w_ed", [NE, DE, D])
    y_d = nc.dram_tensor("y", [S, D], F32, kind="ExternalOutput").ap()

    def scr(name, shape, dt=BF16):
        return nc.dram_tensor(name, shape, dt, kind=skind).ap()

    qnT_d = scr("s_qnT", [4, 128, S])
    qpT_d = scr("s_qpT", [4, 64, S])
    knT_d = scr("s_knT", [4, 128, S])
    kpT_d = scr("s_kpT", [64, S])
    dqT_d = scr("s_dqT", [4, 128, S])
    dkT_d = scr("s_dkT", [4, 128, S])
    vm_d = scr("s_vm", [4, S, 129])
    vd_d = scr("s_vd", [4, S, 129])
    ao_d = scr("s_ao", [S, D])
    h2tok_d = scr("s_h2tok", [S, D])
    xs_d = scr("s_xs", [ROWS, D])
    ys_d = scr("s_ys", [ROWS, D], F32)
    wgtab_d = scr("s_wgtab", [NE * 128, 8 * DE])
    wutab_d = scr("s_wutab", [NE * 128, 8 * DE])
    wdtab_d = scr("s_wdtab", [NE * 128, 4 * D])
    bthr_d = dram_in("bthr", [128, NB * 32])
    if dbg:
        dbg_d = nc.dram_tensor("dbg", [128, 4096], F32, kind="ExternalOutput").ap()

    with ExitStack() as top:
        def sb(name, shape, dt, stack=top):
            return stack.enter_context(nc.sbuf_tensor("sb_" + name, shape, dt))

        def sem(name, stack=top):
            return stack.enter_context(nc.semaphore(name))

        block = top.enter_context(nc.Block())
        pe = Eng(nc.tensor, sem("s_pe"))
        act = Eng(nc.scalar, sem("s_act"))
        dve = Eng(nc.vector, sem("s_dve"))
        pool = Eng(nc.gpsimd, sem("s_pool"))
        sp = Eng(nc.sync, sem("s_sp"))
        engs = [pe, act, dve, pool, sp]
        dma_toks = []
        s_tab = sem("d_tab")
        tab_tok = [None]

        pbanks = [top.enter_context(nc.psum_tensor(f"pb{i}", [128, 512], F32)) for i in range(8)]

        def barrier():
            toks = [(e.sem, e.n) for e in engs if e.n > 0] + list(dma_toks)
            for e in engs:
                e.wait(*toks)
            dma_toks.clear()

        cst = sb("cst", [128, NCST], F32)
        s_cst = sem("d_cst")
        t_cst = sp.dma(cst[:], cst_d[:, :], s_cst)

        def C(name, a=None, b=None):
            o, w = _off[name]
            a = 0 if a is None else a
            b = w if b is None else b
            return cst[:, o + a:o + b]

        ident_bf = sb("ident_bf", [128, 128], BF16)
        ones_bf = sb("ones_bf", [128, 128], BF16)
        ones_f = sb("ones_f", [128, 128], F32)
        negh = sb("negh", [128, 8], F32)
        epsc = sb("epsc", [128, 1], F32)
        modc = sb("modc", [128, 32], F32)
        a1c = sb("a1c", [128, 8], F32)
        a2c = sb("a2c", [128, 8], F32)
        gate1_b = sb("gate1_b", [128, D], F32)
        gate2_b = sb("gate2_b", [128, D], F32)
        neglam = sb("neglam", [128, 1], F32)
        subln_b = sb("subln_b", [128, 128], F32)
        M1_all = sb("M1_all", [128, NT, 32], F32)
        M2_all = sb("M2_all", [128, NT, 32], F32)
        w12_all = sb("w12_all", [128, NT, 2], F32)

        dve.wait(t_cst)
        dve.e.tensor_copy(out=ident_bf[:], in_=C("ident"))
        dve.e.memset(ones_bf[:], 1.0)
        dve.e.memset(ones_f[:], 1.0)
        dve.e.memset(epsc[:], EPS)
        t_c0 = dve.inc(dve.e.memset(negh[:], -0.5))

        def rsqrt(out, in_, scale, n, deps):
            pool.wait(*deps)
            t = pool.inc(pool.e.tensor_scalar(out=out, in0=in_, scalar1=float(scale), scalar2=float(EPS),
                                              op0=ALU.mult, op1=ALU.add))
            pool.wait(t, t_c0)
            return pool.inc(pool.e.tensor_tensor(out=out, in0=out, in1=negh[:, 0:n], op=ALU.pow))

        wA_stack = ExitStack()
        winb = sb("winb", [128, 8, 3200], BF16, wA_stack)
        wqb = sb("wqb", [128, 2, 1024], BF16, wA_stack)
        wkvb = sb("wkvb", [128, 2, 1024], BF16, wA_stack)
        s_w = sem("d_wA")
        tw = None
        for i in range(4):
            tw = pool.dma(winb[:, :, i * 800:(i + 1) * 800],
                          win_d[:, i * 800:(i + 1) * 800].rearrange("(j p) n -> p j n", p=128), s_w)
        tw = pool.dma(wqb[:], wq_d[:, :].rearrange("(j p) n -> p j n", p=128), s_w)
        t_wA = pool.dma(wkvb[:], wkv_d[:, :].rearrange("(j p) n -> p j n", p=128), s_w)

        with ExitStack() as ph:
            sT2 = sb("sT2", [128, 8, 2], F32, ph)
            sTb = sb("sTb", [128, 8, 128], F32, ph)
            wa = [sb(f"wa{i}", [128, 8, 1024], F32, ph) for i in range(2)]
            s_wa = [sem(f"d_wa{i}", ph) for i in range(2)]
            lt = sb("lt", [128, 64], F32, ph)
            lsum = sb("lsum", [128, 2], F32, ph)
            bgt = sb("bgt", [128, 2 * D], F32, ph)
            t_bg = sp.dma(bgt[:], cst2_d[:, 0:2 * D], sem("d_bg", ph))

            dve.e.memset(sT2[:], 0.0)
            t = dve.inc(dve.e.memset(lsum[:], 0.0))
            act.wait(t_cst, t)
            t_s = act.inc(act.e.activation(out=sT2[:, :, 0], in_=C("c"), func=AF.Silu))
            act.wait(t_s, t_c0)
            for j in range(8):
                t_sb = act.inc(act.e.activation(out=sTb[:, j, :], in_=ones_f[:], func=AF.Identity,
                                                scale=sT2[:, j, 0:1]))
            for i, (a, b) in enumerate([("lq1", "lk1"), ("lq2", "lk2")]):
                dve.wait(t)
                t = dve.inc(dve.e.tensor_tensor(out=lt[:], in0=C(a), in1=C(b), op=ALU.mult))
                dve.wait(t)
                t = dve.inc(dve.e.reduce_sum(out=lsum[:, i:i + 1], in_=lt[:], axis=AX.X))
            act.wait(t)
            t = act.inc(act.e.activation(out=lsum[:], in_=lsum[:], func=AF.Exp))
            dve.wait(t)
            t = dve.inc(dve.e.tensor_tensor(out=neglam[:], in0=lsum[:, 1:2], in1=lsum[:, 0:1], op=ALU.subtract))
            dve.wait(t)
            t = dve.inc(dve.e.tensor_scalar_add(neglam[:], neglam[:], -LAMBDA_INIT))
            dve.wait(t)
            t_misc = dve.inc(dve.e.tensor_scalar(out=subln_b[:], in0=C("subln"), scalar1=float(1.0 - LAMBDA_INIT),
                                                 scalar2=None, op0=ALU.mult))

            colidx = {0: 0, 1: 1, 3: 2, 4: 3}
            wa_free = [None, None]
            pA = pbanks[0]
            pG = [pbanks[1], pbanks[2], pbanks[3], pbanks[4]]
            t_last_gate = {}
            for g in range(6):
                sl = g % 2
                t_w = sp.dma(wa[sl][:], wada_d[:, g * 1024:(g + 1) * 1024].rearrange("(j p) n -> p j n", p=128),
                             s_wa[sl], deps=[wa_free[sl]])
                pe.wait(t_w, t_s, t_sb)
                if g in colidx:
                    ci = colidx[g]
                    for m in range(8):
                        col = (ci * 8 + m) * 2
                        for j in range(8):
                            ins = pe.e.matmul(pA[:, col:col + 2], lhsT=wa[sl][:, j, m * 128:(m + 1) * 128],
                                              rhs=sT2[:, j, :], start=(j == 0), stop=(j == 7))
                    wa_free[sl] = pe.inc(ins)
                else:
                    gi = 0 if g == 2 else 1
                    for half in range(2):
                        for j in range(8):
                            ins = pe.e.matmul(pG[gi * 2 + half][:, :], lhsT=sTb[:, j, :],
                                              rhs=wa[sl][:, j, half * 512:(half + 1) * 512],
                                              start=(j == 0), stop=(j == 7))
                    wa_free[sl] = pe.inc(ins)
                    t_last_gate[gi] = wa_free[sl]
            t_pe_mod = (pe.sem, pe.n)
            dve.wait(t_pe_mod, t_cst, t_bg)
            pAv = pA[:, 0:64].rearrange("p (c two) -> p c two", two=2)[:, :, 0]
            t = dve.inc(dve.e.tensor_tensor(out=modc[:], in0=pAv, in1=C("bada"), op=ALU.add))
            for gi, gb in enumerate([gate1_b, gate2_b]):
                for half in range(2):
                    t2 = dve.inc(dve.e.tensor_tensor(out=gb[:, half * 512:(half + 1) * 512],
                                                     in0=pG[gi * 2 + half][:, :],
                                                     in1=bgt[:, gi * D + half * 512:gi * D + (half + 1) * 512], op=ALU.add))
            dve.wait(t)
            dve.e.scalar_tensor_tensor(out=a1c[:], in0=modc[:, 8:16], scalar=1.0, in1=C("g1"),
                                       op0=ALU.add, op1=ALU.mult)
            t_mod = dve.inc(dve.e.scalar_tensor_tensor(out=a2c[:], in0=modc[:, 24:32], scalar=1.0, in1=C("g2"),
                                                       op0=ALU.add, op1=ALU.mult))
            if dbg:
                dve.wait(t_mod, t2, t_misc)
                dve.e.tensor_copy(out=cst[:, 0:32], in_=modc[:])
                t = dve.inc(dve.e.tensor_copy(out=cst[:, 32:33], in_=neglam[:]))
                t = sp.dma(dbg_d[:, 0:64], cst[:, 0:64], s_cst, deps=[t])
                dma_toks.append(t)
                t = sp.dma(dbg_d[:, 1024:2048], gate1_b[:], s_cst, deps=[t])
                dma_toks.append(t)
                t = sp.dma(dbg_d[:, 2048:3072], gate2_b[:], s_cst, deps=[t])
                dma_toks.append(t)
            barrier()
        sh1c = modc[:, 0:8]
        sh2c = modc[:, 16:24]

        if stop == "pro":
            return nc

        with ExitStack() as ph:

            xt = [sb(f"xt{i}", [128, 4, D], F32, ph) for i in range(2)]
            cs = [sb(f"cs{i}", [128, 2, 512], F32, ph) for i in range(2)]
            xn = [sb("xn0", [128, 4, D], BF16, ph)] * 2
            hT = [sb(f"hT{i}", [128, 8, 512], BF16, ph) for i in range(2)]
            ss = [sb(f"ss{i}", [128, 4], F32, ph) for i in range(2)]
            rstd = [sb(f"rstd{i}", [128, 4], F32, ph) for i in range(2)]
            cqg = sb("cqg", [128, 2, 512], BF16, ph)
            ckvg = sb("ckvg", [128, 2, 512], BF16, ph)
            sqq = sb("sqq", [128, 2, 512], BF16, ph)
            sqkv = sb("sqkv", [128, 2, 512], BF16, ph)
            rq_b = sb("rq_b", [128, 512], F32, ph)
            rkv_b = sb("rkv_b", [128, 512], F32, ph)
            rkv_t = sb("rkv_t", [128, 4], F32, ph)
            t1 = [sb(f"t1_{i}", [128, 512], F32, ph) for i in range(2)]
            t2 = [sb(f"t2_{i}", [128, 512], F32, ph) for i in range(2)]
            t3 = [sb("t3_0", [128, 512], F32, ph)] * 2
            qn_st = [sb("qn_st0", [128, 4, 512], BF16, ph)] * 2
            qp_st = [sb("qp_st0", [64, 4, 512], BF16, ph)] * 2
            kn_st = [sb("kn_st0", [128, 4, 512], BF16, ph)] * 2
            kp_st = [sb("kp_st0", [64, 512], BF16, ph)] * 2
            dq_st = [sb("dq_st0", [128, 4, 512], BF16, ph)] * 2
            dk_st = [sb("dk_st0", [128, 4, 512], BF16, ph)] * 2
            vm_st = [sb("vm_st0", [128, 4, 4, 129], BF16, ph)] * 2
            vd_st = [sb("vd_st0", [128, 4, 4, 129], BF16, ph)] * 2
            s_x = [sem(f"d_x{i}", ph) for i in range(2)]
            s_cs = [sem(f"d_cs{i}", ph) for i in range(2)]
            s_stg = {k: sem("d_st_" + k, ph) for k in ["kp", "dq", "dk", "vd", "qn", "qp", "kn", "vm"]}
            stg_free = {k: None for k in s_stg}

            dve.e.memset(vm_st[0][:], 1.0)
            t_ms = dve.inc(dve.e.memset(vd_st[0][:], 1.0))

            trb = Banks([pbanks[0], pbanks[1]])
            fb = Banks([pbanks[2], pbanks[3], pbanks[4], pbanks[5], pbanks[6], pbanks[7]])
            xt_free = [None, None]
            cs_free = [None, None]
            xn_free = [None, None]
            hT_free = [None, None]
            st_free = [None, None]
            tmp_free = [None, None]
            tmpi = [0]
            t3_free = [None]
            cq_free = None

            stA = {}

            def frontA(c):
                sl = c % 2
                cols = slice(c * 512, (c + 1) * 512)
                tx = sp.dma(xt[sl][:], x_d[cols, :].rearrange("(t p) n -> p t n", p=128), s_x[sl], deps=[xt_free[sl]])
                sp.dma(cs[sl][:, 0, :], cos_d[:, cols], s_cs[sl], deps=[cs_free[sl]])
                tcs = sp.dma(cs[sl][:, 1, :], sin_d[:, cols], s_cs[sl])
                cosb = cs[sl][:, 0, :]
                sinb = cs[sl][:, 1, :]
                act.wait(tx, xn_free[0], xn_free[1])
                for t in range(4):
                    tss = act.inc(act.e.activation(out=xn[sl][:, t, :], in_=xt[sl][:, t, :], func=AF.Square,
                                                   accum_out=ss[sl][:, t:t + 1]))
                tr = rsqrt(rstd[sl][:], ss[sl][:], 1.0 / D, 4, [tss])
                act.wait(tr, xn_free[sl])
                txn = []
                for t in range(4):
                    txn.append(act.inc(act.e.activation(out=xn[sl][:, t, :], in_=xt[sl][:, t, :], func=AF.Identity,
                                                        scale=rstd[sl][:, t:t + 1])))
                xt_free[sl] = txn[3]
                dve.wait(hT_free[sl], t_mod)
                for t in range(4):
                    bi, bank, bfree = trb.get()
                    bv = bank[:, :].bitcast(BF16)
                    pe.wait(bfree, txn[t])
                    for j in range(8):
                        ins = pe.e.transpose(out=bv[:, j * 128:(j + 1) * 128], in_=xn[sl][:, t, j * 128:(j + 1) * 128],
                                             identity=ident_bf[:])
                    ttr = pe.inc(ins)
                    dve.wait(ttr)
                    for j in range(8):
                        ins = dve.e.tensor_scalar(out=hT[sl][:, j, t * 128:(t + 1) * 128],
                                                  in0=bv[:, j * 128:(j + 1) * 128],
                                                  scalar1=a1c[:, j:j + 1], scalar2=sh1c[:, j:j + 1],
                                                  op0=ALU.mult, op1=ALU.add)
                    thT = dve.inc(ins)
                    trb.rel(bi, thT)
                xn_free[sl] = ttr
                stA[c] = (tcs, thT)

            frontA(0)
            for c in range(NCH):
                sl = c % 2
                cols = slice(c * 512, (c + 1) * 512)
                cosb = cs[sl][:, 0, :]
                sinb = cs[sl][:, 1, :]
                tcs, thT = stA.pop(c)
                def fm(w, col0, M, src, nk, deps):
                    bi, bank, bfree = fb.get()
                    pe.wait(bfree, *deps)
                    for k in range(nk):
                        ins = pe.e.matmul(bank[0:M, :], lhsT=w[:, k, col0:col0 + M], rhs=src[:, k, :],
                                          start=(k == 0), stop=(k == nk - 1))
                    return bi, bank, pe.inc(ins)

                hs = hT[sl]
                pe.wait(t_wA, tw)
                if stop == "A_w":
                    barrier()
                    return nc
                dve.wait(cq_free)
                act.wait(cq_free)
                for (col0, gname, dst, sq) in [(0, "qg", cqg, sqq), (256, "kvg", ckvg, sqkv)]:
                    for m in range(2):
                        bi, bank, tk = fm(winb, col0 + m * 128, 128, hs, 8, [thT])
                        if stop == "A_mm":
                            barrier()
                            return nc
                        dve.wait(tk)
                        ta = dve.inc(dve.e.tensor_scalar(out=dst[:, m, :], in0=bank[:, :], scalar1=C(gname, m, m + 1),
                                                         scalar2=None, op0=ALU.mult))
                        if stop == "A_dve":
                            barrier()
                            return nc
                        act.wait(tk, ta)
                        tb = act.inc(act.e.activation(out=sq[:, m, :], in_=bank[:, :], func=AF.Square))
                        if stop == "A_act":
                            barrier()
                            return nc
                        fb.rel(bi, [ta, tb])
                t_cqg = ta
                t_sq = tb
                if stop == "A_cq":
                    barrier()
                    return nc
                sfree = [v for v in stg_free.values() if v is not None]
                dve.wait(t_ms, *sfree)
                act.wait(t_ms, *sfree)
                pool.wait(*sfree)

                def rope_pair(wa_col, wb_col, M, w, src, nk, deps, out_ap, extra=None):
                    ia, banka, tka = fm(w, wa_col, M, src, nk, deps)
                    ib, bankb, tkb = fm(w, wb_col, M, src, nk, deps)
                    ti = tmpi[0] % 2
                    tmpi[0] += 1
                    dve.wait(tka, tcs, tmp_free[ti])
                    tA = dve.inc(dve.e.tensor_tensor(out=t1[ti][0:M, :], in0=banka[0:M, :], in1=cosb[0:M, :], op=ALU.mult))
                    dve.wait(tkb)
                    tB = dve.inc(dve.e.tensor_tensor(out=t2[ti][0:M, :], in0=bankb[0:M, :], in1=sinb[0:M, :], op=ALU.mult))
                    fb.rel(ia, tA)
                    fb.rel(ib, tB)
                    pool.wait(tA, tB)
                    if extra is None:
                        tC = pool.inc(pool.e.tensor_tensor(out=out_ap, in0=t1[ti][0:M, :], in1=t2[ti][0:M, :], op=ALU.add))
                    else:
                        eap, etok = extra
                        pool.wait(t3_free[0])
                        tC = pool.inc(pool.e.tensor_tensor(out=t3[ti][0:M, :], in0=t1[ti][0:M, :], in1=t2[ti][0:M, :], op=ALU.add))
                        pool.wait(tC, etok)
                        tC = pool.inc(pool.e.tensor_tensor(out=out_ap, in0=t3[ti][0:M, :], in1=eap, op=ALU.mult))
                        t3_free[0] = tC
                    tmp_free[ti] = tC
                    return tC

                t_kp = rope_pair(512, 2112, 64, winb, hs, 8, [thT], kp_st[sl][:, :])
                for h in range(4):
                    t_dq = rope_pair(576 + h * 128, 2176 + h * 128, 128, winb, hs, 8, [thT], dq_st[sl][:, h, :])
                tstat = {}
                for (sq, dst) in [(sqq, rq_b), (sqkv, rkv_b)]:
                    bi, bank, bfree = fb.get()
                    pe.wait(bfree, t_sq)
                    for m in range(2):
                        ins = pe.e.matmul(bank[:, :], lhsT=ones_bf[:], rhs=sq[:, m, :], start=(m == 0), stop=(m == 1))
                    tk = pe.inc(ins)
                    act.wait(tk)
                    tcp = act.inc(act.e.activation(out=dst[:], in_=bank[:, :], func=AF.Sqrt, scale=1.0 / 256,
                                                   bias=epsc[:, 0:1]))
                    fb.rel(bi, tcp)
                    dve.wait(tcp)
                    tstat[id(dst)] = dve.inc(dve.e.reciprocal(out=dst[:], in_=dst[:]))
                t_rq = tstat[id(rq_b)]
                t_rkv = tstat[id(rkv_b)]
                bi, bank, bfree = fb.get()
                pe.wait(bfree)
                for t in range(4):
                    for m in range(2):
                        ins = pe.e.matmul(bank[:, 2 * t:2 * t + 2], lhsT=sqkv[:, m, t * 128:(t + 1) * 128],
                                          rhs=ones_bf[:, 0:2], start=(m == 0), stop=(m == 1))
                tk = pe.inc(ins)
                dve.wait(tk)
                tcp = dve.inc(dve.e.tensor_copy(out=rkv_t[:], in_=bank[:, 0:8].rearrange("p (t two) -> p t two", two=2)[:, :, 0]))
                fb.rel(bi, tcp)
                t_rkvt = rsqrt(rkv_t[:], rkv_t[:], 1.0 / 256, 4, [tcp])
                for h in range(4):
                    t_dk = rope_pair(1088 + h * 128, 2688 + h * 128, 128, winb, hs, 8, [thT], dk_st[sl][:, h, :])
                if stop == "A_rope":
                    barrier()
                    return nc
                for t in range(4):
                    bi, bank, bfree = fb.get()
                    pe.wait(bfree)
                    for k in range(8):
                        ins = pe.e.matmul(bank[:, :], lhsT=hs[:, k, t * 128:(t + 1) * 128], rhs=winb[:, k, 1600:2112],
                                          start=(k == 0), stop=(k == 7))
                    tk = pe.inc(ins)
                    act.wait(tk)
                    t_vd = act.inc(act.e.activation(out=vd_st[sl][:, t, :, 0:128],
                                                    in_=bank[:, :].rearrange("p (h d) -> p h d", h=4),
                                                    func=AF.Identity))
                    fb.rel(bi, t_vd)
                hT_free[sl] = tk
                if stop == "A_dv":
                    barrier()
                    return nc
                if c + 1 < NCH:
                    frontA(c + 1)
                if stop == "A_stat":
                    barrier()
                    return nc
                for h in range(4):
                    bi, bank, tk = fm(wqb, h * 128, 128, cqg, 2, [t_cqg])
                    dve.wait(tk, t_rq)
                    t_qn = dve.inc(dve.e.tensor_tensor(out=qn_st[sl][:, h, :], in0=bank[:, :], in1=rq_b[:], op=ALU.mult))
                    fb.rel(bi, t_qn)
                for h in range(4):
                    t_qp = rope_pair(512 + h * 64, 768 + h * 64, 64, wqb, cqg, 2, [t_cqg], qp_st[sl][:, h, :],
                                     extra=(rq_b[0:64, :], t_rq))
                if stop == "A_q":
                    barrier()
                    return nc
                for h in range(4):
                    bi, bank, tk = fm(wkvb, h * 128, 128, ckvg, 2, [t_cqg])
                    dve.wait(tk, t_rkv)
                    t_kn = dve.inc(dve.e.tensor_tensor(out=kn_st[sl][:, h, :], in0=bank[:, :], in1=rkv_b[:], op=ALU.mult))
                    fb.rel(bi, t_kn)
                for t in range(4):
                    bi, bank, bfree = fb.get()
                    pe.wait(bfree)
                    for k in range(2):
                        ins = pe.e.matmul(bank[:, :], lhsT=ckvg[:, k, t * 128:(t + 1) * 128], rhs=wkvb[:, k, 512:1024],
                                          start=(k == 0), stop=(k == 1))
                    tk = pe.inc(ins)
                    act.wait(tk, t_rkvt)
                    t_vm = act.inc(act.e.activation(out=vm_st[sl][:, t, :, 0:128],
                                                    in_=bank[:, :].rearrange("p (h d) -> p h d", h=4),
                                                    func=AF.Identity, scale=rkv_t[:, t:t + 1]))
                    fb.rel(bi, t_vm)
                cq_free = [tk, t_qp, t_kn, t_vm]
                cs_free[sl] = [t_qp, t_dk]
                if stop == "A_kv":
                    barrier()
                    return nc
                def store(key, out, in_, dep):
                    stg_free[key] = sp.dma(out, in_, s_stg[key], deps=[dep])
                store("kp", kpT_d[:, cols], kp_st[sl][:, :], t_kp)
                store("dq", dqT_d[:, :, cols].rearrange("h p n -> p h n"), dq_st[sl][:], t_dq)
                store("dk", dkT_d[:, :, cols].rearrange("h p n -> p h n"), dk_st[sl][:], t_dk)
                for t in range(4):
                    rows = slice(c * 512 + t * 128, c * 512 + (t + 1) * 128)
                    store("vd", vd_d[:, rows, :].rearrange("h p d -> p h d"), vd_st[sl][:, t, :, :], t_vd)
                store("qn", qnT_d[:, :, cols].rearrange("h p n -> p h n"), qn_st[sl][:], t_qn)
                store("qp", qpT_d[:, :, cols].rearrange("h p n -> p h n"), qp_st[sl][:], t_qp)
                store("kn", knT_d[:, :, cols].rearrange("h p n -> p h n"), kn_st[sl][:], t_kn)
                for t in range(4):
                    rows = slice(c * 512 + t * 128, c * 512 + (t + 1) * 128)
                    store("vm", vm_d[:, rows, :].rearrange("h p d -> p h d"), vm_st[sl][:, t, :, :], t_vm)
            dma_toks.extend(v for v in stg_free.values() if v is not None)
            barrier()
        wA_stack.close()
        if stop == "A":
            return nc
        with ExitStack() as ph:
            NKT = S // 128
            ksb = [sb(f"k_sb{i}", [128, S], BF16, ph) for i in range(2)]
            kpsb = sb("kp_sb", [128, S], BF16, ph)
            vsb = [sb(f"v_sb{i}", [128, NKT, 129], BF16, ph) for i in range(2)]
            qsb = [sb(f"q_sb{i}", [128, 512], BF16, ph) for i in range(2)]
            qpsb = [sb(f"qp_sb{i}", [128, 512], BF16, ph) for i in range(2)]
            qA = [sb(f"qA{i}", [128, 512], BF16, ph) for i in range(2)]
            qB = [sb(f"qB{i}", [128, 512], BF16, ph) for i in range(2)]
            pT = [sb(f"pT{i}", [128, 512], BF16, ph) for i in range(3)]
            o1 = sb("o1", [128, 4, 128], F32, ph)
            o2 = sb("o2", [128, 4, 128], F32, ph)
            dd = sb("dd", [128, 4, 128], F32, ph)
            sqt = sb("sqt", [128, 4, 128], F32, ph)
            ssq = sb("ssq", [128, 4], F32, ph)
            rinv = sb("rinv", [128, 4], F32, ph)
            accs = sb("accs", [128, 4, 129], F32, ph)
            ao_st = [sb(f"ao_st{i}", [128, 4, 128], BF16, ph) for i in range(2)]
            s_k = [sem(f"d_k{i}", ph) for i in range(2)]
            s_v = [sem(f"d_v{i}", ph) for i in range(2)]
            s_kp = sem("d_kp", ph)
            s_q = [sem(f"d_q{i}", ph) for i in range(2)]
            s_ao = [sem(f"d_ao{i}", ph) for i in range(2)]
            sbk = pbanks[0:4]
            accb = pbanks[4:8]
            dve.e.memset(kpsb[64:128, :], 0.0)
            for i in range(2):
                dve.e.memset(qpsb[i][64:128, :], 0.0)
                dve.e.memset(qA[i][64:128, :], 0.0)
                t_z = dve.inc(dve.e.memset(qB[i][0:64, :], 0.0))
            pe.wait(t_z)
            t_kp = pool.dma(kpsb[0:64, :], kpT_d[:, :], s_kp)
            heads = [("mla", h) for h in range(4)] + [("dif", h) for h in range(4)]
            kv_free = [None, None]
            q_free = [None, None]
            ao_free = [None, None]
            st = {"acc_free": None, "epi": None, "aoi": 0, "qi": 0}
            kv_tok = {}

            def load_kv(hi):
                kind, h = heads[hi]
                sl = hi % 2
                ksrc = knT_d if kind == "mla" else dkT_d
                vsrc = vm_d if kind == "mla" else vd_d
                tk = pool.dma(ksb[sl][:], ksrc[h, :, :], s_k[sl], deps=[kv_free[sl]])
                tv = None
                for g0 in range(0, NKT, 16):
                    g1 = min(NKT, g0 + 16)
                    tv = pool.dma(vsb[sl][:, g0:g1, :],
                                  vsrc[h, g0 * 128:g1 * 128, :].rearrange("(t p) d -> p t d", p=128), s_v[sl])
                kv_tok[hi] = (tk, tv)

            def load_q(hi, qc):
                next_tab()
                next_zero()
                kind, h = heads[hi]
                qs = st["qi"] % 2
                st["qi"] += 1
                cols = slice(qc * 512, (qc + 1) * 512)
                if kind == "mla":
                    pool.dma(qsb[qs][:], qnT_d[h, :, cols], s_q[qs], deps=[q_free[qs]])
                    t = pool.dma(qpsb[qs][0:64, :], qpT_d[h, :, cols], s_q[qs])
                else:
                    pool.dma(qA[qs][0:64, :], dqT_d[h, 0:64, cols], s_q[qs], deps=[q_free[qs]])
                    t = pool.dma(qB[qs][64:128, :], dqT_d[h, 64:128, cols], s_q[qs])
                return qs, t

            tabq = [(tab, src, e) for e in range(NE) for (tab, src) in ((wgtab_d, weg_d), (wutab_d, weu_d), (wdtab_d, wed_d))]

            def next_tab():
                if not tabq:
                    return
                tab, src, e = tabq.pop(0)
                jn = 4 if tab is wdtab_d else 8
                tab_tok[0] = pool.dma(tab[e * 128:(e + 1) * 128, :].rearrange("p (j n) -> p j n", j=jn),
                                      src[e, :, :].rearrange("(j p) n -> p j n", p=128), s_tab)

            zt = sb("zt", [128, 8, D], BF16, ph)
            s_z = sem("d_z", ph)
            tz = dve.inc(dve.e.memset(zt[:], 0.0))
            zq = [(r0, min(1024, ROWS - r0) // 128) for r0 in range(0, ROWS, 1024)]
            ztok = [None]
            zskip = [3]

            def next_zero():
                if zskip[0] > 0:
                    zskip[0] -= 1
                    return
                if not zq:
                    return
                r0, nr = zq.pop(0)
                ztok[0] = sp.dma(xs_d[r0:r0 + nr * 128, :].rearrange("(g p) n -> p g n", p=128), zt[:, 0:nr, :], s_z, deps=[tz])

            pre_q = {}
            load_kv(0)
            for hi, (kind, h) in enumerate(heads):
                sl = hi % 2
                q0 = pre_q.pop(hi) if hi in pre_q else load_q(hi, 0)
                if hi + 1 < len(heads):
                    load_kv(hi + 1)
                tkl, tvl = kv_tok[hi]
                subs = [0] if kind == "mla" else [0, 1]
                scale = (192.0 ** -0.5) if kind == "mla" else 0.125
                items = [(qc, s, kt) for qc in range(NCH) for s in subs for kt in range(NKT)]
                N = len(items)
                tok_qk, tok_exp, tok_pv = {}, {}, {}
                qinfo = {0: q0}

                def emit_qk(i):
                    qc, s, kt = items[i]
                    if s == 0 and kt == 0 and qc + 1 < NCH:
                        qinfo[qc + 1] = load_q(hi, qc + 1)
                    if s == 0 and kt == 0 and qc == NCH - 1 and hi + 1 < len(heads):
                        pre_q[hi + 1] = load_q(hi + 1, 0)
                    qs, tq = qinfo[qc]
                    bank = sbk[i % 4]
                    pe.wait(tq, tkl, tok_exp.get(i - 4))
                    ks = slice(kt * 128, (kt + 1) * 128)
                    if kind == "mla":
                        pe.wait(t_kp)
                        pe.e.matmul(bank[:, :], lhsT=ksb[sl][:, ks], rhs=qsb[qs][:, :], start=True, stop=False)
                        ins = pe.e.matmul(bank[:, :], lhsT=kpsb[:, ks], rhs=qpsb[qs][:, :], start=False, stop=True)
                    else:
                        qq = qA if s == 0 else qB
                        ins = pe.e.matmul(bank[:, :], lhsT=ksb[sl][:, ks], rhs=qq[qs][:, :], start=True, stop=True)
                    tok_qk[i] = pe.inc(ins)
                    if s == subs[-1] and kt == NKT - 1:
                        q_free[qs] = tok_qk[i]

                def emit_exp(i):
                    act.wait(tok_qk[i], tok_pv.get(i - 3))
                    tok_exp[i] = act.inc(act.e.activation(out=pT[i % 3][:], in_=sbk[i % 4][:, :], func=AF.Exp,
                                                          scale=float(scale)))

                def epilogue(i):
                    qc, s, kt = items[i]
                    dve.wait(tok_pv[i], st["epi"])
                    traw = []
                    for qb in range(4):
                        traw.append(dve.inc(dve.e.tensor_copy(out=accs[:, qb, :], in_=accb[qb][:, 0:129])))
                    st["acc_free"] = traw
                    dve.wait(traw[3])
                    t = dve.inc(dve.e.reciprocal(out=rinv[:], in_=accs[:, :, 128]))
                    dve.wait(t)
                    rinv_b = rinv[:, :].unsqueeze(2).broadcast_to([128, 4, 128])
                    rows = slice(qc * 512, (qc + 1) * 512)
                    if kind == "mla":
                        asl = st["aoi"] % 2
                        st["aoi"] += 1
                        dve.wait(ao_free[asl])
                        t = dve.inc(dve.e.tensor_tensor(out=ao_st[asl][:], in0=accs[:, :, 0:128], in1=rinv_b, op=ALU.mult))
                        st["epi"] = t
                        ao_free[asl] = sp.dma(ao_d[rows, h * 128:(h + 1) * 128].rearrange("(t p) d -> p t d", p=128),
                                              ao_st[asl][:], s_ao[asl], deps=[t])
                        return
                    dst = o1 if s == 0 else o2
                    t = dve.inc(dve.e.tensor_tensor(out=dst[:], in0=accs[:, :, 0:128], in1=rinv_b, op=ALU.mult))
                    st["epi"] = t
                    if s == 0:
                        return
                    dve.wait(t, t_misc)
                    t = dve.inc(dve.e.scalar_tensor_tensor(out=dd[:], in0=o2[:], scalar=neglam[:, 0:1], in1=o1[:],
                                                           op0=ALU.mult, op1=ALU.add))
                    dve.wait(t)
                    t = dve.inc(dve.e.tensor_tensor(out=sqt[:], in0=dd[:], in1=dd[:], op=ALU.mult))
                    dve.wait(t)
                    t = dve.inc(dve.e.reduce_sum(out=ssq[:], in_=sqt[:], axis=AX.X))
                    tr = rsqrt(ssq[:], ssq[:], 1.0 / 128, 4, [t])
                    asl = st["aoi"] % 2
                    st["aoi"] += 1
                    dve.wait(tr, ao_free[asl])
                    for qb in range(4):
                        t = dve.inc(dve.e.scalar_tensor_tensor(out=ao_st[asl][:, qb, :], in0=dd[:, qb, :],
                                                               scalar=ssq[:, qb:qb + 1], in1=subln_b[:],
                                                               op0=ALU.mult, op1=ALU.mult))
                    st["epi"] = t
                    ao_free[asl] = sp.dma(ao_d[rows, 512 + h * 128:512 + (h + 1) * 128].rearrange("(t p) d -> p t d", p=128),
                                          ao_st[asl][:], s_ao[asl], deps=[t])

                def emit_pv(i):
                    qc, s, kt = items[i]
                    pe.wait(tok_exp[i], tvl)
                    for qb in range(4):
                        if kt == 0 and st["acc_free"] is not None:
                            pe.wait(st["acc_free"][qb])
                        ins = pe.e.matmul(accb[qb][:, 0:129], lhsT=pT[i % 3][:, qb * 128:(qb + 1) * 128],
                                          rhs=vsb[sl][:, kt, :], start=(kt == 0), stop=(kt == NKT - 1))
                    tok_pv[i] = pe.inc(ins)
                    if kt == NKT - 1:
                        epilogue(i)

                LA = 3
                for i in range(min(LA, N)):
                    emit_qk(i)
                for i in range(N):
                    emit_exp(i)
                    if i + LA < N:
                        emit_qk(i + LA)
                    emit_pv(i)
                kv_free[sl] = tok_pv[N - 1]
            while tabq:
                next_tab()
            zskip[0] = 0
            while zq:
                next_zero()
            dma_toks.append(ztok[0])
            dma_toks.extend(v for v in ao_free if v is not None)
            barrier()
        if stop == "B":
            return nc
        with ExitStack() as ph:
            woutb = sb("woutb", [128, 8, D], BF16, ph)
            wr = sb("wr", [128, 8, 36], F32, ph)
            s_wc = sem("d_wC", ph)
            wst = sb("wst", [128, 8, D], F32, ph)
            t_wst = sp.dma(wst[:], wout_d[:, :].rearrange("(j p) n -> p j n", p=128), s_wc)
            dve.wait(t_wst, t_mod)
            for j in range(8):
                t_wg = dve.inc(dve.e.tensor_tensor(out=woutb[:, j, :], in0=wst[:, j, :], in1=gate1_b[:], op=ALU.mult))
            t_wr = pool.dma(wr[:], wr_d[:, :].rearrange("(j p) n -> p j n", p=128), sem("d_wr", ph))
            t_wC = [t_wg, t_wr]
            ao_t = [sb(f"ao_t{i}", [128, D], BF16, ph) for i in range(2)]
            x_t = [sb(f"x_t{i}", [128, D], F32, ph) for i in range(2)]
            aoT = [sb(f"aoT{i}", [128, 8, 128], BF16, ph) for i in range(2)]
            xm = [sb(f"xm{i}", [128, D], F32, ph) for i in range(2)]
            xn2 = [sb(f"xn2_{i}", [128, D], F32, ph) for i in range(2)]
            h2T = [sb(f"h2T{i}", [128, 8, 128], F32, ph) for i in range(2)]
            h2Tb = [sb(f"h2Tb{i}", [128, 8, 128], BF16, ph) for i in range(2)]
            h2tk = [sb(f"h2tk{i}", [128, D], BF16, ph) for i in range(2)]
            ss2 = sb("ss2", [128, 2], F32, ph)
            L_all = sb("L_all", [128, NT, 36], F32, ph)
            gmx = sb("gmx", [128, NT], F32, ph)
            gmB = sb("gmB", [128, NT, 4], F32, ph)
            r4B = sb("r4B", [128, NT, 4], F32, ph)
            gsB = sb("gsB", [128, NT], F32, ph)
            esB = sb("esB", [128, NT, 8], F32, ph)
            tmpB = sb("tmpB", [128, NT, 8], F32, ph)
            e2B = sb("e2B", [128, NT, 8], F32, ph)
            mk1B = sb("mk1B", [128, NT, 8], F32, ph)
            mk2B = sb("mk2B", [128, NT, 8], F32, ph)
            m1B = sb("m1B", [128, NT], F32, ph)
            m2B = sb("m2B", [128, NT], F32, ph)
            r4 = sb("r4", [128, 4], F32, ph)
            gm = sb("gm", [128, 4], F32, ph)
            sc = sb("rsc", [128, 16], F32, ph)
            es = sb("es", [128, 8], F32, ph)
            e2 = sb("e2", [128, 8], F32, ph)
            mk1 = sb("mk1", [128, 8], F32, ph)
            mk2 = sb("mk2", [128, 8], F32, ph)
            wm = sb("wm", [128, 8], F32, ph)
            s_aot = [sem(f"d_aot{i}", ph) for i in range(2)]
            s_xt = [sem(f"d_xt{i}", ph) for i in range(2)]
            s_xm = [sem(f"d_xm{i}", ph) for i in range(2)]
            s_h2 = [sem(f"d_h2{i}", ph) for i in range(2)]
            aot_free = [None, None]
            xt_free = [None, None]
            xm_free = [None, None]
            h2b_free = [None, None]
            h2tb_rd = [None, None]
            bT = pbanks[0][:, :].bitcast(BF16)
            bM = [pbanks[1], pbanks[2]]
            bX = [pbanks[3], pbanks[4]]
            bR = pbanks[5]
            bT2 = pbanks[6][:, :].bitcast(BF16)
            fr = {"bT2": None, "bT": None, "bM": None, "bX": None, "bR": None, "chain": None}
            aoT_free = [None, None]
            xn2_free = [None, None]
            h2T_free = [None, None]
            stt = {}

            def dv(inst):
                tkn = dve.inc(inst)
                dve.wait(tkn)
                return tkn

            def front(t):
                sl = t % 2
                rows = slice(t * 128, (t + 1) * 128)
                ta = pool.dma(ao_t[sl][:], ao_d[rows, :], s_aot[sl], deps=[aot_free[sl]])
                txl = sp.dma(x_t[sl][:], x_d[rows, :], s_xt[sl], deps=[xt_free[sl]])
                pe.wait(ta, fr["bT"])
                for j in range(8):
                    ins = pe.e.transpose(out=bT[:, j * 128:(j + 1) * 128], in_=ao_t[sl][:, j * 128:(j + 1) * 128],
                                         identity=ident_bf[:])
                tk = pe.inc(ins)
                aot_free[sl] = tk
                act.wait(tk, aoT_free[sl])
                tcp = act.inc(act.e.activation(out=aoT[sl][:].rearrange("p j n -> p (j n)"), in_=bT[:, :], func=AF.Identity))
                fr["bT"] = tcp
                pe.wait(tcp, t_wC, fr["bM"])
                for half in range(2):
                    for j in range(8):
                        ins = pe.e.matmul(bM[half][:, :], lhsT=aoT[sl][:, j, :], rhs=woutb[:, j, half * 512:(half + 1) * 512],
                                          start=(j == 0), stop=(j == 7))
                tk = pe.inc(ins)
                aoT_free[sl] = tk
                dve.wait(tk, xm_free[sl], txl)
                for half in range(2):
                    hs_ = slice(half * 512, (half + 1) * 512)
                    txm = dve.inc(dve.e.tensor_tensor(out=xm[sl][:, hs_], in0=bM[half][:, :], in1=x_t[sl][:, hs_], op=ALU.add))
                fr["bM"] = txm
                xt_free[sl] = txm
                act.wait(txm, xn2_free[sl])
                tss = act.inc(act.e.activation(out=xn2[sl][:], in_=xm[sl][:], func=AF.Square, accum_out=ss2[:, sl:sl + 1]))
                tr = rsqrt(ss2[:, sl:sl + 1], ss2[:, sl:sl + 1], 1.0 / D, 1, [tss])
                act.wait(tr)
                txn = act.inc(act.e.activation(out=xn2[sl][:], in_=xm[sl][:], func=AF.Identity, scale=ss2[:, sl:sl + 1]))
                stt[t] = (txn, tss)

            def back(t):
                sl = t % 2
                rows = slice(t * 128, (t + 1) * 128)
                txn, tss = stt.pop(t)
                pe.wait(txn, fr["bX"])
                for j in range(8):
                    ins = pe.e.transpose(out=bX[j // 4][:, (j % 4) * 128:(j % 4 + 1) * 128], in_=xn2[sl][:, j * 128:(j + 1) * 128],
                                         identity=C("ident"))
                tk = pe.inc(ins)
                xn2_free[sl] = tk
                dve.wait(tk, h2T_free[sl], t_mod)
                for j in range(8):
                    th = dve.inc(dve.e.tensor_scalar(out=h2T[sl][:, j, :], in0=bX[j // 4][:, (j % 4) * 128:(j % 4 + 1) * 128],
                                                     scalar1=a2c[:, j:j + 1], scalar2=sh2c[:, j:j + 1],
                                                     op0=ALU.mult, op1=ALU.add))
                fr["bX"] = th
                act.wait(th, h2tb_rd[sl])
                thb = act.inc(act.e.activation(out=h2Tb[sl][:].rearrange("p j n -> p (j n)"),
                                               in_=h2T[sl][:].rearrange("p j n -> p (j n)"), func=AF.Identity))
                pe.wait(th, fr["bR"])
                for j in range(8):
                    ins = pe.e.matmul(bR[:, 0:36], lhsT=h2T[sl][:, j, :], rhs=wr[:, j, :], start=(j == 0), stop=(j == 7))
                tk = pe.inc(ins)
                h2T_free[sl] = [tk, thb]
                pe.wait(thb, fr["bT2"])
                for j in range(8):
                    ins = pe.e.transpose(out=bT2[:, j * 128:(j + 1) * 128], in_=h2Tb[sl][:, j, :], identity=ident_bf[:])
                tk2 = pe.inc(ins)
                h2tb_rd[sl] = tk2
                act.wait(tk2, h2b_free[sl])
                tcp2 = act.inc(act.e.activation(out=h2tk[sl][:], in_=bT2[:, :], func=AF.Identity))
                fr["bT2"] = tcp2
                dve.wait(tk)
                tl = dve.inc(dve.e.tensor_tensor(out=L_all[:, t, :], in0=bR[:, 0:36], in1=C("brt"), op=ALU.add))
                fr["bR"] = tl
                stt["L"] = tl
                xm_free[sl] = sp.dma(y_d[rows, :], xm[sl][:], s_xm[sl], deps=[txn, tss])
                h2b_free[sl] = sp.dma(h2tok_d[rows, :], h2tk[sl][:], s_h2[sl], deps=[tcp2])

            front(0)
            for t in range(NT):
                if t + 1 < NT:
                    front(t + 1)
                back(t)

            def bc(ap2, n):
                return ap2.unsqueeze(2).broadcast_to([128, NT, n])
            L4 = L_all[:, :, 0:4]
            dve.wait(stt["L"])
            dv(dve.e.tensor_reduce(out=gmx[:], in_=L4, axis=AX.X, op=ALU.max))
            dv(dve.e.tensor_tensor(out=gmB[:], in0=L4, in1=bc(gmx[:, :], 4), op=ALU.is_equal))
            tg = dv(dve.e.tensor_tensor(out=r4B[:], in0=L4, in1=bc(gmx[:, :], 4), op=ALU.subtract))
            act.wait(tg)
            te = act.inc(act.e.activation(out=r4B[:], in_=r4B[:], func=AF.Exp))
            for g_ in range(4):
                dst = esB if g_ == 0 else tmpB
                dv(dve.e.tensor_tensor(out=dst[:], in0=L_all[:, :, 4 + 8 * g_:12 + 8 * g_],
                                       in1=gmB[:, :, g_:g_ + 1].broadcast_to([128, NT, 8]), op=ALU.mult))
                if g_ > 0:
                    dv(dve.e.tensor_tensor(out=esB[:], in0=esB[:], in1=tmpB[:], op=ALU.add))
            dv(dve.e.tensor_reduce(out=m1B[:], in_=esB[:], axis=AX.X, op=ALU.max))
            dv(dve.e.tensor_tensor(out=mk1B[:], in0=esB[:], in1=bc(m1B[:, :], 8), op=ALU.is_equal))
            dv(dve.e.scalar_tensor_tensor(out=e2B[:], in0=mk1B[:], scalar=-1e30, in1=esB[:], op0=ALU.mult, op1=ALU.add))
            dv(dve.e.tensor_reduce(out=m2B[:], in_=e2B[:], axis=AX.X, op=ALU.max))
            dv(dve.e.tensor_tensor(out=mk2B[:], in0=e2B[:], in1=bc(m2B[:, :], 8), op=ALU.is_equal))
            tn = dv(dve.e.tensor_tensor(out=m2B[:], in0=m2B[:], in1=m1B[:], op=ALU.subtract))
            act.wait(tn, te)
            te2 = act.inc(act.e.activation(out=m2B[:], in_=m2B[:], func=AF.Exp))
            dve.wait(te, te2)
            dv(dve.e.tensor_reduce(out=gsB[:], in_=r4B[:], axis=AX.X, op=ALU.add))
            dv(dve.e.reciprocal(out=gsB[:], in_=gsB[:]))
            dv(dve.e.tensor_scalar_add(m1B[:], m2B[:], 1.0))
            dv(dve.e.reciprocal(out=m1B[:], in_=m1B[:]))
            dv(dve.e.tensor_tensor(out=w12_all[:, :, 0], in0=m1B[:], in1=gsB[:], op=ALU.mult))
            dv(dve.e.tensor_tensor(out=w12_all[:, :, 1], in0=w12_all[:, :, 0], in1=m2B[:], op=ALU.mult))
            for Mk, mk in ((M1_all, mk1B), (M2_all, mk2B)):
                dv(dve.e.tensor_tensor(out=Mk[:].rearrange("p t (g e) -> p t g e", g=4),
                                       in0=gmB[:, :, :].unsqueeze(3).broadcast_to([128, NT, 4, 8]),
                                       in1=mk[:, :, :].unsqueeze(2).broadcast_to([128, NT, 4, 8]), op=ALU.mult))
            dma_toks.extend(v for v in xm_free + h2b_free if v is not None)
            barrier()
        if stop == "C":
            if dbg:
                t = sp.dma(dbg_d[:, 0:NT * 32], M1_all[:].rearrange("p t e -> p (t e)"), s_cst)
                sp.wait(t)
            return nc

        NTE = NT * 32
        I32 = mybir.dt.int32
        d1 = ExitStack()
        dest_i = sb("dest_i", [128, 2, NT], I32)
        idxw_i = sb("idxw_i", [128, NB], I32)
        with d1 as ph:
            bthr = sb("bthr", [128, NB, 32], F32, ph)
            t_bt = sp.dma(bthr[:].rearrange("p b e -> p (b e)"), bthr_d[:, :], sem("d_bthr", ph))
            Mb = sb("Mb", [128, NTE], BF16, ph)
            Ub = sb("Ub", [128, 128], BF16, ph)
            Rs = sb("Rs", [128, NT, 32], F32, ph)
            Tts = sb("Tts", [128, NT, 32], F32, ph)
            Pfx = sb("Pfx", [128, NT, 32], F32, ph)
            prod = sb("prod", [128, NT, 32], F32, ph)
            cnt = sb("cnt", [128, 32], F32, ph)
            nbk = sb("nbk", [128, 32], F32, ph)
            pend = sb("pend", [128, 32], F32, ph)
            pstart = sb("pstart", [128, 32], F32, ph)
            NBC = min(32, NB)
            cmpa = sb("cmpa", [128, 32, NBC], F32, ph)
            cmpb = sb("cmpb", [128, NB, 32], F32, ph)
            ebf = sb("ebf", [128, NB], F32, ph)
            dstf = sb("dstf", [128, 2, NT], F32, ph)

            def dv(inst):
                tkn = dve.inc(inst)
                dve.wait(tkn)
                return tkn
            dv(dve.e.tensor_copy(out=Ub[:], in_=C("utri")))
            tmb = dv(dve.e.tensor_tensor(out=Mb[:], in0=M1_all[:].rearrange("p t e -> p (t e)"),
                                         in1=M2_all[:].rearrange("p t e -> p (t e)"), op=ALU.add))
            pe.wait(tmb)
            nchk = (NTE + 511) // 512
            for k in range(nchk):
                c0, c1 = k * 512, min(NTE, (k + 1) * 512)
                pe.e.matmul(pbanks[k][:, 0:c1 - c0], lhsT=Ub[:], rhs=Mb[:, c0:c1], start=True, stop=True)
                ins = pe.e.matmul(pbanks[4 + k][:, 0:c1 - c0], lhsT=ones_bf[:], rhs=Mb[:, c0:c1], start=True, stop=True)
            tk = pe.inc(ins)
            dve.wait(tk)
            for k in range(nchk):
                c0, c1 = k * 512, min(NTE, (k + 1) * 512)
                dve.e.tensor_copy(out=Rs[:].rearrange("p t e -> p (t e)")[:, c0:c1], in_=pbanks[k][:, 0:c1 - c0])
                tcp = dv(dve.e.tensor_copy(out=Tts[:].rearrange("p t e -> p (t e)")[:, c0:c1], in_=pbanks[4 + k][:, 0:c1 - c0]))
            dv(dve.e.memset(Pfx[:, 0, :], 0.0))
            for t in range(1, NT):
                dv(dve.e.tensor_tensor(out=Pfx[:, t, :], in0=Pfx[:, t - 1, :], in1=Tts[:, t - 1, :], op=ALU.add))
            dv(dve.e.tensor_tensor(out=cnt[:], in0=Pfx[:, NT - 1, :], in1=Tts[:, NT - 1, :], op=ALU.add))
            dve.wait(t_bt)
            dv(dve.e.tensor_tensor(out=cmpa[:], in0=cnt[:, :].unsqueeze(2).broadcast_to([128, 32, NBC]),
                                   in1=bthr[:, 0:NBC, :].rearrange("p b e -> p e b"), op=ALU.is_gt))
            dv(dve.e.reduce_sum(out=nbk[:], in_=cmpa[:], axis=AX.X))
            dv(dve.e.tensor_scalar(out=nbk[:], in0=nbk[:], scalar1=float(BLK), scalar2=None, op0=ALU.mult))
            dv(dve.e.tensor_copy(out=pend[:, 0:1], in_=nbk[:, 0:1]))
            for e in range(1, 32):
                dv(dve.e.tensor_tensor(out=pend[:, e:e + 1], in0=pend[:, e - 1:e], in1=nbk[:, e:e + 1], op=ALU.add))
            dv(dve.e.tensor_tensor(out=pstart[:], in0=pend[:], in1=nbk[:], op=ALU.subtract))
            dv(dve.e.tensor_tensor(out=cmpb[:], in0=pend[:, :].unsqueeze(1).broadcast_to([128, NB, 32]), in1=bthr[:],
                                   op=ALU.is_le))
            dv(dve.e.reduce_sum(out=ebf[:], in_=cmpb[:], axis=AX.X))
            dv(dve.e.tensor_scalar(out=ebf[:], in0=ebf[:], scalar1=31.0, scalar2=128.0, op0=ALU.min, op1=ALU.mult))
            dv(dve.e.tensor_scalar(out=ebf[:], in0=ebf[:], scalar1=C("pcol"), scalar2=None, op0=ALU.add))
            t_idxw = dv(dve.e.tensor_copy(out=idxw_i[:], in_=ebf[:]))
            dv(dve.e.tensor_tensor(out=Rs[:], in0=Rs[:], in1=Pfx[:], op=ALU.add))
            dv(dve.e.tensor_tensor(out=Rs[:], in0=Rs[:], in1=pstart[:, :].unsqueeze(1).broadcast_to([128, NT, 32]), op=ALU.add))
            for k, Mk in enumerate([M1_all, M2_all]):
                dv(dve.e.tensor_tensor(out=prod[:], in0=Rs[:], in1=Mk[:], op=ALU.mult))
                dv(dve.e.reduce_sum(out=dstf[:, k, :], in_=prod[:], axis=AX.X))
            t_dest = dv(dve.e.tensor_copy(out=dest_i[:], in_=dstf[:]))
            if dbg:
                sp.dma(dbg_d[:, 0:2 * NT], dstf[:].rearrange("p k t -> p (k t)"), s_cst, deps=[t_dest])
                sp.dma(dbg_d[:, 1024:1024 + NB], ebf[:], s_cst, deps=[t_dest])
                t = sp.dma(dbg_d[:, 2048:2080], cnt[:], s_cst, deps=[t_dest])
                dma_toks.append(t)
            barrier()
        if stop == "D1":
            return nc

        IOA = bass.IndirectOffsetOnAxis
        with ExitStack() as ph:
            NHS = 8
            ht = [sb(f"ht{i}", [128, D], BF16, ph) for i in range(NHS)]
            s_ht = [sem(f"d_ht{i}", ph) for i in range(NHS)]
            s_sc = [sem(f"d_sc{i}", ph) for i in range(NHS)]
            ht_free = [None] * NHS
            for t in range(NT):
                sl = t % NHS
                tl = sp.dma(ht[sl][:], h2tok_d[t * 128:(t + 1) * 128, :], s_ht[sl], deps=[ht_free[sl]])
                pool.wait(tl, t_dest)
                for k in range(2):
                    c = pool.dcnt.get(id(s_sc[sl]), 0) + 16
                    pool.dcnt[id(s_sc[sl])] = c
                    pool.e.indirect_dma_start(out=xs_d[:, :], out_offset=IOA(ap=dest_i[:, k, t:t + 1], axis=0),
                                              in_=ht[sl][:, :], in_offset=None).then_inc(s_sc[sl], 16)
                ht_free[sl] = (s_sc[sl], c)
            dma_toks.extend(v for v in ht_free if v is not None)
            barrier()
        if stop == "D2":
            return nc

        with ExitStack() as ph:
            NQ = BLK // 128
            wgs = [sb(f"wgs{i}", [128, 8, DE], BF16, ph) for i in range(2)]
            wus = [sb(f"wus{i}", [128, 8, DE], BF16, ph) for i in range(2)]
            wds = [sb(f"wds{i}", [128, 4, D], BF16, ph) for i in range(2)]
            xsb = [sb(f"xsb{i}", [128, D], BF16, ph) for i in range(4)]
            xT = [sb(f"xT{i}", [128, 8, BLK], BF16, ph) for i in range(2)]
            sg = [sb(f"sg{i}", [128, 512], F32, ph) for i in range(2)]
            aT = [sb(f"aT{i}", [128, 4, BLK], BF16, ph) for i in range(2)]
            yv = [sb(f"yv{i}", [128, D], F32, ph) for i in range(4)]
            s_wb = [sem(f"d_wb{i}", ph) for i in range(2)]
            s_xs = [sem(f"d_xs{i}", ph) for i in range(4)]
            s_ys = [sem(f"d_ys{i}", ph) for i in range(4)]
            trb = Banks([pbanks[0], pbanks[1]])
            gbk = Banks([pbanks[2], pbanks[3]])
            ubk = Banks([pbanks[4], pbanks[5]])
            ybk = Banks([pbanks[6], pbanks[7]])
            wb_free = [None, None]
            xs_free = [None] * 4
            ys_free = [None] * 4
            xT_free = [None, None]
            xT_ready = {}
            aT_free = [None, None]
            sg_free = [None, None]
            wtok = {}
            cnt = {"x": 0, "f": 0, "y": 0}

            def gather_w(b):
                ws = b % 2
                pool.wait(wb_free[ws], t_idxw, tab_tok[0])
                c = pool.dcnt.get(id(s_wb[ws]), 0)
                for (dst, tab) in ((wgs[ws], wgtab_d), (wus[ws], wutab_d), (wds[ws], wdtab_d)):
                    c += 16
                    pool.e.indirect_dma_start(out=dst[:].rearrange("p j n -> p (j n)"), out_offset=None, in_=tab[:, :],
                                              in_offset=IOA(ap=idxw_i[:, b:b + 1], axis=0)).then_inc(s_wb[ws], 16)
                pool.dcnt[id(s_wb[ws])] = c
                wtok[b] = (s_wb[ws], c)

            def emit_T(b):
                bs = b % 2
                last = None
                for q in range(NQ):
                    sl = cnt["x"] % 4
                    cnt["x"] += 1
                    r0 = b * BLK + q * 128
                    tl = sp.dma(xsb[sl][:], xs_d[r0:r0 + 128, :], s_xs[sl], deps=[xs_free[sl]])
                    bi, bank, bfree = trb.get()
                    bv = bank[:, :].bitcast(BF16)
                    pe.wait(tl, bfree)
                    for j in range(8):
                        ins = pe.e.transpose(out=bv[:, j * 128:(j + 1) * 128], in_=xsb[sl][:, j * 128:(j + 1) * 128],
                                             identity=ident_bf[:])
                    tk = pe.inc(ins)
                    xs_free[sl] = tk
                    act.wait(tk, xT_free[bs])
                    last = act.inc(act.e.activation(out=xT[bs][:, :, q * 128:(q + 1) * 128],
                                                    in_=bv[:, :].rearrange("p (j n) -> p j n", j=8), func=AF.Identity))
                    trb.rel(bi, last)
                xT_ready[b] = last

            def emit_GU(b):
                bs = b % 2
                ws = b % 2
                pe.wait(xT_ready[b], wtok[b])
                for fc in range(4):
                    ssl = cnt["f"] % 2
                    cnt["f"] += 1
                    ig, gbank, gfree = gbk.get()
                    pe.wait(gfree)
                    for j in range(8):
                        ins = pe.e.matmul(gbank[:, :], lhsT=wgs[ws][:, j, fc * 128:(fc + 1) * 128], rhs=xT[bs][:, j, :],
                                          start=(j == 0), stop=(j == 7))
                    tg_ = pe.inc(ins)
                    iu, ubank, ufree = ubk.get()
                    pe.wait(ufree)
                    for j in range(8):
                        ins = pe.e.matmul(ubank[:, :], lhsT=wus[ws][:, j, fc * 128:(fc + 1) * 128], rhs=xT[bs][:, j, :],
                                          start=(j == 0), stop=(j == 7))
                    tu_ = pe.inc(ins)
                    act.wait(tg_, sg_free[ssl])
                    tsg = act.inc(act.e.activation(out=sg[ssl][:], in_=gbank[:, :], func=AF.Silu))
                    gbk.rel(ig, tsg)
                    dve.wait(tsg, tu_)
                    if fc == 0:
                        dve.wait(aT_free[bs])
                    tac = dve.inc(dve.e.tensor_tensor(out=aT[bs][:, fc, :], in0=ubank[:, :], in1=sg[ssl][:], op=ALU.mult))
                    ubk.rel(iu, tac)
                    sg_free[ssl] = tac
                xT_free[bs] = tu_
                return tac

            def emit_DOWN(b, tac):
                bs = b % 2
                ws = b % 2
                ty = None
                for q in range(NQ):
                    sl = cnt["y"] % 4
                    cnt["y"] += 1
                    r0 = b * BLK + q * 128
                    tev = []
                    for half in range(2):
                        iy, ybank, yfree = ybk.get()
                        pe.wait(yfree, tac)
                        for fc in range(4):
                            ins = pe.e.matmul(ybank[:, :], lhsT=aT[bs][:, fc, q * 128:(q + 1) * 128],
                                              rhs=wds[ws][:, fc, half * 512:(half + 1) * 512], start=(fc == 0), stop=(fc == 3))
                        ty = pe.inc(ins)
                        hs_ = slice(half * 512, (half + 1) * 512)
                        dve.wait(ty, ys_free[sl], t_mod)
                        te_ = dve.inc(dve.e.tensor_tensor(out=yv[sl][:, hs_], in0=ybank[:, :], in1=gate2_b[:, hs_], op=ALU.mult))
                        ybk.rel(iy, te_)
                        tev.append(te_)
                    ys_free[sl] = sp.dma(ys_d[r0:r0 + 128, :], yv[sl][:], s_ys[sl], deps=tev)
                aT_free[bs] = ty
                wb_free[ws] = ty

            gather_w(0)
            emit_T(0)
            for b in range(NB):
                if b + 1 < NB:
                    gather_w(b + 1)
                tac = emit_GU(b)
                if b + 1 < NB:
                    emit_T(b + 1)
                emit_DOWN(b, tac)
            dma_toks.extend(v for v in ys_free if v is not None)
            barrier()
        if stop == "D3":
            return nc

        with ExitStack() as ph:
            y1 = [sb(f"y1_{i}", [128, D], F32, ph) for i in range(4)]
            y2 = [sb(f"y2_{i}", [128, D], F32, ph) for i in range(4)]
            xmt = [sb(f"xmt{i}", [128, D], F32, ph) for i in range(4)]
            yo = [sb(f"yo{i}", [128, D], F32, ph) for i in range(4)]
            fgb = sb("fgb", [128, D], F32, ph)
            ssf = sb("ssf", [128, 4], F32, ph)
            s_g = [sem(f"d_g{i}", ph) for i in range(4)]
            s_xmt = [sem(f"d_xmt{i}", ph) for i in range(4)]
            s_yo = [sem(f"d_yo{i}", ph) for i in range(4)]
            t_fg = sp.dma(fgb[:], cst2_d[:, 2 * D:3 * D], sem("d_fg", ph))
            g_free = [None] * 4
            xmt_free = [None] * 4
            yo_free = [None] * 4
            gtok = {}

            def issue_gather(t):
                sl = t % 4
                pool.wait(g_free[sl])
                c = pool.dcnt.get(id(s_g[sl]), 0)
                for k, dst in enumerate([y1[sl], y2[sl]]):
                    c += 16
                    pool.e.indirect_dma_start(out=dst[:, :], out_offset=None, in_=ys_d[:, :],
                                              in_offset=IOA(ap=dest_i[:, k, t:t + 1], axis=0)).then_inc(s_g[sl], 16)
                pool.dcnt[id(s_g[sl])] = c
                gtok[t] = (s_g[sl], c)
                ltok[t] = sp.dma(xmt[sl][:], y_d[t * 128:(t + 1) * 128, :], s_xmt[sl], deps=[xmt_free[sl]])

            ltok = {}
            for t in range(min(3, NT)):
                issue_gather(t)
            for t in range(NT):
                sl = t % 4
                rows = slice(t * 128, (t + 1) * 128)
                if t + 3 < NT:
                    issue_gather(t + 3)
                tg_ = gtok[t]
                tld = ltok[t]
                dve.wait(tg_, tld)
                ta = dve.inc(dve.e.scalar_tensor_tensor(out=xmt[sl][:], in0=y1[sl][:], scalar=w12_all[:, t, 0:1],
                                                        in1=xmt[sl][:], op0=ALU.mult, op1=ALU.add))
                dve.wait(ta)
                t2_ = dve.inc(dve.e.scalar_tensor_tensor(out=xmt[sl][:], in0=y2[sl][:], scalar=w12_all[:, t, 1:2],
                                                         in1=xmt[sl][:], op0=ALU.mult, op1=ALU.add))
                g_free[sl] = t2_
                act.wait(t2_, yo_free[sl])
                t3_ = act.inc(act.e.activation(out=yo[sl][:], in_=xmt[sl][:], func=AF.Square, accum_out=ssf[:, sl:sl + 1]))
                t4_ = rsqrt(ssf[:, sl:sl + 1], ssf[:, sl:sl + 1], 1.0 / D, 1, [t3_])
                dve.wait(t4_, t3_, t_fg)
                t5_ = dve.inc(dve.e.scalar_tensor_tensor(out=yo[sl][:], in0=xmt[sl][:], scalar=ssf[:, sl:sl + 1],
                                                         in1=fgb[:], op0=ALU.mult, op1=ALU.mult))
                xmt_free[sl] = t5_
                yo_free[sl] = sp.dma(y_d[rows, :], yo[sl][:], s_yo[sl], deps=[t5_])
            dma_toks.extend(v for v in yo_free if v is not None)
            barrier()
        return nc


def make_inputs(inputs, b, S):
    f32 = np.float32
    g = lambda k: np.asarray(inputs[k], dtype=f32)

    def col(v, n):
        return np.ascontiguousarray(v.reshape(n, 128).T)

    def rep(v):
        return np.ascontiguousarray(np.broadcast_to(v[None, :], (128, v.shape[0])))

    cst = np.zeros((128, NCST), f32)

    def put(name, arr):
        o, w = _off[name]
        assert arr.shape == (128, w), (name, arr.shape)
        cst[:, o:o + w] = arr

    b_ada = g('b_ada')[0]
    put("c", col(g('c')[b], 8))
    put("bada", np.concatenate([col(b_ada[i * D:(i + 1) * D], 8) for i in (0, 1, 3, 4)], 1))
    put("g1", col(g('norm1_g')[0], 8))
    put("g2", col(g('norm2_g')[0], 8))
    put("qg", col(g('q_a_norm_g')[0], 2))
    put("kvg", col(g('kv_a_norm_g')[0], 2))
    put("lq1", rep(g('lambda_q1')[0]))
    put("lk1", rep(g('lambda_k1')[0]))
    put("lq2", rep(g('lambda_q2')[0]))
    put("lk2", rep(g('lambda_k2')[0]))
    put("subln", rep(g('subln_g')[0]))
    put("brt", rep(np.concatenate([g('b_router_group')[0], g('b_router_expert')[0]])))
    put("ident", np.eye(128, dtype=f32))
    put("utri", np.triu(np.ones((128, 128), f32), 1))
    put("pcol", np.arange(128, dtype=f32)[:, None])
    cst2 = np.ascontiguousarray(np.concatenate([rep(b_ada[2 * D:3 * D]), rep(b_ada[5 * D:6 * D]), rep(g('final_norm_g'))], 1))

    w_in = g('w_in')[0]
    perm64 = np.concatenate([np.arange(32, 64), np.arange(0, 32)])

    def rot_cols(w, nblk):
        idx = np.concatenate([perm64 + 64 * i for i in range(nblk)])
        return w[:, idx]

    w_in_ext = np.ascontiguousarray(np.concatenate(
        [w_in, rot_cols(w_in[:, 512:576], 1), rot_cols(w_in[:, 576:1088], 8), rot_cols(w_in[:, 1088:1600], 8)], 1))
    wq = g('w_q_up')[0].reshape(256, 4, 192)
    wq_n = wq[:, :, :128].reshape(256, 512)
    wq_p = wq[:, :, 128:].reshape(256, 256)
    wq_ext = np.ascontiguousarray(np.concatenate([wq_n, wq_p, rot_cols(wq_p, 4)], 1))
    wkv = g('w_kv_up')[0].reshape(256, 4, 256)
    wkv_ext = np.ascontiguousarray(np.concatenate([wkv[:, :, :128].reshape(256, 512), wkv[:, :, 128:].reshape(256, 512)], 1))
    inv = (10000.0 ** (-np.arange(0, 64, 2, dtype=f32) / 64)).astype(f32)
    ang = np.arange(S, dtype=f32)[None, :] * inv[:, None]
    cos = np.cos(ang).astype(f32)
    sin = np.sin(ang).astype(f32)
    cos64 = np.concatenate([cos, cos], 0)
    sin64 = np.concatenate([-sin, sin], 0)
    cosT = np.ascontiguousarray(np.concatenate([cos64, cos64], 0))
    sinT = np.ascontiguousarray(np.concatenate([sin64, sin64], 0))
    return {
        "x": np.ascontiguousarray(g('x')[b, :S]),
        "cst": cst,
        "cst2": cst2,
        "w_ada": g('w_ada')[0],
        "w_in_ext": w_in_ext,
        "wq_ext": wq_ext,
        "wkv_ext": wkv_ext,
        "cosT": cosT,
        "sinT": sinT,
        "w_out": g('w_out')[0],
        "w_router": np.ascontiguousarray(np.concatenate([g('w_router_group')[0], g('w_router_expert')[0]], 1)),
        "w_eg": g('w_expert_gate')[0],
        "w_eu": g('w_expert_up')[0],
        "w_ed": g('w_expert_down')[0],
        "bthr": np.ascontiguousarray(np.broadcast_to(
            (512.0 * np.arange((2 * S + NE * 512) // 512, dtype=f32))[None, :, None],
            (128, (2 * S + NE * 512) // 512, 32)).reshape(128, -1)),
    }


def kernel(**inputs):
    S = inputs['x'].shape[1]
    B = inputs['x'].shape[0]
    nc = build(S)
    in_maps = [make_inputs(inputs, b, S) for b in range(B)]
    res = run_bass_kernel_spmd(nc, in_maps, core_ids=list(range(B)))
    return np.stack([np.asarray(r["y"], dtype=np.float32) for r in res.results], 0)
```

```python
import math
from contextlib import ExitStack

import numpy as np
import concourse.bass as bass
import concourse.mybir as mybir
from concourse.bass_utils import run_bass_kernel_spmd

F32 = mybir.dt.float32
BF16 = mybir.dt.bfloat16
AF = mybir.ActivationFunctionType
ALU = mybir.AluOpType
AX = mybir.AxisListType

D = 1024
EPS = 1e-6
LAMBDA_INIT = 0.8 - 0.6 * math.exp(0.0)
NE = 32
DE = 512

_off = {}
_n = 0
for _name, _w in [("c", 8), ("bada", 32), ("g1", 8), ("g2", 8), ("qg", 2), ("kvg", 2),
                  ("lq1", 64), ("lk1", 64), ("lq2", 64), ("lk2", 64), ("subln", 128),
                  ("brt", 36), ("ident", 128), ("utri", 128), ("pcol", 1)]:
    _off[_name] = (_n, _w)
    _n += _w
NCST = _n


class Eng:
    def __init__(self, e, sem):
        self.e = e
        self.sem = sem
        self.n = 0
        self.seen = {}
        self.dcnt = {}

    def wait(self, *toks):
        for t in toks:
            if t is None:
                continue
            if isinstance(t, list):
                self.wait(*t)
                continue
            sem, v = t
            k = id(sem)
            if self.seen.get(k, 0) >= v:
                continue
            self.e.wait_ge(sem, v)
            self.seen[k] = v

    def inc(self, inst):
        inst.then_inc(self.sem, 1)
        self.n += 1
        return (self.sem, self.n)

    def dma(self, out, in_, sem, deps=(), **kw):
        self.wait(*deps)
        c = self.dcnt.get(id(sem), 0) + 16
        self.dcnt[id(sem)] = c
        self.e.dma_start(out=out, in_=in_, **kw).then_inc(sem, 16)
        return (sem, c)


class Banks:
    def __init__(self, banks):
        self.banks = banks
        self.free = [None] * len(banks)
        self.i = 0

    def get(self):
        i = self.i
        self.i = (i + 1) % len(self.banks)
        return i, self.banks[i], self.free[i]

    def rel(self, i, tok):
        self.free[i] = tok


def build(S, stop=None, dbg=False):
    assert S % 512 == 0
    NCH = S // 512
    NT = S // 128
    BLK = 512
    ROWS = 2 * S + NE * BLK
    NB = ROWS // BLK
    nc = bass.Bass("TRN2", target_bir_lowering=False)
    skind = "ExternalOutput" if dbg else "Internal"

    def dram_in(name, shape, dt=F32):
        return nc.dram_tensor(name, shape, dt, kind="ExternalInput").ap()

    x_d = dram_in("x", [S, D])
    cst_d = dram_in("cst", [128, NCST])
    cst2_d = dram_in("cst2", [128, 3 * D])
    wada_d = dram_in("w_ada", [D, 6 * D])
    win_d = dram_in("w_in_ext", [D, 3200])
    wq_d = dram_in("wq_ext", [256, 1024])
    wkv_d = dram_in("wkv_ext", [256, 1024])
    cos_d = dram_in("cosT", [128, S])
    sin_d = dram_in("sinT", [128, S])
    wout_d = dram_in("w_out", [D, D])
    wr_d = dram_in("w_router", [D, 36])
    weg_d = dram_in("w_eg", [NE, D, DE])
    weu_d = dram_in("w_eu", [NE, D, DE])
    wed_d = dram_in("w_ed", [NE, DE, D])
    y_d = nc.dram_tensor("y", [S, D], F32, kind="ExternalOutput").ap()

    def scr(name, shape, dt=BF16):
        return nc.dram_tensor(name, shape, dt, kind=skind).ap()

    qnT_d = scr("s_qnT", [4, 128, S])
    qpT_d = scr("s_qpT", [4, 64, S])
    knT_d = scr("s_knT", [4, 128, S])
    kpT_d = scr("s_kpT", [64, S])
    dqT_d = scr("s_dqT", [4, 128, S])
    dkT_d = scr("s_dkT", [4, 128, S])
    vm_d = scr("s_vm", [4, S, 129])
    vd_d = scr("s_vd", [4, S, 129])
    ao_d = scr("s_ao", [S, D])
    h2tok_d = scr("s_h2tok", [S, D])
    xs_d = scr("s_xs", [ROWS, D])
    ys_d = scr("s_ys", [ROWS, D], F32)
    wgtab_d = scr("s_wgtab", [NE * 128, 8 * DE])
    wutab_d = scr("s_wutab", [NE * 128, 8 * DE])
    wdtab_d = scr("s_wdtab", [NE * 128, 4 * D])
    bthr_d = dram_in("bthr", [128, NB * 32])
    if dbg:
        dbg_d = nc.dram_tensor("dbg", [128, 4096], F32, kind="ExternalOutput").ap()

    with ExitStack() as top:
        def sb(name, shape, dt, stack=top):
            return stack.enter_context(nc.sbuf_tensor("sb_" + name, shape, dt))

        def sem(name, stack=top):
            return stack.enter_context(nc.semaphore(name))

        block = top.enter_context(nc.Block())
        pe = Eng(nc.tensor, sem("s_pe"))
        act = Eng(nc.scalar, sem("s_act"))
        dve = Eng(nc.vector, sem("s_dve"))
        pool = Eng(nc.gpsimd, sem("s_pool"))
        sp = Eng(nc.sync, sem("s_sp"))
        engs = [pe, act, dve, pool, sp]
        dma_toks = []
        s_tab = sem("d_tab")
        tab_tok = [None]

        pbanks = [top.enter_context(nc.psum_tensor(f"pb{i}", [128, 512], F32)) for i in range(8)]

        def barrier():
            toks = [(e.sem, e.n) for e in engs if e.n > 0] + list(dma_toks)
            for e in engs:
                e.wait(*toks)
            dma_toks.clear()

        cst = sb("cst", [128, NCST], F32)
        s_cst = sem("d_cst")
        t_cst = sp.dma(cst[:], cst_d[:, :], s_cst)

        def C(name, a=None, b=None):
            o, w = _off[name]
            a = 0 if a is None else a
            b = w if b is None else b
            return cst[:, o + a:o + b]

        ident_bf = sb("ident_bf", [128, 128], BF16)
        ones_bf = sb("ones_bf", [128, 128], BF16)
        ones_f = sb("ones_f", [128, 128], F32)
        negh = sb("negh", [128, 8], F32)
        epsc = sb("epsc", [128, 1], F32)
        modc = sb("modc", [128, 32], F32)
        a1c = sb("a1c", [128, 8], F32)
        a2c = sb("a2c", [128, 8], F32)
        gate1_b = sb("gate1_b", [128, D], F32)
        gate2_b = sb("gate2_b", [128, D], F32)
        neglam = sb("neglam", [128, 1], F32)
        subln_b = sb("subln_b", [128, 128], F32)
        M1_all = sb("M1_all", [128, NT, 32], F32)
        M2_all = sb("M2_all", [128, NT, 32], F32)
        w12_all = sb("w12_all", [128, NT, 2], F32)

        dve.wait(t_cst)
        dve.e.tensor_copy(out=ident_bf[:], in_=C("ident"))
        dve.e.memset(ones_bf[:], 1.0)
        dve.e.memset(ones_f[:], 1.0)
        dve.e.memset(epsc[:], EPS)
        t_c0 = dve.inc(dve.e.memset(negh[:], -0.5))

        def rsqrt(out, in_, scale, n, deps):
            pool.wait(*deps)
            t = pool.inc(pool.e.tensor_scalar(out=out, in0=in_, scalar1=float(scale), scalar2=float(EPS),
                                              op0=ALU.mult, op1=ALU.add))
            pool.wait(t, t_c0)
            return pool.inc(pool.e.tensor_tensor(out=out, in0=out, in1=negh[:, 0:n], op=ALU.pow))

        wA_stack = ExitStack()
        winb = sb("winb", [128, 8, 3200], BF16, wA_stack)
        wqb = sb("wqb", [128, 2, 1024], BF16, wA_stack)
        wkvb = sb("wkvb", [128, 2, 1024], BF16, wA_stack)
        s_w = sem("d_wA")
        tw = None
        for i in range(4):
            tw = pool.dma(winb[:, :, i * 800:(i + 1) * 800],
                          win_d[:, i * 800:(i + 1) * 800].rearrange("(j p) n -> p j n", p=128), s_w)
        tw = pool.dma(wqb[:], wq_d[:, :].rearrange("(j p) n -> p j n", p=128), s_w)
        t_wA = pool.dma(wkvb[:], wkv_d[:, :].rearrange("(j p) n -> p j n", p=128), s_w)

        with ExitStack() as ph:
            sT2 = sb("sT2", [128, 8, 2], F32, ph)
            sTb = sb("sTb", [128, 8, 128], F32, ph)
            wa = [sb(f"wa{i}", [128, 8, 1024], F32, ph) for i in range(2)]
            s_wa = [sem(f"d_wa{i}", ph) for i in range(2)]
            lt = sb("lt", [128, 64], F32, ph)
            lsum = sb("lsum", [128, 2], F32, ph)
            bgt = sb("bgt", [128, 2 * D], F32, ph)
            t_bg = sp.dma(bgt[:], cst2_d[:, 0:2 * D], sem("d_bg", ph))

            dve.e.memset(sT2[:], 0.0)
            t = dve.inc(dve.e.memset(lsum[:], 0.0))
            act.wait(t_cst, t)
            t_s = act.inc(act.e.activation(out=sT2[:, :, 0], in_=C("c"), func=AF.Silu))
            act.wait(t_s, t_c0)
            for j in range(8):
                t_sb = act.inc(act.e.activation(out=sTb[:, j, :], in_=ones_f[:], func=AF.Identity,
                                                scale=sT2[:, j, 0:1]))
            for i, (a, b) in enumerate([("lq1", "lk1"), ("lq2", "lk2")]):
                dve.wait(t)
                t = dve.inc(dve.e.tensor_tensor(out=lt[:], in0=C(a), in1=C(b), op=ALU.mult))
                dve.wait(t)
                t = dve.inc(dve.e.reduce_sum(out=lsum[:, i:i + 1], in_=lt[:], axis=AX.X))
            act.wait(t)
            t = act.inc(act.e.activation(out=lsum[:], in_=lsum[:], func=AF.Exp))
            dve.wait(t)
            t = dve.inc(dve.e.tensor_tensor(out=neglam[:], in0=lsum[:, 1:2], in1=lsum[:, 0:1], op=ALU.subtract))
            dve.wait(t)
            t = dve.inc(dve.e.tensor_scalar_add(neglam[:], neglam[:], -LAMBDA_INIT))
            dve.wait(t)
            t_misc = dve.inc(dve.e.tensor_scalar(out=subln_b[:], in0=C("subln"), scalar1=float(1.0 - LAMBDA_INIT),
                                                 scalar2=None, op0=ALU.mult))

            colidx = {0: 0, 1: 1, 3: 2, 4: 3}
            wa_free = [None, None]
            pA = pbanks[0]
            pG = [pbanks[1], pbanks[2], pbanks[3], pbanks[4]]
            t_last_gate = {}
            for g in range(6):
                sl = g % 2
                t_w = sp.dma(wa[sl][:], wada_d[:, g * 1024:(g + 1) * 1024].rearrange("(j p) n -> p j n", p=128),
                             s_wa[sl], deps=[wa_free[sl]])
                pe.wait(t_w, t_s, t_sb)
                if g in colidx:
                    ci = colidx[g]
                    for m in range(8):
                        col = (ci * 8 + m) * 2
                        for j in range(8):
                            ins = pe.e.matmul(pA[:, col:col + 2], lhsT=wa[sl][:, j, m * 128:(m + 1) * 128],
                                              rhs=sT2[:, j, :], start=(j == 0), stop=(j == 7))
                    wa_free[sl] = pe.inc(ins)
                else:
                    gi = 0 if g == 2 else 1
                    for half in range(2):
                        for j in range(8):
                            ins = pe.e.matmul(pG[gi * 2 + half][:, :], lhsT=sTb[:, j, :],
                                              rhs=wa[sl][:, j, half * 512:(half + 1) * 512],
                                              start=(j == 0), stop=(j == 7))
                    wa_free[sl] = pe.inc(ins)
                    t_last_gate[gi] = wa_free[sl]
            t_pe_mod = (pe.sem, pe.n)
            dve.wait(t_pe_mod, t_cst, t_bg)
            pAv = pA[:, 0:64].rearrange("p (c two) -> p c two", two=2)[:, :, 0]
            t = dve.inc(dve.e.tensor_tensor(out=modc[:], in0=pAv, in1=C("bada"), op=ALU.add))
            for gi, gb in enumerate([gate1_b, gate2_b]):
                for half in range(2):
                    t2 = dve.inc(dve.e.tensor_tensor(out=gb[:, half * 512:(half + 1) * 512],
                                                     in0=pG[gi * 2 + half][:, :],
                                                     in1=bgt[:, gi * D + half * 512:gi * D + (half + 1) * 512], op=ALU.add))
            dve.wait(t)
            dve.e.scalar_tensor_tensor(out=a1c[:], in0=modc[:, 8:16], scalar=1.0, in1=C("g1"),
                                       op0=ALU.add, op1=ALU.mult)
            t_mod = dve.inc(dve.e.scalar_tensor_tensor(out=a2c[:], in0=modc[:, 24:32], scalar=1.0, in1=C("g2"),
                                                       op0=ALU.add, op1=ALU.mult))
            if dbg:
                dve.wait(t_mod, t2, t_misc)
                dve.e.tensor_copy(out=cst[:, 0:32], in_=modc[:])
                t = dve.inc(dve.e.tensor_copy(out=cst[:, 32:33], in_=neglam[:]))
                t = sp.dma(dbg_d[:, 0:64], cst[:, 0:64], s_cst, deps=[t])
                dma_toks.append(t)
                t = sp.dma(dbg_d[:, 1024:2048], gate1_b[:], s_cst, deps=[t])
                dma_toks.append(t)
                t = sp.dma(dbg_d[:, 2048:3072], gate2_b[:], s_cst, deps=[t])
                dma_toks.append(t)
            barrier()
        sh1c = modc[:, 0:8]
        sh2c = modc[:, 16:24]

        if stop == "pro":
            return nc

        with ExitStack() as ph:

            xt = [sb(f"xt{i}", [128, 4, D], F32, ph) for i in range(2)]
            cs = [sb(f"cs{i}", [128, 2, 512], F32, ph) for i in range(2)]
            xn = [sb("xn0", [128, 4, D], BF16, ph)] * 2
            hT = [sb(f"hT{i}", [128, 8, 512], BF16, ph) for i in range(2)]
            ss = [sb(f"ss{i}", [128, 4], F32, ph) for i in range(2)]
            rstd = [sb(f"rstd{i}", [128, 4], F32, ph) for i in range(2)]
            cqg = sb("cqg", [128, 2, 512], BF16, ph)
            ckvg = sb("ckvg", [128, 2, 512], BF16, ph)
            sqq = sb("sqq", [128, 2, 512], BF16, ph)
            sqkv = sb("sqkv", [128, 2, 512], BF16, ph)
            rq_b = sb("rq_b", [128, 512], F32, ph)
            rkv_b = sb("rkv_b", [128, 512], F32, ph)
            rkv_t = sb("rkv_t", [128, 4], F32, ph)
            t1 = [sb(f"t1_{i}", [128, 512], F32, ph) for i in range(2)]
            t2 = [sb(f"t2_{i}", [128, 512], F32, ph) for i in range(2)]
            t3 = [sb("t3_0", [128, 512], F32, ph)] * 2
            qn_st = [sb("qn_st0", [128, 4, 512], BF16, ph)] * 2
            qp_st = [sb("qp_st0", [64, 4, 512], BF16, ph)] * 2
            kn_st = [sb("kn_st0", [128, 4, 512], BF16, ph)] * 2
            kp_st = [sb("kp_st0", [64, 512], BF16, ph)] * 2
            dq_st = [sb("dq_st0", [128, 4, 512], BF16, ph)] * 2
            dk_st = [sb("dk_st0", [128, 4, 512], BF16, ph)] * 2
            vm_st = [sb("vm_st0", [128, 4, 4, 129], BF16, ph)] * 2
            vd_st = [sb("vd_st0", [128, 4, 4, 129], BF16, ph)] * 2
            s_x = [sem(f"d_x{i}", ph) for i in range(2)]
            s_cs = [sem(f"d_cs{i}", ph) for i in range(2)]
            s_stg = {k: sem("d_st_" + k, ph) for k in ["kp", "dq", "dk", "vd", "qn", "qp", "kn", "vm"]}
            stg_free = {k: None for k in s_stg}

            dve.e.memset(vm_st[0][:], 1.0)
            t_ms = dve.inc(dve.e.memset(vd_st[0][:], 1.0))

            trb = Banks([pbanks[0], pbanks[1]])
            fb = Banks([pbanks[2], pbanks[3], pbanks[4], pbanks[5], pbanks[6], pbanks[7]])
            xt_free = [None, None]
            cs_free = [None, None]
            xn_free = [None, None]
            hT_free = [None, None]
            st_free = [None, None]
            tmp_free = [None, None]
            tmpi = [0]
            t3_free = [None]
            cq_free = None

            stA = {}
            stA_a = {}

            def frontA_a(c):
                sl = c % 2
                cols = slice(c * 512, (c + 1) * 512)
                tx = sp.dma(xt[sl][:], x_d[cols, :].rearrange("(t p) n -> p t n", p=128), s_x[sl], deps=[xt_free[sl]])
                sp.dma(cs[sl][:, 0, :], cos_d[:, cols], s_cs[sl], deps=[cs_free[sl]])
                tcs = sp.dma(cs[sl][:, 1, :], sin_d[:, cols], s_cs[sl])
                cosb = cs[sl][:, 0, :]
                sinb = cs[sl][:, 1, :]
                act.wait(tx, xn_free[0], xn_free[1])
                for t in range(4):
                    tss = act.inc(act.e.activation(out=xn[sl][:, t, :], in_=xt[sl][:, t, :], func=AF.Square,
                                                   accum_out=ss[sl][:, t:t + 1]))
                tr = rsqrt(rstd[sl][:], ss[sl][:], 1.0 / D, 4, [tss])
                act.wait(tr, xn_free[sl])
                txn = []
                for t in range(4):
                    txn.append(act.inc(act.e.activation(out=xn[sl][:, t, :], in_=xt[sl][:, t, :], func=AF.Identity,
                                                        scale=rstd[sl][:, t:t + 1])))
                xt_free[sl] = txn[3]
                stA_a[c] = (tcs, txn)

            def frontA_b(c):
                sl = c % 2
                tcs, txn = stA_a.pop(c)
                dve.wait(hT_free[sl], t_mod)
                for t in range(4):
                    bi, bank, bfree = trb.get()
                    bv = bank[:, :].bitcast(BF16)
                    pe.wait(bfree, txn[t])
                    for j in range(8):
                        ins = pe.e.transpose(out=bv[:, j * 128:(j + 1) * 128], in_=xn[sl][:, t, j * 128:(j + 1) * 128],
                                             identity=ident_bf[:])
                    ttr = pe.inc(ins)
                    dve.wait(ttr)
                    for j in range(8):
                        ins = dve.e.tensor_scalar(out=hT[sl][:, j, t * 128:(t + 1) * 128],
                                                  in0=bv[:, j * 128:(j + 1) * 128],
                                                  scalar1=a1c[:, j:j + 1], scalar2=sh1c[:, j:j + 1],
                                                  op0=ALU.mult, op1=ALU.add)
                    thT = dve.inc(ins)
                    trb.rel(bi, thT)
                xn_free[sl] = ttr
                stA[c] = (tcs, thT)

            frontA_a(0)
            frontA_b(0)
            for c in range(NCH):
                sl = c % 2
                cols = slice(c * 512, (c + 1) * 512)
                cosb = cs[sl][:, 0, :]
                sinb = cs[sl][:, 1, :]
                tcs, thT = stA.pop(c)
                def fm(w, col0, M, src, nk, deps):
                    bi, bank, bfree = fb.get()
                    pe.wait(bfree, *deps)
                    for k in range(nk):
                        ins = pe.e.matmul(bank[0:M, :], lhsT=w[:, k, col0:col0 + M], rhs=src[:, k, :],
                                          start=(k == 0), stop=(k == nk - 1))
                    return bi, bank, pe.inc(ins)

                hs = hT[sl]
                pe.wait(t_wA, tw)
                if stop == "A_w":
                    barrier()
                    return nc
                dve.wait(cq_free)
                act.wait(cq_free)
                for (col0, gname, dst, sq) in [(0, "qg", cqg, sqq), (256, "kvg", ckvg, sqkv)]:
                    for m in range(2):
                        bi, bank, tk = fm(winb, col0 + m * 128, 128, hs, 8, [thT])
                        if stop == "A_mm":
                            barrier()
                            return nc
                        dve.wait(tk)
                        ta = dve.inc(dve.e.tensor_scalar(out=dst[:, m, :], in0=bank[:, :], scalar1=C(gname, m, m + 1),
                                                         scalar2=None, op0=ALU.mult))
                        if stop == "A_dve":
                            barrier()
                            return nc
                        act.wait(tk, ta)
                        tb = act.inc(act.e.activation(out=sq[:, m, :], in_=bank[:, :], func=AF.Square))
                        if stop == "A_act":
                            barrier()
                            return nc
                        fb.rel(bi, [ta, tb])
                t_cqg = ta
                t_sq = tb
                if stop == "A_cq":
                    barrier()
                    return nc
                if c + 1 < NCH:
                    frontA_a(c + 1)
                sfree = [v for v in stg_free.values() if v is not None]
                dve.wait(t_ms, *sfree)
                act.wait(t_ms, *sfree)
                pool.wait(*sfree)

                def rope_pair(wa_col, wb_col, M, w, src, nk, deps, out_ap, extra=None):
                    ia, banka, tka = fm(w, wa_col, M, src, nk, deps)
                    ib, bankb, tkb = fm(w, wb_col, M, src, nk, deps)
                    ti = tmpi[0] % 2
                    tmpi[0] += 1
                    dve.wait(tka, tcs, tmp_free[ti])
                    tA = dve.inc(dve.e.tensor_tensor(out=t1[ti][0:M, :], in0=banka[0:M, :], in1=cosb[0:M, :], op=ALU.mult))
                    dve.wait(tkb)
                    tB = dve.inc(dve.e.tensor_tensor(out=t2[ti][0:M, :], in0=bankb[0:M, :], in1=sinb[0:M, :], op=ALU.mult))
                    fb.rel(ia, tA)
                    fb.rel(ib, tB)
                    pool.wait(tA, tB)
                    if extra is None:
                        tC = pool.inc(pool.e.tensor_tensor(out=out_ap, in0=t1[ti][0:M, :], in1=t2[ti][0:M, :], op=ALU.add))
                    else:
                        eap, etok = extra
                        pool.wait(t3_free[0])
                        tC = pool.inc(pool.e.tensor_tensor(out=t3[ti][0:M, :], in0=t1[ti][0:M, :], in1=t2[ti][0:M, :], op=ALU.add))
                        pool.wait(tC, etok)
                        tC = pool.inc(pool.e.tensor_tensor(out=out_ap, in0=t3[ti][0:M, :], in1=eap, op=ALU.mult))
                        t3_free[0] = tC
                    tmp_free[ti] = tC
                    return tC

                t_kp = rope_pair(512, 2112, 64, winb, hs, 8, [thT], kp_st[sl][:, :])
                for h in range(4):
                    t_dq = rope_pair(576 + h * 128, 2176 + h * 128, 128, winb, hs, 8, [thT], dq_st[sl][:, h, :])
                tstat = {}
                for (sq, dst) in [(sqq, rq_b), (sqkv, rkv_b)]:
                    bi, bank, bfree = fb.get()
                    pe.wait(bfree, t_sq)
                    for m in range(2):
                        ins = pe.e.matmul(bank[:, :], lhsT=ones_bf[:], rhs=sq[:, m, :], start=(m == 0), stop=(m == 1))
                    tk = pe.inc(ins)
                    act.wait(tk)
                    tcp = act.inc(act.e.activation(out=dst[:], in_=bank[:, :], func=AF.Sqrt, scale=1.0 / 256,
                                                   bias=epsc[:, 0:1]))
                    fb.rel(bi, tcp)
                    dve.wait(tcp)
                    tstat[id(dst)] = dve.inc(dve.e.reciprocal(out=dst[:], in_=dst[:]))
                t_rq = tstat[id(rq_b)]
                t_rkv = tstat[id(rkv_b)]
                bi, bank, bfree = fb.get()
                pe.wait(bfree)
                for t in range(4):
                    for m in range(2):
                        ins = pe.e.matmul(bank[:, 2 * t:2 * t + 2], lhsT=sqkv[:, m, t * 128:(t + 1) * 128],
                                          rhs=ones_bf[:, 0:2], start=(m == 0), stop=(m == 1))
                tk = pe.inc(ins)
                dve.wait(tk)
                tcp = dve.inc(dve.e.tensor_copy(out=rkv_t[:], in_=bank[:, 0:8].rearrange("p (t two) -> p t two", two=2)[:, :, 0]))
                fb.rel(bi, tcp)
                t_rkvt = rsqrt(rkv_t[:], rkv_t[:], 1.0 / 256, 4, [tcp])
                for h in range(4):
                    t_dk = rope_pair(1088 + h * 128, 2688 + h * 128, 128, winb, hs, 8, [thT], dk_st[sl][:, h, :])
                if stop == "A_rope":
                    barrier()
                    return nc
                for t in range(4):
                    bi, bank, bfree = fb.get()
                    pe.wait(bfree)
                    for k in range(8):
                        ins = pe.e.matmul(bank[:, :], lhsT=hs[:, k, t * 128:(t + 1) * 128], rhs=winb[:, k, 1600:2112],
                                          start=(k == 0), stop=(k == 7))
                    tk = pe.inc(ins)
                    act.wait(tk)
                    t_vd = act.inc(act.e.activation(out=vd_st[sl][:, t, :, 0:128],
                                                    in_=bank[:, :].rearrange("p (h d) -> p h d", h=4),
                                                    func=AF.Identity))
                    fb.rel(bi, t_vd)
                hT_free[sl] = tk
                if stop == "A_dv":
                    barrier()
                    return nc
                if c + 1 < NCH:
                    frontA_b(c + 1)
                if stop == "A_stat":
                    barrier()
                    return nc
                for h in range(4):
                    bi, bank, tk = fm(wqb, h * 128, 128, cqg, 2, [t_cqg])
                    dve.wait(tk, t_rq)
                    t_qn = dve.inc(dve.e.tensor_tensor(out=qn_st[sl][:, h, :], in0=bank[:, :], in1=rq_b[:], op=ALU.mult))
                    fb.rel(bi, t_qn)
                for h in range(4):
                    t_qp = rope_pair(512 + h * 64, 768 + h * 64, 64, wqb, cqg, 2, [t_cqg], qp_st[sl][:, h, :],
                                     extra=(rq_b[0:64, :], t_rq))
                if stop == "A_q":
                    barrier()
                    return nc
                for h in range(4):
                    bi, bank, tk = fm(wkvb, h * 128, 128, ckvg, 2, [t_cqg])
                    dve.wait(tk, t_rkv)
                    t_kn = dve.inc(dve.e.tensor_tensor(out=kn_st[sl][:, h, :], in0=bank[:, :], in1=rkv_b[:], op=ALU.mult))
                    fb.rel(bi, t_kn)
                for t in range(4):
                    bi, bank, bfree = fb.get()
                    pe.wait(bfree)
                    for k in range(2):
                        ins = pe.e.matmul(bank[:, :], lhsT=ckvg[:, k, t * 128:(t + 1) * 128], rhs=wkvb[:, k, 512:1024],
                                          start=(k == 0), stop=(k == 1))
                    tk = pe.inc(ins)
                    act.wait(tk, t_rkvt)
                    t_vm = act.inc(act.e.activation(out=vm_st[sl][:, t, :, 0:128],
                                                    in_=bank[:, :].rearrange("p (h d) -> p h d", h=4),
                                                    func=AF.Identity, scale=rkv_t[:, t:t + 1]))
                    fb.rel(bi, t_vm)
                cq_free = [tk, t_qp, t_kn, t_vm]
                cs_free[sl] = [t_qp, t_dk]
                if stop == "A_kv":
                    barrier()
                    return nc
                def store(key, out, in_, dep):
                    stg_free[key] = sp.dma(out, in_, s_stg[key], deps=[dep])
                store("kp", kpT_d[:, cols], kp_st[sl][:, :], t_kp)
                store("dq", dqT_d[:, :, cols].rearrange("h p n -> p h n"), dq_st[sl][:], t_dq)
                store("dk", dkT_d[:, :, cols].rearrange("h p n -> p h n"), dk_st[sl][:], t_dk)
                for t in range(4):
                    rows = slice(c * 512 + t * 128, c * 512 + (t + 1) * 128)
                    store("vd", vd_d[:, rows, :].rearrange("h p d -> p h d"), vd_st[sl][:, t, :, :], t_vd)
                store("qn", qnT_d[:, :, cols].rearrange("h p n -> p h n"), qn_st[sl][:], t_qn)
                store("qp", qpT_d[:, :, cols].rearrange("h p n -> p h n"), qp_st[sl][:], t_qp)
                store("kn", knT_d[:, :, cols].rearrange("h p n -> p h n"), kn_st[sl][:], t_kn)
                for t in range(4):
                    rows = slice(c * 512 + t * 128, c * 512 + (t + 1) * 128)
                    store("vm", vm_d[:, rows, :].rearrange("h p d -> p h d"), vm_st[sl][:, t, :, :], t_vm)
            dma_toks.extend(v for v in stg_free.values() if v is not None)
            barrier()
        wA_stack.close()
        if stop == "A":
            return nc
        with ExitStack() as ph:
            NKT = S // 128
            ksb = [sb(f"k_sb{i}", [128, S], BF16, ph) for i in range(2)]
            kpsb = sb("kp_sb", [128, S], BF16, ph)
            vsb = [sb(f"v_sb{i}", [128, NKT, 129], BF16, ph) for i in range(2)]
            qsb = [sb(f"q_sb{i}", [128, 512], BF16, ph) for i in range(2)]
            qpsb = [sb(f"qp_sb{i}", [128, 512], BF16, ph) for i in range(2)]
            qA = [sb(f"qA{i}", [128, 512], BF16, ph) for i in range(2)]
            qB = [sb(f"qB{i}", [128, 512], BF16, ph) for i in range(2)]
            pT = [sb(f"pT{i}", [128, 512], BF16, ph) for i in range(3)]
            o1 = sb("o1", [128, 4, 128], F32, ph)
            o2 = sb("o2", [128, 4, 128], F32, ph)
            dd = sb("dd", [128, 4, 128], F32, ph)
            sqt = sb("sqt", [128, 4, 128], F32, ph)
            ssq = sb("ssq", [128, 4], F32, ph)
            rinv = sb("rinv", [128, 4], F32, ph)
            accs = sb("accs", [128, 4, 129], F32, ph)
            ao_st = [sb(f"ao_st{i}", [128, 4, 128], BF16, ph) for i in range(2)]
            s_k = [sem(f"d_k{i}", ph) for i in range(2)]
            s_v = [sem(f"d_v{i}", ph) for i in range(2)]
            s_kp = sem("d_kp", ph)
            s_q = [sem(f"d_q{i}", ph) for i in range(2)]
            s_ao = [sem(f"d_ao{i}", ph) for i in range(2)]
            sbk = pbanks[0:4]
            accb = pbanks[4:8]
            dve.e.memset(kpsb[64:128, :], 0.0)
            for i in range(2):
                dve.e.memset(qpsb[i][64:128, :], 0.0)
                dve.e.memset(qA[i][64:128, :], 0.0)
                t_z = dve.inc(dve.e.memset(qB[i][0:64, :], 0.0))
            pe.wait(t_z)
            t_kp = pool.dma(kpsb[0:64, :], kpT_d[:, :], s_kp)
            heads = [("mla", h) for h in range(4)] + [("dif", h) for h in range(4)]
            kv_free = [None, None]
            q_free = [None, None]
            ao_free = [None, None]
            st = {"acc_free": None, "epi": None, "aoi": 0, "qi": 0}
            kv_tok = {}

            def load_kv(hi):
                kind, h = heads[hi]
                sl = hi % 2
                ksrc = knT_d if kind == "mla" else dkT_d
                vsrc = vm_d if kind == "mla" else vd_d
                tk = pool.dma(ksb[sl][:], ksrc[h, :, :], s_k[sl], deps=[kv_free[sl]])
                tv = None
                for g0 in range(0, NKT, 16):
                    g1 = min(NKT, g0 + 16)
                    tv = pool.dma(vsb[sl][:, g0:g1, :],
                                  vsrc[h, g0 * 128:g1 * 128, :].rearrange("(t p) d -> p t d", p=128), s_v[sl])
                kv_tok[hi] = (tk, tv)

            def load_q(hi, qc):
                next_tab()
                next_zero()
                kind, h = heads[hi]
                qs = st["qi"] % 2
                st["qi"] += 1
                cols = slice(qc * 512, (qc + 1) * 512)
                if kind == "mla":
                    pool.dma(qsb[qs][:], qnT_d[h, :, cols], s_q[qs], deps=[q_free[qs]])
                    t = pool.dma(qpsb[qs][0:64, :], qpT_d[h, :, cols], s_q[qs])
                else:
                    pool.dma(qA[qs][0:64, :], dqT_d[h, 0:64, cols], s_q[qs], deps=[q_free[qs]])
                    t = pool.dma(qB[qs][64:128, :], dqT_d[h, 64:128, cols], s_q[qs])
                return qs, t

            tabq = [(tab, src, e) for e in range(NE) for (tab, src) in ((wgtab_d, weg_d), (wutab_d, weu_d), (wdtab_d, wed_d))]

            def next_tab():
                if not tabq:
                    return
                tab, src, e = tabq.pop(0)
                jn = 4 if tab is wdtab_d else 8
                tab_tok[0] = pool.dma(tab[e * 128:(e + 1) * 128, :].rearrange("p (j n) -> p j n", j=jn),
                                      src[e, :, :].rearrange("(j p) n -> p j n", p=128), s_tab)

            zt = sb("zt", [128, 8, D], BF16, ph)
            s_z = sem("d_z", ph)
            tz = dve.inc(dve.e.memset(zt[:], 0.0))
            zq = [(r0, min(1024, ROWS - r0) // 128) for r0 in range(0, ROWS, 1024)]
            ztok = [None]
            zskip = [3]

            def next_zero():
                if zskip[0] > 0:
                    zskip[0] -= 1
                    return
                if not zq:
                    return
                r0, nr = zq.pop(0)
                ztok[0] = sp.dma(xs_d[r0:r0 + nr * 128, :].rearrange("(g p) n -> p g n", p=128), zt[:, 0:nr, :], s_z, deps=[tz])

            pre_q = {}
            load_kv(0)
            for hi, (kind, h) in enumerate(heads):
                sl = hi % 2
                q0 = pre_q.pop(hi) if hi in pre_q else load_q(hi, 0)
                if hi + 1 < len(heads):
                    load_kv(hi + 1)
                tkl, tvl = kv_tok[hi]
                subs = [0] if kind == "mla" else [0, 1]
                scale = (192.0 ** -0.5) if kind == "mla" else 0.125
                items = [(qc, s, kt) for qc in range(NCH) for s in subs for kt in range(NKT)]
                N = len(items)
                tok_qk, tok_exp, tok_pv = {}, {}, {}
                qinfo = {0: q0}

                def emit_qk(i):
                    qc, s, kt = items[i]
                    if s == 0 and kt == 0 and qc + 1 < NCH:
                        qinfo[qc + 1] = load_q(hi, qc + 1)
                    if s == 0 and kt == 0 and qc == NCH - 1 and hi + 1 < len(heads):
                        pre_q[hi + 1] = load_q(hi + 1, 0)
                    qs, tq = qinfo[qc]
                    bank = sbk[i % 4]
                    pe.wait(tq, tkl, tok_exp.get(i - 4))
                    ks = slice(kt * 128, (kt + 1) * 128)
                    if kind == "mla":
                        pe.wait(t_kp)
                        pe.e.matmul(bank[:, :], lhsT=ksb[sl][:, ks], rhs=qsb[qs][:, :], start=True, stop=False)
                        ins = pe.e.matmul(bank[:, :], lhsT=kpsb[:, ks], rhs=qpsb[qs][:, :], start=False, stop=True)
                    else:
                        qq = qA if s == 0 else qB
                        ins = pe.e.matmul(bank[:, :], lhsT=ksb[sl][:, ks], rhs=qq[qs][:, :], start=True, stop=True)
                    tok_qk[i] = pe.inc(ins)
                    if s == subs[-1] and kt == NKT - 1:
                        q_free[qs] = tok_qk[i]

                def emit_exp(i):
                    act.wait(tok_qk[i], tok_pv.get(i - 3))
                    tok_exp[i] = act.inc(act.e.activation(out=pT[i % 3][:], in_=sbk[i % 4][:, :], func=AF.Exp,
                                                          scale=float(scale)))

                def epilogue(i):
                    qc, s, kt = items[i]
                    dve.wait(tok_pv[i], st["epi"])
                    traw = []
                    for qb in range(4):
                        traw.append(dve.inc(dve.e.tensor_copy(out=accs[:, qb, :], in_=accb[qb][:, 0:129])))
                    st["acc_free"] = traw
                    dve.wait(traw[3])
                    t = dve.inc(dve.e.reciprocal(out=rinv[:], in_=accs[:, :, 128]))
                    dve.wait(t)
                    rinv_b = rinv[:, :].unsqueeze(2).broadcast_to([128, 4, 128])
                    rows = slice(qc * 512, (qc + 1) * 512)
                    if kind == "mla":
                        asl = st["aoi"] % 2
                        st["aoi"] += 1
                        dve.wait(ao_free[asl])
                        t = dve.inc(dve.e.tensor_tensor(out=ao_st[asl][:], in0=accs[:, :, 0:128], in1=rinv_b, op=ALU.mult))
                        st["epi"] = t
                        ao_free[asl] = sp.dma(ao_d[rows, h * 128:(h + 1) * 128].rearrange("(t p) d -> p t d", p=128),
                                              ao_st[asl][:], s_ao[asl], deps=[t])
                        return
                    dst = o1 if s == 0 else o2
                    t = dve.inc(dve.e.tensor_tensor(out=dst[:], in0=accs[:, :, 0:128], in1=rinv_b, op=ALU.mult))
                    st["epi"] = t
                    if s == 0:
                        return
                    dve.wait(t, t_misc)
                    t = dve.inc(dve.e.scalar_tensor_tensor(out=dd[:], in0=o2[:], scalar=neglam[:, 0:1], in1=o1[:],
                                                           op0=ALU.mult, op1=ALU.add))
                    dve.wait(t)
                    t = dve.inc(dve.e.tensor_tensor(out=sqt[:], in0=dd[:], in1=dd[:], op=ALU.mult))
                    dve.wait(t)
                    t = dve.inc(dve.e.reduce_sum(out=ssq[:], in_=sqt[:], axis=AX.X))
                    tr = rsqrt(ssq[:], ssq[:], 1.0 / 128, 4, [t])
                    asl = st["aoi"] % 2
                    st["aoi"] += 1
                    dve.wait(tr, ao_free[asl])
                    for qb in range(4):
                        t = dve.inc(dve.e.scalar_tensor_tensor(out=ao_st[asl][:, qb, :], in0=dd[:, qb, :],
                                                               scalar=ssq[:, qb:qb + 1], in1=subln_b[:],
                                                               op0=ALU.mult, op1=ALU.mult))
                    st["epi"] = t
                    ao_free[asl] = sp.dma(ao_d[rows, 512 + h * 128:512 + (h + 1) * 128].rearrange("(t p) d -> p t d", p=128),
                                          ao_st[asl][:], s_ao[asl], deps=[t])

                def emit_pv(i):
                    qc, s, kt = items[i]
                    pe.wait(tok_exp[i], tvl)
                    for qb in range(4):
                        if kt == 0 and st["acc_free"] is not None:
                            pe.wait(st["acc_free"][qb])
                        ins = pe.e.matmul(accb[qb][:, 0:129], lhsT=pT[i % 3][:, qb * 128:(qb + 1) * 128],
                                          rhs=vsb[sl][:, kt, :], start=(kt == 0), stop=(kt == NKT - 1))
                    tok_pv[i] = pe.inc(ins)
                    if kt == NKT - 1:
                        epilogue(i)

                LA = 3
                for i in range(min(LA, N)):
                    emit_qk(i)
                for i in range(N):
                    emit_exp(i)
                    if i + LA < N:
                        emit_qk(i + LA)
                    emit_pv(i)
                kv_free[sl] = tok_pv[N - 1]
            while tabq:
                next_tab()
            zskip[0] = 0
            while zq:
                next_zero()
            dma_toks.append(ztok[0])
            dma_toks.extend(v for v in ao_free if v is not None)
            barrier()
        if stop == "B":
            return nc
        with ExitStack() as ph:
            woutb = sb("woutb", [128, 8, D], BF16, ph)
            wr = sb("wr", [128, 8, 36], F32, ph)
            s_wc = sem("d_wC", ph)
            wst = sb("wst", [128, 8, D], F32, ph)
            t_wst = sp.dma(wst[:], wout_d[:, :].rearrange("(j p) n -> p j n", p=128), s_wc)
            dve.wait(t_wst, t_mod)
            for j in range(8):
                t_wg = dve.inc(dve.e.tensor_tensor(out=woutb[:, j, :], in0=wst[:, j, :], in1=gate1_b[:], op=ALU.mult))
            t_wr = pool.dma(wr[:], wr_d[:, :].rearrange("(j p) n -> p j n", p=128), sem("d_wr", ph))
            t_wC = [t_wg, t_wr]
            ao_t = [sb(f"ao_t{i}", [128, D], BF16, ph) for i in range(2)]
            x_t = [sb(f"x_t{i}", [128, D], F32, ph) for i in range(2)]
            aoT = [sb(f"aoT{i}", [128, 8, 128], BF16, ph) for i in range(2)]
            xm = [sb(f"xm{i}", [128, D], F32, ph) for i in range(2)]
            xn2 = [sb(f"xn2_{i}", [128, D], F32, ph) for i in range(2)]
            h2T = [sb(f"h2T{i}", [128, 8, 128], F32, ph) for i in range(2)]
            h2Tb = [sb(f"h2Tb{i}", [128, 8, 128], BF16, ph) for i in range(2)]
            h2tk = [sb(f"h2tk{i}", [128, D], BF16, ph) for i in range(2)]
            ss2 = sb("ss2", [128, 2], F32, ph)
            L_all = sb("L_all", [128, NT, 36], F32, ph)
            gmx = sb("gmx", [128, NT], F32, ph)
            gmB = sb("gmB", [128, NT, 4], F32, ph)
            r4B = sb("r4B", [128, NT, 4], F32, ph)
            gsB = sb("gsB", [128, NT], F32, ph)
            esB = sb("esB", [128, NT, 8], F32, ph)
            tmpB = sb("tmpB", [128, NT, 8], F32, ph)
            e2B = sb("e2B", [128, NT, 8], F32, ph)
            mk1B = sb("mk1B", [128, NT, 8], F32, ph)
            mk2B = sb("mk2B", [128, NT, 8], F32, ph)
            m1B = sb("m1B", [128, NT], F32, ph)
            m2B = sb("m2B", [128, NT], F32, ph)
            r4 = sb("r4", [128, 4], F32, ph)
            gm = sb("gm", [128, 4], F32, ph)
            sc = sb("rsc", [128, 16], F32, ph)
            es = sb("es", [128, 8], F32, ph)
            e2 = sb("e2", [128, 8], F32, ph)
            mk1 = sb("mk1", [128, 8], F32, ph)
            mk2 = sb("mk2", [128, 8], F32, ph)
            wm = sb("wm", [128, 8], F32, ph)
            s_aot = [sem(f"d_aot{i}", ph) for i in range(2)]
            s_xt = [sem(f"d_xt{i}", ph) for i in range(2)]
            s_xm = [sem(f"d_xm{i}", ph) for i in range(2)]
            s_h2 = [sem(f"d_h2{i}", ph) for i in range(2)]
            aot_free = [None, None]
            xt_free = [None, None]
            xm_free = [None, None]
            h2b_free = [None, None]
            h2tb_rd = [None, None]
            bT = pbanks[0][:, :].bitcast(BF16)
            bM = [pbanks[1], pbanks[2]]
            bX = [pbanks[3], pbanks[4]]
            bR = pbanks[5]
            bT2 = pbanks[6][:, :].bitcast(BF16)
            fr = {"bT2": None, "bT": None, "bM": None, "bX": None, "bR": None, "chain": None}
            aoT_free = [None, None]
            xn2_free = [None, None]
            h2T_free = [None, None]
            stt = {}

            def dv(inst):
                tkn = dve.inc(inst)
                dve.wait(tkn)
                return tkn

            def front(t):
                sl = t % 2
                rows = slice(t * 128, (t + 1) * 128)
                ta = pool.dma(ao_t[sl][:], ao_d[rows, :], s_aot[sl], deps=[aot_free[sl]])
                txl = sp.dma(x_t[sl][:], x_d[rows, :], s_xt[sl], deps=[xt_free[sl]])
                pe.wait(ta, fr["bT"])
                for j in range(8):
                    ins = pe.e.transpose(out=bT[:, j * 128:(j + 1) * 128], in_=ao_t[sl][:, j * 128:(j + 1) * 128],
                                         identity=ident_bf[:])
                tk = pe.inc(ins)
                aot_free[sl] = tk
                act.wait(tk, aoT_free[sl])
                tcp = act.inc(act.e.activation(out=aoT[sl][:].rearrange("p j n -> p (j n)"), in_=bT[:, :], func=AF.Identity))
                fr["bT"] = tcp
                pe.wait(tcp, t_wC, fr["bM"])
                for half in range(2):
                    for j in range(8):
                        ins = pe.e.matmul(bM[half][:, :], lhsT=aoT[sl][:, j, :], rhs=woutb[:, j, half * 512:(half + 1) * 512],
                                          start=(j == 0), stop=(j == 7))
                tk = pe.inc(ins)
                aoT_free[sl] = tk
                dve.wait(tk, xm_free[sl], txl)
                for half in range(2):
                    hs_ = slice(half * 512, (half + 1) * 512)
                    txm = dve.inc(dve.e.tensor_tensor(out=xm[sl][:, hs_], in0=bM[half][:, :], in1=x_t[sl][:, hs_], op=ALU.add))
                fr["bM"] = txm
                xt_free[sl] = txm
                act.wait(txm, xn2_free[sl])
                tss = act.inc(act.e.activation(out=xn2[sl][:], in_=xm[sl][:], func=AF.Square, accum_out=ss2[:, sl:sl + 1]))
                tr = rsqrt(ss2[:, sl:sl + 1], ss2[:, sl:sl + 1], 1.0 / D, 1, [tss])
                act.wait(tr)
                txn = act.inc(act.e.activation(out=xn2[sl][:], in_=xm[sl][:], func=AF.Identity, scale=ss2[:, sl:sl + 1]))
                stt[t] = (txn, tss)

            def back(t):
                sl = t % 2
                rows = slice(t * 128, (t + 1) * 128)
                txn, tss = stt.pop(t)
                pe.wait(txn, fr["bX"])
                for j in range(8):
                    ins = pe.e.transpose(out=bX[j // 4][:, (j % 4) * 128:(j % 4 + 1) * 128], in_=xn2[sl][:, j * 128:(j + 1) * 128],
                                         identity=C("ident"))
                tk = pe.inc(ins)
                xn2_free[sl] = tk
                dve.wait(tk, h2T_free[sl], t_mod)
                for j in range(8):
                    th = dve.inc(dve.e.tensor_scalar(out=h2T[sl][:, j, :], in0=bX[j // 4][:, (j % 4) * 128:(j % 4 + 1) * 128],
                                                     scalar1=a2c[:, j:j + 1], scalar2=sh2c[:, j:j + 1],
                                                     op0=ALU.mult, op1=ALU.add))
                fr["bX"] = th
                act.wait(th, h2tb_rd[sl])
                thb = act.inc(act.e.activation(out=h2Tb[sl][:].rearrange("p j n -> p (j n)"),
                                               in_=h2T[sl][:].rearrange("p j n -> p (j n)"), func=AF.Identity))
                pe.wait(th, fr["bR"])
                for j in range(8):
                    ins = pe.e.matmul(bR[:, 0:36], lhsT=h2T[sl][:, j, :], rhs=wr[:, j, :], start=(j == 0), stop=(j == 7))
                tk = pe.inc(ins)
                h2T_free[sl] = [tk, thb]
                pe.wait(thb, fr["bT2"])
                for j in range(8):
                    ins = pe.e.transpose(out=bT2[:, j * 128:(j + 1) * 128], in_=h2Tb[sl][:, j, :], identity=ident_bf[:])
                tk2 = pe.inc(ins)
                h2tb_rd[sl] = tk2
                act.wait(tk2, h2b_free[sl])
                tcp2 = act.inc(act.e.activation(out=h2tk[sl][:], in_=bT2[:, :], func=AF.Identity))
                fr["bT2"] = tcp2
                dve.wait(tk)
                tl = dve.inc(dve.e.tensor_tensor(out=L_all[:, t, :], in0=bR[:, 0:36], in1=C("brt"), op=ALU.add))
                fr["bR"] = tl
                stt["L"] = tl
                xm_free[sl] = sp.dma(y_d[rows, :], xm[sl][:], s_xm[sl], deps=[txn, tss])
                h2b_free[sl] = sp.dma(h2tok_d[rows, :], h2tk[sl][:], s_h2[sl], deps=[tcp2])

            front(0)
            for t in range(NT):
                if t + 1 < NT:
                    front(t + 1)
                back(t)

            def bc(ap2, n):
                return ap2.unsqueeze(2).broadcast_to([128, NT, n])
            L4 = L_all[:, :, 0:4]
            dve.wait(stt["L"])
            dv(dve.e.tensor_reduce(out=gmx[:], in_=L4, axis=AX.X, op=ALU.max))
            dv(dve.e.tensor_tensor(out=gmB[:], in0=L4, in1=bc(gmx[:, :], 4), op=ALU.is_equal))
            tg = dv(dve.e.tensor_tensor(out=r4B[:], in0=L4, in1=bc(gmx[:, :], 4), op=ALU.subtract))
            act.wait(tg)
            te = act.inc(act.e.activation(out=r4B[:], in_=r4B[:], func=AF.Exp))
            for g_ in range(4):
                dst = esB if g_ == 0 else tmpB
                dv(dve.e.tensor_tensor(out=dst[:], in0=L_all[:, :, 4 + 8 * g_:12 + 8 * g_],
                                       in1=gmB[:, :, g_:g_ + 1].broadcast_to([128, NT, 8]), op=ALU.mult))
                if g_ > 0:
                    dv(dve.e.tensor_tensor(out=esB[:], in0=esB[:], in1=tmpB[:], op=ALU.add))
            dv(dve.e.tensor_reduce(out=m1B[:], in_=esB[:], axis=AX.X, op=ALU.max))
            dv(dve.e.tensor_tensor(out=mk1B[:], in0=esB[:], in1=bc(m1B[:, :], 8), op=ALU.is_equal))
            dv(dve.e.scalar_tensor_tensor(out=e2B[:], in0=mk1B[:], scalar=-1e30, in1=esB[:], op0=ALU.mult, op1=ALU.add))
            dv(dve.e.tensor_reduce(out=m2B[:], in_=e2B[:], axis=AX.X, op=ALU.max))
            dv(dve.e.tensor_tensor(out=mk2B[:], in0=e2B[:], in1=bc(m2B[:, :], 8), op=ALU.is_equal))
            tn = dv(dve.e.tensor_tensor(out=m2B[:], in0=m2B[:], in1=m1B[:], op=ALU.subtract))
            act.wait(tn, te)
            te2 = act.inc(act.e.activation(out=m2B[:], in_=m2B[:], func=AF.Exp))
            dve.wait(te, te2)
            dv(dve.e.tensor_reduce(out=gsB[:], in_=r4B[:], axis=AX.X, op=ALU.add))
            dv(dve.e.reciprocal(out=gsB[:], in_=gsB[:]))
            dv(dve.e.tensor_scalar_add(m1B[:], m2B[:], 1.0))
            dv(dve.e.reciprocal(out=m1B[:], in_=m1B[:]))
            dv(dve.e.tensor_tensor(out=w12_all[:, :, 0], in0=m1B[:], in1=gsB[:], op=ALU.mult))
            dv(dve.e.tensor_tensor(out=w12_all[:, :, 1], in0=w12_all[:, :, 0], in1=m2B[:], op=ALU.mult))
            for Mk, mk in ((M1_all, mk1B), (M2_all, mk2B)):
                dv(dve.e.tensor_tensor(out=Mk[:].rearrange("p t (g e) -> p t g e", g=4),
                                       in0=gmB[:, :, :].unsqueeze(3).broadcast_to([128, NT, 4, 8]),
                                       in1=mk[:, :, :].unsqueeze(2).broadcast_to([128, NT, 4, 8]), op=ALU.mult))
            dma_toks.extend(v for v in xm_free + h2b_free if v is not None)
            barrier()
        if stop == "C":
            if dbg:
                t = sp.dma(dbg_d[:, 0:NT * 32], M1_all[:].rearrange("p t e -> p (t e)"), s_cst)
                sp.wait(t)
            return nc

        NTE = NT * 32
        I32 = mybir.dt.int32
        d1 = ExitStack()
        dest_i = sb("dest_i", [128, 2, NT], I32)
        idxw_i = sb("idxw_i", [128, NB], I32)
        with d1 as ph:
            bthr = sb("bthr", [128, NB, 32], F32, ph)
            t_bt = sp.dma(bthr[:].rearrange("p b e -> p (b e)"), bthr_d[:, :], sem("d_bthr", ph))
            Mb = sb("Mb", [128, NTE], BF16, ph)
            Ub = sb("Ub", [128, 128], BF16, ph)
            Rs = sb("Rs", [128, NT, 32], F32, ph)
            Tts = sb("Tts", [128, NT, 32], F32, ph)
            Pfx = sb("Pfx", [128, NT, 32], F32, ph)
            prod = sb("prod", [128, NT, 32], F32, ph)
            cnt = sb("cnt", [128, 32], F32, ph)
            nbk = sb("nbk", [128, 32], F32, ph)
            pend = sb("pend", [128, 32], F32, ph)
            pstart = sb("pstart", [128, 32], F32, ph)
            NBC = min(32, NB)
            cmpa = sb("cmpa", [128, 32, NBC], F32, ph)
            cmpb = sb("cmpb", [128, NB, 32], F32, ph)
            ebf = sb("ebf", [128, NB], F32, ph)
            dstf = sb("dstf", [128, 2, NT], F32, ph)

            def dv(inst):
                tkn = dve.inc(inst)
                dve.wait(tkn)
                return tkn
            dv(dve.e.tensor_copy(out=Ub[:], in_=C("utri")))
            tmb = dv(dve.e.tensor_tensor(out=Mb[:], in0=M1_all[:].rearrange("p t e -> p (t e)"),
                                         in1=M2_all[:].rearrange("p t e -> p (t e)"), op=ALU.add))
            pe.wait(tmb)
            nchk = (NTE + 511) // 512
            for k in range(nchk):
                c0, c1 = k * 512, min(NTE, (k + 1) * 512)
                pe.e.matmul(pbanks[k][:, 0:c1 - c0], lhsT=Ub[:], rhs=Mb[:, c0:c1], start=True, stop=True)
                ins = pe.e.matmul(pbanks[4 + k][:, 0:c1 - c0], lhsT=ones_bf[:], rhs=Mb[:, c0:c1], start=True, stop=True)
            tk = pe.inc(ins)
            dve.wait(tk)
            for k in range(nchk):
                c0, c1 = k * 512, min(NTE, (k + 1) * 512)
                dve.e.tensor_copy(out=Rs[:].rearrange("p t e -> p (t e)")[:, c0:c1], in_=pbanks[k][:, 0:c1 - c0])
                tcp = dv(dve.e.tensor_copy(out=Tts[:].rearrange("p t e -> p (t e)")[:, c0:c1], in_=pbanks[4 + k][:, 0:c1 - c0]))
            dv(dve.e.memset(Pfx[:, 0, :], 0.0))
            for t in range(1, NT):
                dv(dve.e.tensor_tensor(out=Pfx[:, t, :], in0=Pfx[:, t - 1, :], in1=Tts[:, t - 1, :], op=ALU.add))
            dv(dve.e.tensor_tensor(out=cnt[:], in0=Pfx[:, NT - 1, :], in1=Tts[:, NT - 1, :], op=ALU.add))
            dve.wait(t_bt)
            dv(dve.e.tensor_tensor(out=cmpa[:], in0=cnt[:, :].unsqueeze(2).broadcast_to([128, 32, NBC]),
                                   in1=bthr[:, 0:NBC, :].rearrange("p b e -> p e b"), op=ALU.is_gt))
            dv(dve.e.reduce_sum(out=nbk[:], in_=cmpa[:], axis=AX.X))
            dv(dve.e.tensor_scalar(out=nbk[:], in0=nbk[:], scalar1=float(BLK), scalar2=None, op0=ALU.mult))
            dv(dve.e.tensor_copy(out=pend[:, 0:1], in_=nbk[:, 0:1]))
            for e in range(1, 32):
                dv(dve.e.tensor_tensor(out=pend[:, e:e + 1], in0=pend[:, e - 1:e], in1=nbk[:, e:e + 1], op=ALU.add))
            dv(dve.e.tensor_tensor(out=pstart[:], in0=pend[:], in1=nbk[:], op=ALU.subtract))
            dv(dve.e.tensor_tensor(out=cmpb[:], in0=pend[:, :].unsqueeze(1).broadcast_to([128, NB, 32]), in1=bthr[:],
                                   op=ALU.is_le))
            dv(dve.e.reduce_sum(out=ebf[:], in_=cmpb[:], axis=AX.X))
            dv(dve.e.tensor_scalar(out=ebf[:], in0=ebf[:], scalar1=31.0, scalar2=128.0, op0=ALU.min, op1=ALU.mult))
            dv(dve.e.tensor_scalar(out=ebf[:], in0=ebf[:], scalar1=C("pcol"), scalar2=None, op0=ALU.add))
            t_idxw = dv(dve.e.tensor_copy(out=idxw_i[:], in_=ebf[:]))
            dv(dve.e.tensor_tensor(out=Rs[:], in0=Rs[:], in1=Pfx[:], op=ALU.add))
            dv(dve.e.tensor_tensor(out=Rs[:], in0=Rs[:], in1=pstart[:, :].unsqueeze(1).broadcast_to([128, NT, 32]), op=ALU.add))
            for k, Mk in enumerate([M1_all, M2_all]):
                dv(dve.e.tensor_tensor(out=prod[:], in0=Rs[:], in1=Mk[:], op=ALU.mult))
                dv(dve.e.reduce_sum(out=dstf[:, k, :], in_=prod[:], axis=AX.X))
            t_dest = dv(dve.e.tensor_copy(out=dest_i[:], in_=dstf[:]))
            if dbg:
                sp.dma(dbg_d[:, 0:2 * NT], dstf[:].rearrange("p k t -> p (k t)"), s_cst, deps=[t_dest])
                sp.dma(dbg_d[:, 1024:1024 + NB], ebf[:], s_cst, deps=[t_dest])
                t = sp.dma(dbg_d[:, 2048:2080], cnt[:], s_cst, deps=[t_dest])
                dma_toks.append(t)
            barrier()
        if stop == "D1":
            return nc

        IOA = bass.IndirectOffsetOnAxis
        with ExitStack() as ph:
            NHS = 8
            ht = [sb(f"ht{i}", [128, D], BF16, ph) for i in range(NHS)]
            s_ht = [sem(f"d_ht{i}", ph) for i in range(NHS)]
            s_sc = [sem(f"d_sc{i}", ph) for i in range(NHS)]
            ht_free = [None] * NHS
            for t in range(NT):
                sl = t % NHS
                tl = sp.dma(ht[sl][:], h2tok_d[t * 128:(t + 1) * 128, :], s_ht[sl], deps=[ht_free[sl]])
                pool.wait(tl, t_dest)
                for k in range(2):
                    c = pool.dcnt.get(id(s_sc[sl]), 0) + 16
                    pool.dcnt[id(s_sc[sl])] = c
                    pool.e.indirect_dma_start(out=xs_d[:, :], out_offset=IOA(ap=dest_i[:, k, t:t + 1], axis=0),
                                              in_=ht[sl][:, :], in_offset=None).then_inc(s_sc[sl], 16)
                ht_free[sl] = (s_sc[sl], c)
            dma_toks.extend(v for v in ht_free if v is not None)
            barrier()
        if stop == "D2":
            return nc

        with ExitStack() as ph:
            NQ = BLK // 128
            wgs = [sb(f"wgs{i}", [128, 8, DE], BF16, ph) for i in range(2)]
            wus = [sb(f"wus{i}", [128, 8, DE], BF16, ph) for i in range(2)]
            wds = [sb(f"wds{i}", [128, 4, D], BF16, ph) for i in range(2)]
            xsb = [sb(f"xsb{i}", [128, D], BF16, ph) for i in range(4)]
            xT = [sb(f"xT{i}", [128, 8, BLK], BF16, ph) for i in range(2)]
            sg = [sb(f"sg{i}", [128, 512], F32, ph) for i in range(2)]
            aT = [sb(f"aT{i}", [128, 4, BLK], BF16, ph) for i in range(2)]
            yv = [sb(f"yv{i}", [128, D], F32, ph) for i in range(4)]
            s_wb = [sem(f"d_wb{i}", ph) for i in range(2)]
            s_xs = [sem(f"d_xs{i}", ph) for i in range(4)]
            s_ys = [sem(f"d_ys{i}", ph) for i in range(4)]
            trb = Banks([pbanks[0], pbanks[1]])
            gbk = Banks([pbanks[2], pbanks[3]])
            ubk = Banks([pbanks[4], pbanks[5]])
            ybk = Banks([pbanks[6], pbanks[7]])
            wb_free = [None, None]
            xs_free = [None] * 4
            ys_free = [None] * 4
            xT_free = [None, None]
            xT_ready = {}
            aT_free = [None, None]
            sg_free = [None, None]
            wtok = {}
            cnt = {"x": 0, "f": 0, "y": 0}

            def gather_w(b):
                ws = b % 2
                pool.wait(wb_free[ws], t_idxw, tab_tok[0])
                c = pool.dcnt.get(id(s_wb[ws]), 0)
                for (dst, tab) in ((wgs[ws], wgtab_d), (wus[ws], wutab_d), (wds[ws], wdtab_d)):
                    c += 16
                    pool.e.indirect_dma_start(out=dst[:].rearrange("p j n -> p (j n)"), out_offset=None, in_=tab[:, :],
                                              in_offset=IOA(ap=idxw_i[:, b:b + 1], axis=0)).then_inc(s_wb[ws], 16)
                pool.dcnt[id(s_wb[ws])] = c
                wtok[b] = (s_wb[ws], c)

            def emit_T(b):
                bs = b % 2
                lasts = []
                for q in range(NQ):
                    sl = cnt["x"] % 4
                    cnt["x"] += 1
                    r0 = b * BLK + q * 128
                    tl = sp.dma(xsb[sl][:], xs_d[r0:r0 + 128, :], s_xs[sl], deps=[xs_free[sl]])
                    bi, bank, bfree = trb.get()
                    bv = bank[:, :].bitcast(BF16)
                    pe.wait(tl, bfree)
                    for j in range(8):
                        ins = pe.e.transpose(out=bv[:, j * 128:(j + 1) * 128], in_=xsb[sl][:, j * 128:(j + 1) * 128],
                                             identity=ident_bf[:])
                    tk = pe.inc(ins)
                    xs_free[sl] = tk
                    if q % 2 == 0:
                        act.wait(tk, xT_free[bs])
                        last = act.inc(act.e.activation(out=xT[bs][:, :, q * 128:(q + 1) * 128],
                                                        in_=bv[:, :].rearrange("p (j n) -> p j n", j=8), func=AF.Identity))
                    else:
                        dve.wait(tk, xT_free[bs])
                        last = dve.inc(dve.e.tensor_copy(out=xT[bs][:, :, q * 128:(q + 1) * 128],
                                                         in_=bv[:, :].rearrange("p (j n) -> p j n", j=8)))
                    trb.rel(bi, last)
                    lasts.append(last)
                xT_ready[b] = lasts

            def emit_GU(b):
                bs = b % 2
                ws = b % 2
                pe.wait(xT_ready[b], wtok[b])
                for fc in range(4):
                    ssl = cnt["f"] % 2
                    cnt["f"] += 1
                    ig, gbank, gfree = gbk.get()
                    pe.wait(gfree)
                    for j in range(8):
                        ins = pe.e.matmul(gbank[:, :], lhsT=wgs[ws][:, j, fc * 128:(fc + 1) * 128], rhs=xT[bs][:, j, :],
                                          start=(j == 0), stop=(j == 7))
                    tg_ = pe.inc(ins)
                    iu, ubank, ufree = ubk.get()
                    pe.wait(ufree)
                    for j in range(8):
                        ins = pe.e.matmul(ubank[:, :], lhsT=wus[ws][:, j, fc * 128:(fc + 1) * 128], rhs=xT[bs][:, j, :],
                                          start=(j == 0), stop=(j == 7))
                    tu_ = pe.inc(ins)
                    act.wait(tg_, sg_free[ssl])
                    tsg = act.inc(act.e.activation(out=sg[ssl][:], in_=gbank[:, :], func=AF.Silu))
                    gbk.rel(ig, tsg)
                    dve.wait(tsg, tu_)
                    if fc == 0:
                        dve.wait(aT_free[bs])
                    tac = dve.inc(dve.e.tensor_tensor(out=aT[bs][:, fc, :], in0=ubank[:, :], in1=sg[ssl][:], op=ALU.mult))
                    ubk.rel(iu, tac)
                    sg_free[ssl] = tac
                xT_free[bs] = tu_
                return tac

            def emit_DOWN(b, tac):
                bs = b % 2
                ws = b % 2
                ty = None
                for q in range(NQ):
                    sl = cnt["y"] % 4
                    cnt["y"] += 1
                    r0 = b * BLK + q * 128
                    tev = []
                    for half in range(2):
                        iy, ybank, yfree = ybk.get()
                        pe.wait(yfree, tac)
                        for fc in range(4):
                            ins = pe.e.matmul(ybank[:, :], lhsT=aT[bs][:, fc, q * 128:(q + 1) * 128],
                                              rhs=wds[ws][:, fc, half * 512:(half + 1) * 512], start=(fc == 0), stop=(fc == 3))
                        ty = pe.inc(ins)
                        hs_ = slice(half * 512, (half + 1) * 512)
                        dve.wait(ty, ys_free[sl], t_mod)
                        te_ = dve.inc(dve.e.tensor_tensor(out=yv[sl][:, hs_], in0=ybank[:, :], in1=gate2_b[:, hs_], op=ALU.mult))
                        ybk.rel(iy, te_)
                        tev.append(te_)
                    ys_free[sl] = sp.dma(ys_d[r0:r0 + 128, :], yv[sl][:], s_ys[sl], deps=tev)
                aT_free[bs] = ty
                wb_free[ws] = ty

            gather_w(0)
            emit_T(0)
            for b in range(NB):
                if b + 1 < NB:
                    gather_w(b + 1)
                tac = emit_GU(b)
                if b + 1 < NB:
                    emit_T(b + 1)
                emit_DOWN(b, tac)
            dma_toks.extend(v for v in ys_free if v is not None)
            barrier()
        if stop == "D3":
            return nc

        with ExitStack() as ph:
            y1 = [sb(f"y1_{i}", [128, D], F32, ph) for i in range(4)]
            y2 = [sb(f"y2_{i}", [128, D], F32, ph) for i in range(4)]
            xmt = [sb(f"xmt{i}", [128, D], F32, ph) for i in range(4)]
            yo = [sb(f"yo{i}", [128, D], F32, ph) for i in range(4)]
            fgb = sb("fgb", [128, D], F32, ph)
            ssf = sb("ssf", [128, 4], F32, ph)
            s_g = [sem(f"d_g{i}", ph) for i in range(4)]
            s_xmt = [sem(f"d_xmt{i}", ph) for i in range(4)]
            s_yo = [sem(f"d_yo{i}", ph) for i in range(4)]
            t_fg = sp.dma(fgb[:], cst2_d[:, 2 * D:3 * D], sem("d_fg", ph))
            g_free = [None] * 4
            xmt_free = [None] * 4
            yo_free = [None] * 4
            gtok = {}

            def issue_gather(t):
                sl = t % 4
                pool.wait(g_free[sl])
                c = pool.dcnt.get(id(s_g[sl]), 0)
                for k, dst in enumerate([y1[sl], y2[sl]]):
                    c += 16
                    pool.e.indirect_dma_start(out=dst[:, :], out_offset=None, in_=ys_d[:, :],
                                              in_offset=IOA(ap=dest_i[:, k, t:t + 1], axis=0)).then_inc(s_g[sl], 16)
                pool.dcnt[id(s_g[sl])] = c
                gtok[t] = (s_g[sl], c)
                ltok[t] = sp.dma(xmt[sl][:], y_d[t * 128:(t + 1) * 128, :], s_xmt[sl], deps=[xmt_free[sl]])

            ltok = {}
            for t in range(min(3, NT)):
                issue_gather(t)
            for t in range(NT):
                sl = t % 4
                rows = slice(t * 128, (t + 1) * 128)
                if t + 3 < NT:
                    issue_gather(t + 3)
                tg_ = gtok[t]
                tld = ltok[t]
                dve.wait(tg_, tld)
                ta = dve.inc(dve.e.scalar_tensor_tensor(out=xmt[sl][:], in0=y1[sl][:], scalar=w12_all[:, t, 0:1],
                                                        in1=xmt[sl][:], op0=ALU.mult, op1=ALU.add))
                dve.wait(ta)
                t2_ = dve.inc(dve.e.scalar_tensor_tensor(out=xmt[sl][:], in0=y2[sl][:], scalar=w12_all[:, t, 1:2],
                                                         in1=xmt[sl][:], op0=ALU.mult, op1=ALU.add))
                g_free[sl] = t2_
                act.wait(t2_, yo_free[sl])
                t3_ = act.inc(act.e.activation(out=yo[sl][:], in_=xmt[sl][:], func=AF.Square, accum_out=ssf[:, sl:sl + 1]))
                t4_ = rsqrt(ssf[:, sl:sl + 1], ssf[:, sl:sl + 1], 1.0 / D, 1, [t3_])
                dve.wait(t4_, t3_, t_fg)
                t5_ = dve.inc(dve.e.scalar_tensor_tensor(out=yo[sl][:], in0=xmt[sl][:], scalar=ssf[:, sl:sl + 1],
                                                         in1=fgb[:], op0=ALU.mult, op1=ALU.mult))
                xmt_free[sl] = t5_
                yo_free[sl] = sp.dma(y_d[rows, :], yo[sl][:], s_yo[sl], deps=[t5_])
            dma_toks.extend(v for v in yo_free if v is not None)
            barrier()
        return nc


def make_inputs(inputs, b, S):
    f32 = np.float32
    g = lambda k: np.asarray(inputs[k], dtype=f32)

    def col(v, n):
        return np.ascontiguousarray(v.reshape(n, 128).T)

    def rep(v):
        return np.ascontiguousarray(np.broadcast_to(v[None, :], (128, v.shape[0])))

    cst = np.zeros((128, NCST), f32)

    def put(name, arr):
        o, w = _off[name]
        assert arr.shape == (128, w), (name, arr.shape)
        cst[:, o:o + w] = arr

    b_ada = g('b_ada')[0]
    put("c", col(g('c')[b], 8))
    put("bada", np.concatenate([col(b_ada[i * D:(i + 1) * D], 8) for i in (0, 1, 3, 4)], 1))
    put("g1", col(g('norm1_g')[0], 8))
    put("g2", col(g('norm2_g')[0], 8))
    put("qg", col(g('q_a_norm_g')[0], 2))
    put("kvg", col(g('kv_a_norm_g')[0], 2))
    put("lq1", rep(g('lambda_q1')[0]))
    put("lk1", rep(g('lambda_k1')[0]))
    put("lq2", rep(g('lambda_q2')[0]))
    put("lk2", rep(g('lambda_k2')[0]))
    put("subln", rep(g('subln_g')[0]))
    put("brt", rep(np.concatenate([g('b_router_group')[0], g('b_router_expert')[0]])))
    put("ident", np.eye(128, dtype=f32))
    put("utri", np.triu(np.ones((128, 128), f32), 1))
    put("pcol", np.arange(128, dtype=f32)[:, None])
    cst2 = np.ascontiguousarray(np.concatenate([rep(b_ada[2 * D:3 * D]), rep(b_ada[5 * D:6 * D]), rep(g('final_norm_g'))], 1))

    w_in = g('w_in')[0]
    perm64 = np.concatenate([np.arange(32, 64), np.arange(0, 32)])

    def rot_cols(w, nblk):
        idx = np.concatenate([perm64 + 64 * i for i in range(nblk)])
        return w[:, idx]

    w_in_ext = np.ascontiguousarray(np.concatenate(
        [w_in, rot_cols(w_in[:, 512:576], 1), rot_cols(w_in[:, 576:1088], 8), rot_cols(w_in[:, 1088:1600], 8)], 1))
    wq = g('w_q_up')[0].reshape(256, 4, 192)
    wq_n = wq[:, :, :128].reshape(256, 512)
    wq_p = wq[:, :, 128:].reshape(256, 256)
    wq_ext = np.ascontiguousarray(np.concatenate([wq_n, wq_p, rot_cols(wq_p, 4)], 1))
    wkv = g('w_kv_up')[0].reshape(256, 4, 256)
    wkv_ext = np.ascontiguousarray(np.concatenate([wkv[:, :, :128].reshape(256, 512), wkv[:, :, 128:].reshape(256, 512)], 1))
    inv = (10000.0 ** (-np.arange(0, 64, 2, dtype=f32) / 64)).astype(f32)
    ang = np.arange(S, dtype=f32)[None, :] * inv[:, None]
    cos = np.cos(ang).astype(f32)
    sin = np.sin(ang).astype(f32)
    cos64 = np.concatenate([cos, cos], 0)
    sin64 = np.concatenate([-sin, sin], 0)
    cosT = np.ascontiguousarray(np.concatenate([cos64, cos64], 0))
    sinT = np.ascontiguousarray(np.concatenate([sin64, sin64], 0))
    return {
        "x": np.ascontiguousarray(g('x')[b, :S]),
        "cst": cst,
        "cst2": cst2,
        "w_ada": g('w_ada')[0],
        "w_in_ext": w_in_ext,
        "wq_ext": wq_ext,
        "wkv_ext": wkv_ext,
        "cosT": cosT,
        "sinT": sinT,
        "w_out": g('w_out')[0],
        "w_router": np.ascontiguousarray(np.concatenate([g('w_router_group')[0], g('w_router_expert')[0]], 1)),
        "w_eg": g('w_expert_gate')[0],
        "w_eu": g('w_expert_up')[0],
        "w_ed": g('w_expert_down')[0],
        "bthr": np.ascontiguousarray(np.broadcast_to(
            (512.0 * np.arange((2 * S + NE * 512) // 512, dtype=f32))[None, :, None],
            (128, (2 * S + NE * 512) // 512, 32)).reshape(128, -1)),
    }


def kernel(**inputs):
    S = inputs['x'].shape[1]
    B = inputs['x'].shape[0]
    nc = build(S)
    in_maps = [make_inputs(inputs, b, S) for b in range(B)]
    res = run_bass_kernel_spmd(nc, in_maps, core_ids=list(range(B)))
    return np.stack([np.asarray(r["y"], dtype=np.float32) for r in res.results], 0)
```

```python
import math
from contextlib import ExitStack

import numpy as np
import concourse.bass as bass
import concourse.mybir as mybir
from concourse.bass_utils import run_bass_kernel_spmd

F32 = mybir.dt.float32
BF16 = mybir.dt.bfloat16
AF = mybir.ActivationFunctionType
ALU = mybir.AluOpType
AX = mybir.AxisListType

D = 1024
EPS = 1e-6
LAMBDA_INIT = 0.8 - 0.6 * math.exp(0.0)
NE = 32
DE = 512

_off = {}
_n = 0
for _name, _w in [("c", 8), ("bada", 32), ("g1", 8), ("g2", 8), ("qg", 2), ("kvg", 2),
                  ("lq1", 64), ("lk1", 64), ("lq2", 64), ("lk2", 64), ("subln", 128),
                  ("brt", 36), ("ident", 128), ("utri", 128), ("pcol", 1)]:
    _off[_name] = (_n, _w)
    _n += _w
NCST = _n


class Eng:
    def __init__(self, e, sem):
        self.e = e
        self.sem = sem
        self.n = 0
        self.seen = {}
        self.dcnt = {}

    def wait(self, *toks):
        for t in toks:
            if t is None:
                continue
            if isinstance(t, list):
                self.wait(*t)
                continue
            sem, v = t
            k = id(sem)
            if self.seen.get(k, 0) >= v:
                continue
            self.e.wait_ge(sem, v)
            self.seen[k] = v

    def inc(self, inst):
        inst.then_inc(self.sem, 1)
        self.n += 1
        return (self.sem, self.n)

    def dma(self, out, in_, sem, deps=(), **kw):
        self.wait(*deps)
        c = self.dcnt.get(id(sem), 0) + 16
        self.dcnt[id(sem)] = c
        self.e.dma_start(out=out, in_=in_, **kw).then_inc(sem, 16)
        return (sem, c)


class Banks:
    def __init__(self, banks):
        self.banks = banks
        self.free = [None] * len(banks)
        self.i = 0

    def get(self):
        i = self.i
        self.i = (i + 1) % len(self.banks)
        return i, self.banks[i], self.free[i]

    def rel(self, i, tok):
        self.free[i] = tok


def build(S, stop=None, dbg=False):
    assert S % 512 == 0
    NCH = S // 512
    NT = S // 128
    BLK = 512
    ROWS = 2 * S + NE * BLK
    NB = ROWS // BLK
    nc = bass.Bass("TRN2", target_bir_lowering=False)
    skind = "ExternalOutput" if dbg else "Internal"

    def dram_in(name, shape, dt=F32):
        return nc.dram_tensor(name, shape, dt, kind="ExternalInput").ap()

    x_d = dram_in("x", [S, D])
    cst_d = dram_in("cst", [128, NCST])
    cst2_d = dram_in("cst2", [128, 3 * D])
    wada_d = dram_in("w_ada", [D, 6 * D])
    win_d = dram_in("w_in_ext", [D, 3200])
    wq_d = dram_in("wq_ext", [256, 1024])
    wkv_d = dram_in("wkv_ext", [256, 1024])
    cos_d = dram_in("cosT", [128, S])
    sin_d = dram_in("sinT", [128, S])
    wout_d = dram_in("w_out", [D, D])
    wr_d = dram_in("w_router", [D, 36])
    weg_d = dram_in("w_eg", [NE, D, DE])
    weu_d = dram_in("w_eu", [NE, D, DE])
    wed_d = dram_in("w_ed", [NE, DE, D])
    y_d = nc.dram_tensor("y", [S, D], F32, kind="ExternalOutput").ap()

    def scr(name, shape, dt=BF16):
        return nc.dram_tensor(name, shape, dt, kind=skind).ap()

    qnT_d = scr("s_qnT", [4, 128, S])
    qpT_d = scr("s_qpT", [4, 64, S])
    knT_d = scr("s_knT", [4, 128, S])
    kpT_d = scr("s_kpT", [64, S])
    dqT_d = scr("s_dqT", [4, 128, S])
    dkT_d = scr("s_dkT", [4, 128, S])
    vm_d = scr("s_vm", [4, S, 129])
    vd_d = scr("s_vd", [4, S, 129])
    ao_d = scr("s_ao", [S, D])
    h2tok_d = scr("s_h2tok", [S, D])
    xs_d = scr("s_xs", [ROWS, D])
    ys_d = scr("s_ys", [ROWS, D], F32)
    wgtab_d = scr("s_wgtab", [NE * 128, 8 * DE])
    wutab_d = scr("s_wutab", [NE * 128, 8 * DE])
    wdtab_d = scr("s_wdtab", [NE * 128, 4 * D])
    bthr_d = dram_in("bthr", [128, NB * 32])
    if dbg:
        dbg_d = nc.dram_tensor("dbg", [128, 4096], F32, kind="ExternalOutput").ap()

    with ExitStack() as top:
        def sb(name, shape, dt, stack=top):
            return stack.enter_context(nc.sbuf_tensor("sb_" + name, shape, dt))

        def sem(name, stack=top):
            return stack.enter_context(nc.semaphore(name))

        block = top.enter_context(nc.Block())
        pe = Eng(nc.tensor, sem("s_pe"))
        act = Eng(nc.scalar, sem("s_act"))
        dve = Eng(nc.vector, sem("s_dve"))
        pool = Eng(nc.gpsimd, sem("s_pool"))
        sp = Eng(nc.sync, sem("s_sp"))
        engs = [pe, act, dve, pool, sp]
        dma_toks = []
        s_tab = sem("d_tab")
        tab_tok = [None]

        pbanks = [top.enter_context(nc.psum_tensor(f"pb{i}", [128, 512], F32)) for i in range(8)]

        def barrier():
            toks = [(e.sem, e.n) for e in engs if e.n > 0] + list(dma_toks)
            for e in engs:
                e.wait(*toks)
            dma_toks.clear()

        cst = sb("cst", [128, NCST], F32)
        s_cst = sem("d_cst")
        t_cst = sp.dma(cst[:], cst_d[:, :], s_cst)

        def C(name, a=None, b=None):
            o, w = _off[name]
            a = 0 if a is None else a
            b = w if b is None else b
            return cst[:, o + a:o + b]

        ident_bf = sb("ident_bf", [128, 128], BF16)
        ones_bf = sb("ones_bf", [128, 128], BF16)
        ones_f = sb("ones_f", [128, 128], F32)
        negh = sb("negh", [128, 8], F32)
        epsc = sb("epsc", [128, 1], F32)
        modc = sb("modc", [128, 32], F32)
        a1c = sb("a1c", [128, 8], F32)
        a2c = sb("a2c", [128, 8], F32)
        gate1_b = sb("gate1_b", [128, D], F32)
        gate2_b = sb("gate2_b", [128, D], F32)
        neglam = sb("neglam", [128, 1], F32)
        subln_b = sb("subln_b", [128, 128], F32)
        M1_all = sb("M1_all", [128, NT, 32], F32)
        M2_all = sb("M2_all", [128, NT, 32], F32)
        w12_all = sb("w12_all", [128, NT, 2], F32)

        dve.wait(t_cst)
        dve.e.tensor_copy(out=ident_bf[:], in_=C("ident"))
        dve.e.memset(ones_bf[:], 1.0)
        dve.e.memset(ones_f[:], 1.0)
        dve.e.memset(epsc[:], EPS)
        t_c0 = dve.inc(dve.e.memset(negh[:], -0.5))

        def rsqrt(out, in_, scale, n, deps):
            pool.wait(*deps)
            t = pool.inc(pool.e.tensor_scalar(out=out, in0=in_, scalar1=float(scale), scalar2=float(EPS),
                                              op0=ALU.mult, op1=ALU.add))
            pool.wait(t, t_c0)
            return pool.inc(pool.e.tensor_tensor(out=out, in0=out, in1=negh[:, 0:n], op=ALU.pow))

        wA_stack = ExitStack()
        winb = sb("winb", [128, 8, 3200], BF16, wA_stack)
        wqb = sb("wqb", [128, 2, 1024], BF16, wA_stack)
        wkvb = sb("wkvb", [128, 2, 1024], BF16, wA_stack)
        s_w = sem("d_wA")
        tw = None
        for i in range(4):
            tw = pool.dma(winb[:, :, i * 800:(i + 1) * 800],
                          win_d[:, i * 800:(i + 1) * 800].rearrange("(j p) n -> p j n", p=128), s_w)
        tw = pool.dma(wqb[:], wq_d[:, :].rearrange("(j p) n -> p j n", p=128), s_w)
        t_wA = pool.dma(wkvb[:], wkv_d[:, :].rearrange("(j p) n -> p j n", p=128), s_w)

        with ExitStack() as ph:
            sT2 = sb("sT2", [128, 8, 2], F32, ph)
            sTb = sb("sTb", [128, 8, 128], F32, ph)
            wa = [sb(f"wa{i}", [128, 8, 1024], F32, ph) for i in range(2)]
            s_wa = [sem(f"d_wa{i}", ph) for i in range(2)]
            lt = sb("lt", [128, 64], F32, ph)
            lsum = sb("lsum", [128, 2], F32, ph)
            bgt = sb("bgt", [128, 2 * D], F32, ph)
            t_bg = sp.dma(bgt[:], cst2_d[:, 0:2 * D], sem("d_bg", ph))

            dve.e.memset(sT2[:], 0.0)
            t = dve.inc(dve.e.memset(lsum[:], 0.0))
            act.wait(t_cst, t)
            t_s = act.inc(act.e.activation(out=sT2[:, :, 0], in_=C("c"), func=AF.Silu))
            act.wait(t_s, t_c0)
            for j in range(8):
                t_sb = act.inc(act.e.activation(out=sTb[:, j, :], in_=ones_f[:], func=AF.Identity,
                                                scale=sT2[:, j, 0:1]))
            for i, (a, b) in enumerate([("lq1", "lk1"), ("lq2", "lk2")]):
                dve.wait(t)
                t = dve.inc(dve.e.tensor_tensor(out=lt[:], in0=C(a), in1=C(b), op=ALU.mult))
                dve.wait(t)
                t = dve.inc(dve.e.reduce_sum(out=lsum[:, i:i + 1], in_=lt[:], axis=AX.X))
            act.wait(t)
            t = act.inc(act.e.activation(out=lsum[:], in_=lsum[:], func=AF.Exp))
            dve.wait(t)
            t = dve.inc(dve.e.tensor_tensor(out=neglam[:], in0=lsum[:, 1:2], in1=lsum[:, 0:1], op=ALU.subtract))
            dve.wait(t)
            t = dve.inc(dve.e.tensor_scalar_add(neglam[:], neglam[:], -LAMBDA_INIT))
            dve.wait(t)
            t_misc = dve.inc(dve.e.tensor_scalar(out=subln_b[:], in0=C("subln"), scalar1=float(1.0 - LAMBDA_INIT),
                                                 scalar2=None, op0=ALU.mult))

            colidx = {0: 0, 1: 1, 3: 2, 4: 3}
            wa_free = [None, None]
            pA = pbanks[0]
            pG = [pbanks[1], pbanks[2], pbanks[3], pbanks[4]]
            t_last_gate = {}
            for g in range(6):
                sl = g % 2
                t_w = sp.dma(wa[sl][:], wada_d[:, g * 1024:(g + 1) * 1024].rearrange("(j p) n -> p j n", p=128),
                             s_wa[sl], deps=[wa_free[sl]])
                pe.wait(t_w, t_s, t_sb)
                if g in colidx:
                    ci = colidx[g]
                    for m in range(8):
                        col = (ci * 8 + m) * 2
                        for j in range(8):
                            ins = pe.e.matmul(pA[:, col:col + 2], lhsT=wa[sl][:, j, m * 128:(m + 1) * 128],
                                              rhs=sT2[:, j, :], start=(j == 0), stop=(j == 7))
                    wa_free[sl] = pe.inc(ins)
                else:
                    gi = 0 if g == 2 else 1
                    for half in range(2):
                        for j in range(8):
                            ins = pe.e.matmul(pG[gi * 2 + half][:, :], lhsT=sTb[:, j, :],
                                              rhs=wa[sl][:, j, half * 512:(half + 1) * 512],
                                              start=(j == 0), stop=(j == 7))
                    wa_free[sl] = pe.inc(ins)
                    t_last_gate[gi] = wa_free[sl]
            t_pe_mod = (pe.sem, pe.n)
            dve.wait(t_pe_mod, t_cst, t_bg)
            pAv = pA[:, 0:64].rearrange("p (c two) -> p c two", two=2)[:, :, 0]
            t = dve.inc(dve.e.tensor_tensor(out=modc[:], in0=pAv, in1=C("bada"), op=ALU.add))
            for gi, gb in enumerate([gate1_b, gate2_b]):
                for half in range(2):
                    t2 = dve.inc(dve.e.tensor_tensor(out=gb[:, half * 512:(half + 1) * 512],
                                                     in0=pG[gi * 2 + half][:, :],
                                                     in1=bgt[:, gi * D + half * 512:gi * D + (half + 1) * 512], op=ALU.add))
            dve.wait(t)
            dve.e.scalar_tensor_tensor(out=a1c[:], in0=modc[:, 8:16], scalar=1.0, in1=C("g1"),
                                       op0=ALU.add, op1=ALU.mult)
            t_mod = dve.inc(dve.e.scalar_tensor_tensor(out=a2c[:], in0=modc[:, 24:32], scalar=1.0, in1=C("g2"),
                                                       op0=ALU.add, op1=ALU.mult))
            if dbg:
                dve.wait(t_mod, t2, t_misc)
                dve.e.tensor_copy(out=cst[:, 0:32], in_=modc[:])
                t = dve.inc(dve.e.tensor_copy(out=cst[:, 32:33], in_=neglam[:]))
                t = sp.dma(dbg_d[:, 0:64], cst[:, 0:64], s_cst, deps=[t])
                dma_toks.append(t)
                t = sp.dma(dbg_d[:, 1024:2048], gate1_b[:], s_cst, deps=[t])
                dma_toks.append(t)
                t = sp.dma(dbg_d[:, 2048:3072], gate2_b[:], s_cst, deps=[t])
                dma_toks.append(t)
            barrier()
        sh1c = modc[:, 0:8]
        sh2c = modc[:, 16:24]

        if stop == "pro":
            return nc

        with ExitStack() as ph:

            xt = [sb(f"xt{i}", [128, 4, D], F32, ph) for i in range(2)]
            cs = [sb(f"cs{i}", [128, 2, 512], F32, ph) for i in range(2)]
            xn = [sb("xn0", [128, 4, D], BF16, ph)] * 2
            hT = [sb(f"hT{i}", [128, 8, 512], BF16, ph) for i in range(2)]
            ss = [sb(f"ss{i}", [128, 4], F32, ph) for i in range(2)]
            rstd = [sb(f"rstd{i}", [128, 4], F32, ph) for i in range(2)]
            cqg = sb("cqg", [128, 2, 512], BF16, ph)
            ckvg = sb("ckvg", [128, 2, 512], BF16, ph)
            sqq = sb("sqq", [128, 2, 512], BF16, ph)
            sqkv = sb("sqkv", [128, 2, 512], BF16, ph)
            rq_b = sb("rq_b", [128, 512], F32, ph)
            rkv_b = sb("rkv_b", [128, 512], F32, ph)
            rkv_t = sb("rkv_t", [128, 4], F32, ph)
            t1 = [sb(f"t1_{i}", [128, 512], F32, ph) for i in range(2)]
            t2 = [sb(f"t2_{i}", [128, 512], F32, ph) for i in range(2)]
            t3 = [sb("t3_0", [128, 512], F32, ph)] * 2
            qn_st = [sb("qn_st0", [128, 4, 512], BF16, ph)] * 2
            qp_st = [sb("qp_st0", [64, 4, 512], BF16, ph)] * 2
            kn_st = [sb("kn_st0", [128, 4, 512], BF16, ph)] * 2
            kp_st = [sb("kp_st0", [64, 512], BF16, ph)] * 2
            dq_st = [sb("dq_st0", [128, 4, 512], BF16, ph)] * 2
            dk_st = [sb("dk_st0", [128, 4, 512], BF16, ph)] * 2
            vm_st = [sb("vm_st0", [128, 4, 4, 129], BF16, ph)] * 2
            vd_st = [sb("vd_st0", [128, 4, 4, 129], BF16, ph)] * 2
            s_x = [sem(f"d_x{i}", ph) for i in range(2)]
            s_cs = [sem(f"d_cs{i}", ph) for i in range(2)]
            s_stg = {k: sem("d_st_" + k, ph) for k in ["kp", "dq", "dk", "vd", "qn", "qp", "kn", "vm"]}
            stg_free = {k: None for k in s_stg}

            dve.e.memset(vm_st[0][:], 1.0)
            t_ms = dve.inc(dve.e.memset(vd_st[0][:], 1.0))

            trb = Banks([pbanks[0], pbanks[1]])
            fb = Banks([pbanks[2], pbanks[3], pbanks[4], pbanks[5], pbanks[6], pbanks[7]])
            xt_free = [None, None]
            cs_free = [None, None]
            xn_free = [None, None]
            hT_free = [None, None]
            st_free = [None, None]
            tmp_free = [None, None]
            tmpi = [0]
            t3_free = [None]
            cq_free = None

            stA = {}
            stA_a = {}

            def frontA_a(c):
                sl = c % 2
                cols = slice(c * 512, (c + 1) * 512)
                tx = sp.dma(xt[sl][:], x_d[cols, :].rearrange("(t p) n -> p t n", p=128), s_x[sl], deps=[xt_free[sl]])
                sp.dma(cs[sl][:, 0, :], cos_d[:, cols], s_cs[sl], deps=[cs_free[sl]])
                tcs = sp.dma(cs[sl][:, 1, :], sin_d[:, cols], s_cs[sl])
                cosb = cs[sl][:, 0, :]
                sinb = cs[sl][:, 1, :]
                act.wait(tx, xn_free[0], xn_free[1])
                for t in range(4):
                    tss = act.inc(act.e.activation(out=xn[sl][:, t, :], in_=xt[sl][:, t, :], func=AF.Square,
                                                   accum_out=ss[sl][:, t:t + 1]))
                tr = rsqrt(rstd[sl][:], ss[sl][:], 1.0 / D, 4, [tss])
                act.wait(tr, xn_free[sl])
                txn = []
                for t in range(4):
                    txn.append(act.inc(act.e.activation(out=xn[sl][:, t, :], in_=xt[sl][:, t, :], func=AF.Identity,
                                                        scale=rstd[sl][:, t:t + 1])))
                xt_free[sl] = txn[3]
                stA_a[c] = (tcs, txn)

            def frontA_b(c):
                sl = c % 2
                tcs, txn = stA_a.pop(c)
                dve.wait(hT_free[sl], t_mod)
                for t in range(4):
                    bi, bank, bfree = trb.get()
                    bv = bank[:, :].bitcast(BF16)
                    pe.wait(bfree, txn[t])
                    for j in range(8):
                        ins = pe.e.transpose(out=bv[:, j * 128:(j + 1) * 128], in_=xn[sl][:, t, j * 128:(j + 1) * 128],
                                             identity=ident_bf[:])
                    ttr = pe.inc(ins)
                    dve.wait(ttr)
                    for j in range(8):
                        ins = dve.e.tensor_scalar(out=hT[sl][:, j, t * 128:(t + 1) * 128],
                                                  in0=bv[:, j * 128:(j + 1) * 128],
                                                  scalar1=a1c[:, j:j + 1], scalar2=sh1c[:, j:j + 1],
                                                  op0=ALU.mult, op1=ALU.add)
                    thT = dve.inc(ins)
                    trb.rel(bi, thT)
                xn_free[sl] = ttr
                stA[c] = (tcs, thT)

            frontA_a(0)
            frontA_b(0)
            for c in range(NCH):
                sl = c % 2
                cols = slice(c * 512, (c + 1) * 512)
                cosb = cs[sl][:, 0, :]
                sinb = cs[sl][:, 1, :]
                tcs, thT = stA.pop(c)
                def fm(w, col0, M, src, nk, deps):
                    bi, bank, bfree = fb.get()
                    pe.wait(bfree, *deps)
                    for k in range(nk):
                        ins = pe.e.matmul(bank[0:M, :], lhsT=w[:, k, col0:col0 + M], rhs=src[:, k, :],
                                          start=(k == 0), stop=(k == nk - 1))
                    return bi, bank, pe.inc(ins)

                hs = hT[sl]
                pe.wait(t_wA, tw)
                if stop == "A_w":
                    barrier()
                    return nc
                dve.wait(cq_free)
                act.wait(cq_free)
                for (col0, gname, dst, sq) in [(0, "qg", cqg, sqq), (256, "kvg", ckvg, sqkv)]:
                    for m in range(2):
                        bi, bank, tk = fm(winb, col0 + m * 128, 128, hs, 8, [thT])
                        if stop == "A_mm":
                            barrier()
                            return nc
                        dve.wait(tk)
                        ta = dve.inc(dve.e.tensor_scalar(out=dst[:, m, :], in0=bank[:, :], scalar1=C(gname, m, m + 1),
                                                         scalar2=None, op0=ALU.mult))
                        if stop == "A_dve":
                            barrier()
                            return nc
                        act.wait(tk, ta)
                        tb = act.inc(act.e.activation(out=sq[:, m, :], in_=bank[:, :], func=AF.Square))
                        if stop == "A_act":
                            barrier()
                            return nc
                        fb.rel(bi, [ta, tb])
                t_cqg = ta
                t_sq = tb
                if stop == "A_cq":
                    barrier()
                    return nc
                if c + 1 < NCH:
                    frontA_a(c + 1)
                sfree = [v for v in stg_free.values() if v is not None]
                dve.wait(t_ms, *sfree)
                act.wait(t_ms, *sfree)
                pool.wait(*sfree)

                def rope_pair(wa_col, wb_col, M, w, src, nk, deps, out_ap, extra=None):
                    ia, banka, tka = fm(w, wa_col, M, src, nk, deps)
                    ib, bankb, tkb = fm(w, wb_col, M, src, nk, deps)
                    ti = tmpi[0] % 2
                    tmpi[0] += 1
                    dve.wait(tka, tcs, tmp_free[ti])
                    tA = dve.inc(dve.e.tensor_tensor(out=t1[ti][0:M, :], in0=banka[0:M, :], in1=cosb[0:M, :], op=ALU.mult))
                    dve.wait(tkb)
                    tB = dve.inc(dve.e.tensor_tensor(out=t2[ti][0:M, :], in0=bankb[0:M, :], in1=sinb[0:M, :], op=ALU.mult))
                    fb.rel(ia, tA)
                    fb.rel(ib, tB)
                    pool.wait(tA, tB)
                    if extra is None:
                        tC = pool.inc(pool.e.tensor_tensor(out=out_ap, in0=t1[ti][0:M, :], in1=t2[ti][0:M, :], op=ALU.add))
                    else:
                        eap, etok = extra
                        pool.wait(t3_free[0])
                        tC = pool.inc(pool.e.tensor_tensor(out=t3[ti][0:M, :], in0=t1[ti][0:M, :], in1=t2[ti][0:M, :], op=ALU.add))
                        pool.wait(tC, etok)
                        tC = pool.inc(pool.e.tensor_tensor(out=out_ap, in0=t3[ti][0:M, :], in1=eap, op=ALU.mult))
                        t3_free[0] = tC
                    tmp_free[ti] = tC
                    return tC

                t_kp = rope_pair(512, 2112, 64, winb, hs, 8, [thT], kp_st[sl][:, :])
                for h in range(4):
                    t_dq = rope_pair(576 + h * 128, 2176 + h * 128, 128, winb, hs, 8, [thT], dq_st[sl][:, h, :])
                tstat = {}
                for (sq, dst) in [(sqq, rq_b), (sqkv, rkv_b)]:
                    bi, bank, bfree = fb.get()
                    pe.wait(bfree, t_sq)
                    for m in range(2):
                        ins = pe.e.matmul(bank[:, :], lhsT=ones_bf[:], rhs=sq[:, m, :], start=(m == 0), stop=(m == 1))
                    tk = pe.inc(ins)
                    act.wait(tk)
                    tcp = act.inc(act.e.activation(out=dst[:], in_=bank[:, :], func=AF.Sqrt, scale=1.0 / 256,
                                                   bias=epsc[:, 0:1]))
                    fb.rel(bi, tcp)
                    dve.wait(tcp)
                    tstat[id(dst)] = dve.inc(dve.e.reciprocal(out=dst[:], in_=dst[:]))
                t_rq = tstat[id(rq_b)]
                t_rkv = tstat[id(rkv_b)]
                bi, bank, bfree = fb.get()
                pe.wait(bfree)
                for t in range(4):
                    for m in range(2):
                        ins = pe.e.matmul(bank[:, 2 * t:2 * t + 2], lhsT=sqkv[:, m, t * 128:(t + 1) * 128],
                                          rhs=ones_bf[:, 0:2], start=(m == 0), stop=(m == 1))
                tk = pe.inc(ins)
                dve.wait(tk)
                tcp = dve.inc(dve.e.tensor_copy(out=rkv_t[:], in_=bank[:, 0:8].rearrange("p (t two) -> p t two", two=2)[:, :, 0]))
                fb.rel(bi, tcp)
                t_rkvt = rsqrt(rkv_t[:], rkv_t[:], 1.0 / 256, 4, [tcp])
                for h in range(4):
                    t_dk = rope_pair(1088 + h * 128, 2688 + h * 128, 128, winb, hs, 8, [thT], dk_st[sl][:, h, :])
                if stop == "A_rope":
                    barrier()
                    return nc
                for t in range(4):
                    bi, bank, bfree = fb.get()
                    pe.wait(bfree)
                    for k in range(8):
                        ins = pe.e.matmul(bank[:, :], lhsT=hs[:, k, t * 128:(t + 1) * 128], rhs=winb[:, k, 1600:2112],
                                          start=(k == 0), stop=(k == 7))
                    tk = pe.inc(ins)
                    act.wait(tk)
                    t_vd = act.inc(act.e.activation(out=vd_st[sl][:, t, :, 0:128],
                                                    in_=bank[:, :].rearrange("p (h d) -> p h d", h=4),
                                                    func=AF.Identity))
                    fb.rel(bi, t_vd)
                hT_free[sl] = tk
                if stop == "A_dv":
                    barrier()
                    return nc
                if c + 1 < NCH:
                    frontA_b(c + 1)
                if stop == "A_stat":
                    barrier()
                    return nc
                for h in range(4):
                    bi, bank, tk = fm(wqb, h * 128, 128, cqg, 2, [t_cqg])
                    dve.wait(tk, t_rq)
                    t_qn = dve.inc(dve.e.tensor_tensor(out=qn_st[sl][:, h, :], in0=bank[:, :], in1=rq_b[:], op=ALU.mult))
                    fb.rel(bi, t_qn)
                for h in range(4):
                    t_qp = rope_pair(512 + h * 64, 768 + h * 64, 64, wqb, cqg, 2, [t_cqg], qp_st[sl][:, h, :],
                                     extra=(rq_b[0:64, :], t_rq))
                if stop == "A_q":
                    barrier()
                    return nc
                for h in range(4):
                    bi, bank, tk = fm(wkvb, h * 128, 128, ckvg, 2, [t_cqg])
                    dve.wait(tk, t_rkv)
                    t_kn = dve.inc(dve.e.tensor_tensor(out=kn_st[sl][:, h, :], in0=bank[:, :], in1=rkv_b[:], op=ALU.mult))
                    fb.rel(bi, t_kn)
                for t in range(4):
                    bi, bank, bfree = fb.get()
                    pe.wait(bfree)
                    for k in range(2):
                        ins = pe.e.matmul(bank[:, :], lhsT=ckvg[:, k, t * 128:(t + 1) * 128], rhs=wkvb[:, k, 512:1024],
                                          start=(k == 0), stop=(k == 1))
                    tk = pe.inc(ins)
                    act.wait(tk, t_rkvt)
                    t_vm = act.inc(act.e.activation(out=vm_st[sl][:, t, :, 0:128],
                                                    in_=bank[:, :].rearrange("p (h d) -> p h d", h=4),
                                                    func=AF.Identity, scale=rkv_t[:, t:t + 1]))
                    fb.rel(bi, t_vm)
                cq_free = [tk, t_qp, t_kn, t_vm]
                cs_free[sl] = [t_qp, t_dk]
                if stop == "A_kv":
                    barrier()
                    return nc
                def store(key, out, in_, dep):
                    stg_free[key] = sp.dma(out, in_, s_stg[key], deps=[dep])
                store("kp", kpT_d[:, cols], kp_st[sl][:, :], t_kp)
                store("dq", dqT_d[:, :, cols].rearrange("h p n -> p h n"), dq_st[sl][:], t_dq)
                store("dk", dkT_d[:, :, cols].rearrange("h p n -> p h n"), dk_st[sl][:], t_dk)
                for t in range(4):
                    rows = slice(c * 512 + t * 128, c * 512 + (t + 1) * 128)
                    store("vd", vd_d[:, rows, :].rearrange("h p d -> p h d"), vd_st[sl][:, t, :, :], t_vd)
                store("qn", qnT_d[:, :, cols].rearrange("h p n -> p h n"), qn_st[sl][:], t_qn)
                store("qp", qpT_d[:, :, cols].rearrange("h p n -> p h n"), qp_st[sl][:], t_qp)
                store("kn", knT_d[:, :, cols].rearrange("h p n -> p h n"), kn_st[sl][:], t_kn)
                for t in range(4):
                    rows = slice(c * 512 + t * 128, c * 512 + (t + 1) * 128)
                    store("vm", vm_d[:, rows, :].rearrange("h p d -> p h d"), vm_st[sl][:, t, :, :], t_vm)
            dma_toks.extend(v for v in stg_free.values() if v is not None)
            barrier()
        wA_stack.close()
        if stop == "A":
            return nc
        with ExitStack() as ph:
            NKT = S // 128
            ksb = [sb(f"k_sb{i}", [128, S], BF16, ph) for i in range(2)]
            kpsb = sb("kp_sb", [128, S], BF16, ph)
            vsb = [sb(f"v_sb{i}", [128, NKT, 129], BF16, ph) for i in range(2)]
            qsb = [sb(f"q_sb{i}", [128, 512], BF16, ph) for i in range(2)]
            qpsb = [sb(f"qp_sb{i}", [128, 512], BF16, ph) for i in range(2)]
            qA = [sb(f"qA{i}", [128, 512], BF16, ph) for i in range(2)]
            qB = [sb(f"qB{i}", [128, 512], BF16, ph) for i in range(2)]
            pT = [sb(f"pT{i}", [128, 512], BF16, ph) for i in range(3)]
            o1 = sb("o1", [128, 4, 128], F32, ph)
            o2 = sb("o2", [128, 4, 128], F32, ph)
            dd = sb("dd", [128, 4, 128], F32, ph)
            sqt = sb("sqt", [128, 4, 128], F32, ph)
            ssq = sb("ssq", [128, 4], F32, ph)
            rinv = sb("rinv", [128, 4], F32, ph)
            accs = sb("accs", [128, 4, 129], F32, ph)
            ao_st = [sb(f"ao_st{i}", [128, 4, 128], BF16, ph) for i in range(2)]
            s_k = [sem(f"d_k{i}", ph) for i in range(2)]
            s_v = [sem(f"d_v{i}", ph) for i in range(2)]
            s_kp = sem("d_kp", ph)
            s_q = [sem(f"d_q{i}", ph) for i in range(2)]
            s_ao = [sem(f"d_ao{i}", ph) for i in range(2)]
            sbk = pbanks[0:4]
            accb = pbanks[4:8]
            dve.e.memset(kpsb[64:128, :], 0.0)
            for i in range(2):
                dve.e.memset(qpsb[i][64:128, :], 0.0)
                dve.e.memset(qA[i][64:128, :], 0.0)
                t_z = dve.inc(dve.e.memset(qB[i][0:64, :], 0.0))
            pe.wait(t_z)
            t_kp = pool.dma(kpsb[0:64, :], kpT_d[:, :], s_kp)
            heads = [("mla", h) for h in range(4)] + [("dif", h) for h in range(4)]
            kv_free = [None, None]
            q_free = [None, None]
            ao_free = [None, None]
            st = {"acc_free": None, "epi": None, "aoi": 0, "qi": 0}
            kv_tok = {}

            def load_kv(hi):
                kind, h = heads[hi]
                sl = hi % 2
                ksrc = knT_d if kind == "mla" else dkT_d
                vsrc = vm_d if kind == "mla" else vd_d
                tk = pool.dma(ksb[sl][:], ksrc[h, :, :], s_k[sl], deps=[kv_free[sl]])
                tv = None
                for g0 in range(0, NKT, 16):
                    g1 = min(NKT, g0 + 16)
                    tv = pool.dma(vsb[sl][:, g0:g1, :],
                                  vsrc[h, g0 * 128:g1 * 128, :].rearrange("(t p) d -> p t d", p=128), s_v[sl])
                kv_tok[hi] = (tk, tv)

            def load_q(hi, qc):
                next_tab()
                next_zero()
                kind, h = heads[hi]
                qs = st["qi"] % 2
                st["qi"] += 1
                cols = slice(qc * 512, (qc + 1) * 512)
                if kind == "mla":
                    pool.dma(qsb[qs][:], qnT_d[h, :, cols], s_q[qs], deps=[q_free[qs]])
                    t = pool.dma(qpsb[qs][0:64, :], qpT_d[h, :, cols], s_q[qs])
                else:
                    pool.dma(qA[qs][0:64, :], dqT_d[h, 0:64, cols], s_q[qs], deps=[q_free[qs]])
                    t = pool.dma(qB[qs][64:128, :], dqT_d[h, 64:128, cols], s_q[qs])
                return qs, t

            tabq = [(tab, src, e) for e in range(NE) for (tab, src) in ((wgtab_d, weg_d), (wutab_d, weu_d), (wdtab_d, wed_d))]

            def next_tab():
                if not tabq:
                    return
                tab, src, e = tabq.pop(0)
                jn = 4 if tab is wdtab_d else 8
                tab_tok[0] = pool.dma(tab[e * 128:(e + 1) * 128, :].rearrange("p (j n) -> p j n", j=jn),
                                      src[e, :, :].rearrange("(j p) n -> p j n", p=128), s_tab)

            zt = sb("zt", [128, 8, D], BF16, ph)
            s_z = sem("d_z", ph)
            tz = dve.inc(dve.e.memset(zt[:], 0.0))
            zq = [(r0, min(1024, ROWS - r0) // 128) for r0 in range(0, ROWS, 1024)]
            ztok = [None]
            zskip = [3]

            def next_zero():
                if zskip[0] > 0:
                    zskip[0] -= 1
                    return
                if not zq:
                    return
                r0, nr = zq.pop(0)
                ztok[0] = sp.dma(xs_d[r0:r0 + nr * 128, :].rearrange("(g p) n -> p g n", p=128), zt[:, 0:nr, :], s_z, deps=[tz])

            pre_q = {}
            load_kv(0)
            for hi, (kind, h) in enumerate(heads):
                sl = hi % 2
                q0 = pre_q.pop(hi) if hi in pre_q else load_q(hi, 0)
                if hi + 1 < len(heads):
                    load_kv(hi + 1)
                tkl, tvl = kv_tok[hi]
                subs = [0] if kind == "mla" else [0, 1]
                scale = (192.0 ** -0.5) if kind == "mla" else 0.125
                items = [(qc, s, kt) for qc in range(NCH) for s in subs for kt in range(NKT)]
                N = len(items)
                tok_qk, tok_exp, tok_pv = {}, {}, {}
                qinfo = {0: q0}

                def emit_qk(i):
                    qc, s, kt = items[i]
                    if s == 0 and kt == 0 and qc + 1 < NCH:
                        qinfo[qc + 1] = load_q(hi, qc + 1)
                    if s == 0 and kt == 0 and qc == NCH - 1 and hi + 1 < len(heads):
                        pre_q[hi + 1] = load_q(hi + 1, 0)
                    qs, tq = qinfo[qc]
                    bank = sbk[i % 4]
                    pe.wait(tq, tkl, tok_exp.get(i - 4))
                    ks = slice(kt * 128, (kt + 1) * 128)
                    if kind == "mla":
                        pe.wait(t_kp)
                        pe.e.matmul(bank[:, :], lhsT=ksb[sl][:, ks], rhs=qsb[qs][:, :], start=True, stop=False)
                        ins = pe.e.matmul(bank[:, :], lhsT=kpsb[:, ks], rhs=qpsb[qs][:, :], start=False, stop=True)
                    else:
                        qq = qA if s == 0 else qB
                        ins = pe.e.matmul(bank[:, :], lhsT=ksb[sl][:, ks], rhs=qq[qs][:, :], start=True, stop=True)
                    tok_qk[i] = pe.inc(ins)
                    if s == subs[-1] and kt == NKT - 1:
                        q_free[qs] = tok_qk[i]

                def emit_exp(i):
                    act.wait(tok_qk[i], tok_pv.get(i - 3))
                    tok_exp[i] = act.inc(act.e.activation(out=pT[i % 3][:], in_=sbk[i % 4][:, :], func=AF.Exp,
                                                          scale=float(scale)))

                def epilogue(i):
                    qc, s, kt = items[i]
                    dve.wait(tok_pv[i], st["epi"])
                    traw = []
                    for qb in range(4):
                        traw.append(dve.inc(dve.e.tensor_copy(out=accs[:, qb, :], in_=accb[qb][:, 0:129])))
                    st["acc_free"] = traw
                    dve.wait(traw[3])
                    t = dve.inc(dve.e.reciprocal(out=rinv[:], in_=accs[:, :, 128]))
                    dve.wait(t)
                    rinv_b = rinv[:, :].unsqueeze(2).broadcast_to([128, 4, 128])
                    rows = slice(qc * 512, (qc + 1) * 512)
                    if kind == "mla":
                        asl = st["aoi"] % 2
                        st["aoi"] += 1
                        dve.wait(ao_free[asl])
                        t = dve.inc(dve.e.tensor_tensor(out=ao_st[asl][:], in0=accs[:, :, 0:128], in1=rinv_b, op=ALU.mult))
                        st["epi"] = t
                        ao_free[asl] = sp.dma(ao_d[rows, h * 128:(h + 1) * 128].rearrange("(t p) d -> p t d", p=128),
                                              ao_st[asl][:], s_ao[asl], deps=[t])
                        return
                    dst = o1 if s == 0 else o2
                    t = dve.inc(dve.e.tensor_tensor(out=dst[:], in0=accs[:, :, 0:128], in1=rinv_b, op=ALU.mult))
                    st["epi"] = t
                    if s == 0:
                        return
                    dve.wait(t, t_misc)
                    t = dve.inc(dve.e.scalar_tensor_tensor(out=dd[:], in0=o2[:], scalar=neglam[:, 0:1], in1=o1[:],
                                                           op0=ALU.mult, op1=ALU.add))
                    dve.wait(t)
                    t = dve.inc(dve.e.tensor_tensor(out=sqt[:], in0=dd[:], in1=dd[:], op=ALU.mult))
                    dve.wait(t)
                    t = dve.inc(dve.e.reduce_sum(out=ssq[:], in_=sqt[:], axis=AX.X))
                    tr = rsqrt(ssq[:], ssq[:], 1.0 / 128, 4, [t])
                    asl = st["aoi"] % 2
                    st["aoi"] += 1
                    dve.wait(tr, ao_free[asl])
                    for qb in range(4):
                        t = dve.inc(dve.e.scalar_tensor_tensor(out=ao_st[asl][:, qb, :], in0=dd[:, qb, :],
                                                               scalar=ssq[:, qb:qb + 1], in1=subln_b[:],
                                                               op0=ALU.mult, op1=ALU.mult))
                    st["epi"] = t
                    ao_free[asl] = sp.dma(ao_d[rows, 512 + h * 128:512 + (h + 1) * 128].rearrange("(t p) d -> p t d", p=128),
                                          ao_st[asl][:], s_ao[asl], deps=[t])

                def emit_pv(i):
                    qc, s, kt = items[i]
                    pe.wait(tok_exp[i], tvl)
                    for qb in range(4):
                        if kt == 0 and st["acc_free"] is not None:
                            pe.wait(st["acc_free"][qb])
                        ins = pe.e.matmul(accb[qb][:, 0:129], lhsT=pT[i % 3][:, qb * 128:(qb + 1) * 128],
                                          rhs=vsb[sl][:, kt, :], start=(kt == 0), stop=(kt == NKT - 1))
                    tok_pv[i] = pe.inc(ins)
                    if kt == NKT - 1:
                        epilogue(i)

                LA = 3
                for i in range(min(LA, N)):
                    emit_qk(i)
                for i in range(N):
                    emit_exp(i)
                    if i + LA < N:
                        emit_qk(i + LA)
                    emit_pv(i)
                kv_free[sl] = tok_pv[N - 1]
            while tabq:
                next_tab()
            zskip[0] = 0
            while zq:
                next_zero()
            dma_toks.append(ztok[0])
            dma_toks.extend(v for v in ao_free if v is not None)
            barrier()
        if stop == "B":
            return nc
        with ExitStack() as ph:
            woutb = sb("woutb", [128, 8, D], BF16, ph)
            wr = sb("wr", [128, 8, 36], F32, ph)
            s_wc = sem("d_wC", ph)
            wst = sb("wst", [128, 8, D], F32, ph)
            t_wst = sp.dma(wst[:], wout_d[:, :].rearrange("(j p) n -> p j n", p=128), s_wc)
            dve.wait(t_wst, t_mod)
            for j in range(8):
                t_wg = dve.inc(dve.e.tensor_tensor(out=woutb[:, j, :], in0=wst[:, j, :], in1=gate1_b[:], op=ALU.mult))
            t_wr = pool.dma(wr[:], wr_d[:, :].rearrange("(j p) n -> p j n", p=128), sem("d_wr", ph))
            t_wC = [t_wg, t_wr]
            ao_t = [sb(f"ao_t{i}", [128, D], BF16, ph) for i in range(2)]
            x_t = [sb(f"x_t{i}", [128, D], F32, ph) for i in range(2)]
            aoT = [sb(f"aoT{i}", [128, 8, 128], BF16, ph) for i in range(2)]
            xm = [sb(f"xm{i}", [128, D], F32, ph) for i in range(2)]
            xn2 = [sb(f"xn2_{i}", [128, D], F32, ph) for i in range(2)]
            h2T = [sb(f"h2T{i}", [128, 8, 128], F32, ph) for i in range(2)]
            h2Tb = [sb(f"h2Tb{i}", [128, 8, 128], BF16, ph) for i in range(2)]
            h2tk = [sb(f"h2tk{i}", [128, D], BF16, ph) for i in range(2)]
            ss2 = sb("ss2", [128, 2], F32, ph)
            L_all = sb("L_all", [128, NT, 36], F32, ph)
            gmx = sb("gmx", [128, NT], F32, ph)
            gmB = sb("gmB", [128, NT, 4], F32, ph)
            r4B = sb("r4B", [128, NT, 4], F32, ph)
            gsB = sb("gsB", [128, NT], F32, ph)
            esB = sb("esB", [128, NT, 8], F32, ph)
            tmpB = sb("tmpB", [128, NT, 8], F32, ph)
            e2B = sb("e2B", [128, NT, 8], F32, ph)
            mk1B = sb("mk1B", [128, NT, 8], F32, ph)
            mk2B = sb("mk2B", [128, NT, 8], F32, ph)
            m1B = sb("m1B", [128, NT], F32, ph)
            m2B = sb("m2B", [128, NT], F32, ph)
            r4 = sb("r4", [128, 4], F32, ph)
            gm = sb("gm", [128, 4], F32, ph)
            sc = sb("rsc", [128, 16], F32, ph)
            es = sb("es", [128, 8], F32, ph)
            e2 = sb("e2", [128, 8], F32, ph)
            mk1 = sb("mk1", [128, 8], F32, ph)
            mk2 = sb("mk2", [128, 8], F32, ph)
            wm = sb("wm", [128, 8], F32, ph)
            s_aot = [sem(f"d_aot{i}", ph) for i in range(2)]
            s_xt = [sem(f"d_xt{i}", ph) for i in range(2)]
            s_xm = [sem(f"d_xm{i}", ph) for i in range(2)]
            s_h2 = [sem(f"d_h2{i}", ph) for i in range(2)]
            aot_free = [None, None]
            xt_free = [None, None]
            xm_free = [None, None]
            h2b_free = [None, None]
            h2tb_rd = [None, None]
            bT = pbanks[0][:, :].bitcast(BF16)
            bM = [pbanks[1], pbanks[2]]
            bX = [pbanks[3], pbanks[4]]
            bR = pbanks[5]
            bT2 = pbanks[6][:, :].bitcast(BF16)
            fr = {"bT2": None, "bT": None, "bM": None, "bX": None, "bR": None, "chain": None}
            aoT_free = [None, None]
            xn2_free = [None, None]
            h2T_free = [None, None]
            stt = {}

            def dv(inst):
                tkn = dve.inc(inst)
                dve.wait(tkn)
                return tkn

            def front(t):
                sl = t % 2
                rows = slice(t * 128, (t + 1) * 128)
                ta = pool.dma(ao_t[sl][:], ao_d[rows, :], s_aot[sl], deps=[aot_free[sl]])
                txl = sp.dma(x_t[sl][:], x_d[rows, :], s_xt[sl], deps=[xt_free[sl]])
                pe.wait(ta, fr["bT"])
                for j in range(8):
                    ins = pe.e.transpose(out=bT[:, j * 128:(j + 1) * 128], in_=ao_t[sl][:, j * 128:(j + 1) * 128],
                                         identity=ident_bf[:])
                tk = pe.inc(ins)
                aot_free[sl] = tk
                act.wait(tk, aoT_free[sl])
                aoTf = aoT[sl][:].rearrange("p j n -> p (j n)")
                tcp0 = act.inc(act.e.activation(out=aoTf[:, 0:512], in_=bT[:, 0:512], func=AF.Identity))
                tcp = act.inc(act.e.activation(out=aoTf[:, 512:1024], in_=bT[:, 512:1024], func=AF.Identity))
                fr["bT"] = tcp
                pe.wait(tcp0, t_wC, fr["bM"])
                for half in range(2):
                    for j in range(8):
                        if j == 4:
                            pe.wait(tcp)
                        ins = pe.e.matmul(bM[half][:, :], lhsT=aoT[sl][:, j, :], rhs=woutb[:, j, half * 512:(half + 1) * 512],
                                          start=(j == 0), stop=(j == 7))
                tk = pe.inc(ins)
                aoT_free[sl] = tk
                dve.wait(tk, xm_free[sl], txl)
                for half in range(2):
                    hs_ = slice(half * 512, (half + 1) * 512)
                    txm = dve.inc(dve.e.tensor_tensor(out=xm[sl][:, hs_], in0=bM[half][:, :], in1=x_t[sl][:, hs_], op=ALU.add))
                fr["bM"] = txm
                xt_free[sl] = txm
                act.wait(txm, xn2_free[sl])
                tss = act.inc(act.e.activation(out=xn2[sl][:], in_=xm[sl][:], func=AF.Square, accum_out=ss2[:, sl:sl + 1]))
                tr = rsqrt(ss2[:, sl:sl + 1], ss2[:, sl:sl + 1], 1.0 / D, 1, [tss])
                act.wait(tr)
                txn = act.inc(act.e.activation(out=xn2[sl][:], in_=xm[sl][:], func=AF.Identity, scale=ss2[:, sl:sl + 1]))
                stt[t] = (txn, tss)

            def back(t):
                sl = t % 2
                rows = slice(t * 128, (t + 1) * 128)
                txn, tss = stt.pop(t)
                pe.wait(txn, fr["bX"])
                tkb = []
                for j in range(8):
                    ins = pe.e.transpose(out=bX[j // 4][:, (j % 4) * 128:(j % 4 + 1) * 128], in_=xn2[sl][:, j * 128:(j + 1) * 128],
                                         identity=C("ident"))
                    if j % 4 == 3:
                        tkb.append(pe.inc(ins))
                tk = tkb[1]
                xn2_free[sl] = tk
                dve.wait(h2T_free[sl], t_mod)
                thj = []
                for j in range(8):
                    dve.wait(tkb[j // 4])
                    th = dve.inc(dve.e.tensor_scalar(out=h2T[sl][:, j, :], in0=bX[j // 4][:, (j % 4) * 128:(j % 4 + 1) * 128],
                                                     scalar1=a2c[:, j:j + 1], scalar2=sh2c[:, j:j + 1],
                                                     op0=ALU.mult, op1=ALU.add))
                    thj.append(th)
                fr["bX"] = th
                act.wait(th, h2tb_rd[sl])
                thb = act.inc(act.e.activation(out=h2Tb[sl][:].rearrange("p j n -> p (j n)"),
                                               in_=h2T[sl][:].rearrange("p j n -> p (j n)"), func=AF.Identity))
                pe.wait(fr["bR"])
                for j in range(8):
                    pe.wait(thj[j])
                    ins = pe.e.matmul(bR[:, 0:36], lhsT=h2T[sl][:, j, :], rhs=wr[:, j, :], start=(j == 0), stop=(j == 7))
                tk = pe.inc(ins)
                h2T_free[sl] = [tk, thb]
                pe.wait(thb, fr["bT2"])
                for j in range(8):
                    ins = pe.e.transpose(out=bT2[:, j * 128:(j + 1) * 128], in_=h2Tb[sl][:, j, :], identity=ident_bf[:])
                tk2 = pe.inc(ins)
                h2tb_rd[sl] = tk2
                act.wait(tk2, h2b_free[sl])
                tcp2 = act.inc(act.e.activation(out=h2tk[sl][:], in_=bT2[:, :], func=AF.Identity))
                fr["bT2"] = tcp2
                dve.wait(tk)
                tl = dve.inc(dve.e.tensor_tensor(out=L_all[:, t, :], in0=bR[:, 0:36], in1=C("brt"), op=ALU.add))
                fr["bR"] = tl
                stt["L"] = tl
                xm_free[sl] = sp.dma(y_d[rows, :], xm[sl][:], s_xm[sl], deps=[txn, tss])
                h2b_free[sl] = sp.dma(h2tok_d[rows, :], h2tk[sl][:], s_h2[sl], deps=[tcp2])

            front(0)
            for t in range(NT):
                if t + 1 < NT:
                    front(t + 1)
                back(t)

            def bc(ap2, n):
                return ap2.unsqueeze(2).broadcast_to([128, NT, n])
            L4 = L_all[:, :, 0:4]
            dve.wait(stt["L"])
            dv(dve.e.tensor_reduce(out=gmx[:], in_=L4, axis=AX.X, op=ALU.max))
            dv(dve.e.tensor_tensor(out=gmB[:], in0=L4, in1=bc(gmx[:, :], 4), op=ALU.is_equal))
            tg = dv(dve.e.tensor_tensor(out=r4B[:], in0=L4, in1=bc(gmx[:, :], 4), op=ALU.subtract))
            act.wait(tg)
            te = act.inc(act.e.activation(out=r4B[:], in_=r4B[:], func=AF.Exp))
            for g_ in range(4):
                dst = esB if g_ == 0 else tmpB
                dv(dve.e.tensor_tensor(out=dst[:], in0=L_all[:, :, 4 + 8 * g_:12 + 8 * g_],
                                       in1=gmB[:, :, g_:g_ + 1].broadcast_to([128, NT, 8]), op=ALU.mult))
                if g_ > 0:
                    dv(dve.e.tensor_tensor(out=esB[:], in0=esB[:], in1=tmpB[:], op=ALU.add))
            dv(dve.e.tensor_reduce(out=m1B[:], in_=esB[:], axis=AX.X, op=ALU.max))
            dv(dve.e.tensor_tensor(out=mk1B[:], in0=esB[:], in1=bc(m1B[:, :], 8), op=ALU.is_equal))
            dv(dve.e.scalar_tensor_tensor(out=e2B[:], in0=mk1B[:], scalar=-1e30, in1=esB[:], op0=ALU.mult, op1=ALU.add))
            dv(dve.e.tensor_reduce(out=m2B[:], in_=e2B[:], axis=AX.X, op=ALU.max))
            dv(dve.e.tensor_tensor(out=mk2B[:], in0=e2B[:], in1=bc(m2B[:, :], 8), op=ALU.is_equal))
            tn = dv(dve.e.tensor_tensor(out=m2B[:], in0=m2B[:], in1=m1B[:], op=ALU.subtract))
            act.wait(tn, te)
            te2 = act.inc(act.e.activation(out=m2B[:], in_=m2B[:], func=AF.Exp))
            dve.wait(te, te2)
            dv(dve.e.tensor_reduce(out=gsB[:], in_=r4B[:], axis=AX.X, op=ALU.add))
            dv(dve.e.reciprocal(out=gsB[:], in_=gsB[:]))
            dv(dve.e.tensor_scalar_add(m1B[:], m2B[:], 1.0))
            dv(dve.e.reciprocal(out=m1B[:], in_=m1B[:]))
            dv(dve.e.tensor_tensor(out=w12_all[:, :, 0], in0=m1B[:], in1=gsB[:], op=ALU.mult))
            dv(dve.e.tensor_tensor(out=w12_all[:, :, 1], in0=w12_all[:, :, 0], in1=m2B[:], op=ALU.mult))
            for Mk, mk in ((M1_all, mk1B), (M2_all, mk2B)):
                dv(dve.e.tensor_tensor(out=Mk[:].rearrange("p t (g e) -> p t g e", g=4),
                                       in0=gmB[:, :, :].unsqueeze(3).broadcast_to([128, NT, 4, 8]),
                                       in1=mk[:, :, :].unsqueeze(2).broadcast_to([128, NT, 4, 8]), op=ALU.mult))
            dma_toks.extend(v for v in xm_free + h2b_free if v is not None)
            barrier()
        if stop == "C":
            if dbg:
                t = sp.dma(dbg_d[:, 0:NT * 32], M1_all[:].rearrange("p t e -> p (t e)"), s_cst)
                sp.wait(t)
            return nc

        NTE = NT * 32
        I32 = mybir.dt.int32
        d1 = ExitStack()
        dest_i = sb("dest_i", [128, 2, NT], I32)
        idxw_i = sb("idxw_i", [128, NB], I32)
        with d1 as ph:
            bthr = sb("bthr", [128, NB, 32], F32, ph)
            t_bt = sp.dma(bthr[:].rearrange("p b e -> p (b e)"), bthr_d[:, :], sem("d_bthr", ph))
            Mb = sb("Mb", [128, NTE], BF16, ph)
            Ub = sb("Ub", [128, 128], BF16, ph)
            Rs = sb("Rs", [128, NT, 32], F32, ph)
            Tts = sb("Tts", [128, NT, 32], F32, ph)
            Pfx = sb("Pfx", [128, NT, 32], F32, ph)
            prod = sb("prod", [128, NT, 32], F32, ph)
            cnt = sb("cnt", [128, 32], F32, ph)
            nbk = sb("nbk", [128, 32], F32, ph)
            pend = sb("pend", [128, 32], F32, ph)
            pstart = sb("pstart", [128, 32], F32, ph)
            NBC = min(32, NB)
            cmpa = sb("cmpa", [128, 32, NBC], F32, ph)
            cmpb = sb("cmpb", [128, NB, 32], F32, ph)
            ebf = sb("ebf", [128, NB], F32, ph)
            dstf = sb("dstf", [128, 2, NT], F32, ph)

            def dv(inst):
                tkn = dve.inc(inst)
                dve.wait(tkn)
                return tkn
            dv(dve.e.tensor_copy(out=Ub[:], in_=C("utri")))
            tmb = dv(dve.e.tensor_tensor(out=Mb[:], in0=M1_all[:].rearrange("p t e -> p (t e)"),
                                         in1=M2_all[:].rearrange("p t e -> p (t e)"), op=ALU.add))
            pe.wait(tmb)
            nchk = (NTE + 511) // 512
            for k in range(nchk):
                c0, c1 = k * 512, min(NTE, (k + 1) * 512)
                pe.e.matmul(pbanks[k][:, 0:c1 - c0], lhsT=Ub[:], rhs=Mb[:, c0:c1], start=True, stop=True)
                ins = pe.e.matmul(pbanks[4 + k][:, 0:c1 - c0], lhsT=ones_bf[:], rhs=Mb[:, c0:c1], start=True, stop=True)
            tk = pe.inc(ins)
            dve.wait(tk)
            for k in range(nchk):
                c0, c1 = k * 512, min(NTE, (k + 1) * 512)
                dve.e.tensor_copy(out=Rs[:].rearrange("p t e -> p (t e)")[:, c0:c1], in_=pbanks[k][:, 0:c1 - c0])
                tcp = dv(dve.e.tensor_copy(out=Tts[:].rearrange("p t e -> p (t e)")[:, c0:c1], in_=pbanks[4 + k][:, 0:c1 - c0]))
            dv(dve.e.memset(Pfx[:, 0, :], 0.0))
            for t in range(1, NT):
                dv(dve.e.tensor_tensor(out=Pfx[:, t, :], in0=Pfx[:, t - 1, :], in1=Tts[:, t - 1, :], op=ALU.add))
            dv(dve.e.tensor_tensor(out=cnt[:], in0=Pfx[:, NT - 1, :], in1=Tts[:, NT - 1, :], op=ALU.add))
            dve.wait(t_bt)
            dv(dve.e.tensor_tensor(out=cmpa[:], in0=cnt[:, :].unsqueeze(2).broadcast_to([128, 32, NBC]),
                                   in1=bthr[:, 0:NBC, :].rearrange("p b e -> p e b"), op=ALU.is_gt))
            dv(dve.e.reduce_sum(out=nbk[:], in_=cmpa[:], axis=AX.X))
            dv(dve.e.tensor_scalar(out=nbk[:], in0=nbk[:], scalar1=float(BLK), scalar2=None, op0=ALU.mult))
            dv(dve.e.tensor_copy(out=pend[:, 0:1], in_=nbk[:, 0:1]))
            for e in range(1, 32):
                dv(dve.e.tensor_tensor(out=pend[:, e:e + 1], in0=pend[:, e - 1:e], in1=nbk[:, e:e + 1], op=ALU.add))
            dv(dve.e.tensor_tensor(out=pstart[:], in0=pend[:], in1=nbk[:], op=ALU.subtract))
            dv(dve.e.tensor_tensor(out=cmpb[:], in0=pend[:, :].unsqueeze(1).broadcast_to([128, NB, 32]), in1=bthr[:],
                                   op=ALU.is_le))
            dv(dve.e.reduce_sum(out=ebf[:], in_=cmpb[:], axis=AX.X))
            dv(dve.e.tensor_scalar(out=ebf[:], in0=ebf[:], scalar1=31.0, scalar2=128.0, op0=ALU.min, op1=ALU.mult))
            dv(dve.e.tensor_scalar(out=ebf[:], in0=ebf[:], scalar1=C("pcol"), scalar2=None, op0=ALU.add))
            t_idxw = dv(dve.e.tensor_copy(out=idxw_i[:], in_=ebf[:]))
            dv(dve.e.tensor_tensor(out=Rs[:], in0=Rs[:], in1=Pfx[:], op=ALU.add))
            dv(dve.e.tensor_tensor(out=Rs[:], in0=Rs[:], in1=pstart[:, :].unsqueeze(1).broadcast_to([128, NT, 32]), op=ALU.add))
            for k, Mk in enumerate([M1_all, M2_all]):
                dv(dve.e.tensor_tensor(out=prod[:], in0=Rs[:], in1=Mk[:], op=ALU.mult))
                dv(dve.e.reduce_sum(out=dstf[:, k, :], in_=prod[:], axis=AX.X))
            t_dest = dv(dve.e.tensor_copy(out=dest_i[:], in_=dstf[:]))
            if dbg:
                sp.dma(dbg_d[:, 0:2 * NT], dstf[:].rearrange("p k t -> p (k t)"), s_cst, deps=[t_dest])
                sp.dma(dbg_d[:, 1024:1024 + NB], ebf[:], s_cst, deps=[t_dest])
                t = sp.dma(dbg_d[:, 2048:2080], cnt[:], s_cst, deps=[t_dest])
                dma_toks.append(t)
            barrier()
        if stop == "D1":
            return nc

        IOA = bass.IndirectOffsetOnAxis
        with ExitStack() as ph:
            NHS = 8
            ht = [sb(f"ht{i}", [128, D], BF16, ph) for i in range(NHS)]
            s_ht = [sem(f"d_ht{i}", ph) for i in range(NHS)]
            s_sc = [sem(f"d_sc{i}", ph) for i in range(NHS)]
            ht_free = [None] * NHS
            for t in range(NT):
                sl = t % NHS
                tl = sp.dma(ht[sl][:], h2tok_d[t * 128:(t + 1) * 128, :], s_ht[sl], deps=[ht_free[sl]])
                pool.wait(tl, t_dest)
                for k in range(2):
                    c = pool.dcnt.get(id(s_sc[sl]), 0) + 16
                    pool.dcnt[id(s_sc[sl])] = c
                    pool.e.indirect_dma_start(out=xs_d[:, :], out_offset=IOA(ap=dest_i[:, k, t:t + 1], axis=0),
                                              in_=ht[sl][:, :], in_offset=None).then_inc(s_sc[sl], 16)
                ht_free[sl] = (s_sc[sl], c)
            dma_toks.extend(v for v in ht_free if v is not None)
            barrier()
        if stop == "D2":
            return nc

        with ExitStack() as ph:
            NQ = BLK // 128
            wgs = [sb(f"wgs{i}", [128, 8, DE], BF16, ph) for i in range(2)]
            wus = [sb(f"wus{i}", [128, 8, DE], BF16, ph) for i in range(2)]
            wds = [sb(f"wds{i}", [128, 4, D], BF16, ph) for i in range(2)]
            xsb = [sb(f"xsb{i}", [128, D], BF16, ph) for i in range(4)]
            xT = [sb(f"xT{i}", [128, 8, BLK], BF16, ph) for i in range(2)]
            sg = [sb(f"sg{i}", [128, 512], F32, ph) for i in range(2)]
            aT = [sb(f"aT{i}", [128, 4, BLK], BF16, ph) for i in range(2)]
            yv = [sb(f"yv{i}", [128, D], F32, ph) for i in range(4)]
            s_wb = [sem(f"d_wb{i}", ph) for i in range(2)]
            s_xs = [sem(f"d_xs{i}", ph) for i in range(4)]
            s_ys = [sem(f"d_ys{i}", ph) for i in range(4)]
            trb = Banks([pbanks[0], pbanks[1]])
            gbk = Banks([pbanks[2], pbanks[3]])
            ubk = Banks([pbanks[4], pbanks[5]])
            ybk = Banks([pbanks[6], pbanks[7]])
            wb_free = [None, None]
            xs_free = [None] * 4
            ys_free = [None] * 4
            xT_free = [None, None]
            xT_ready = {}
            aT_free = [None, None]
            sg_free = [None, None]
            wtok = {}
            cnt = {"x": 0, "f": 0, "y": 0}

            def gather_w(b):
                ws = b % 2
                pool.wait(wb_free[ws], t_idxw, tab_tok[0])
                c = pool.dcnt.get(id(s_wb[ws]), 0)
                for (dst, tab) in ((wgs[ws], wgtab_d), (wus[ws], wutab_d), (wds[ws], wdtab_d)):
                    c += 16
                    pool.e.indirect_dma_start(out=dst[:].rearrange("p j n -> p (j n)"), out_offset=None, in_=tab[:, :],
                                              in_offset=IOA(ap=idxw_i[:, b:b + 1], axis=0)).then_inc(s_wb[ws], 16)
                pool.dcnt[id(s_wb[ws])] = c
                wtok[b] = (s_wb[ws], c)

            def emit_T(b):
                bs = b % 2
                lasts = []
                for q in range(NQ):
                    sl = cnt["x"] % 4
                    cnt["x"] += 1
                    r0 = b * BLK + q * 128
                    tl = sp.dma(xsb[sl][:], xs_d[r0:r0 + 128, :], s_xs[sl], deps=[xs_free[sl]])
                    bi, bank, bfree = trb.get()
                    bv = bank[:, :].bitcast(BF16)
                    pe.wait(tl, bfree)
                    for j in range(8):
                        ins = pe.e.transpose(out=bv[:, j * 128:(j + 1) * 128], in_=xsb[sl][:, j * 128:(j + 1) * 128],
                                             identity=ident_bf[:])
                    tk = pe.inc(ins)
                    xs_free[sl] = tk
                    if q % 2 == 0:
                        act.wait(tk, xT_free[bs])
                        last = act.inc(act.e.activation(out=xT[bs][:, :, q * 128:(q + 1) * 128],
                                                        in_=bv[:, :].rearrange("p (j n) -> p j n", j=8), func=AF.Identity))
                    else:
                        dve.wait(tk, xT_free[bs])
                        last = dve.inc(dve.e.tensor_copy(out=xT[bs][:, :, q * 128:(q + 1) * 128],
                                                         in_=bv[:, :].rearrange("p (j n) -> p j n", j=8)))
                    trb.rel(bi, last)
                    lasts.append(last)
                xT_ready[b] = lasts

            def emit_GU(b):
                bs = b % 2
                ws = b % 2
                pe.wait(xT_ready[b], wtok[b])
                for fc in range(4):
                    ssl = cnt["f"] % 2
                    cnt["f"] += 1
                    ig, gbank, gfree = gbk.get()
                    pe.wait(gfree)
                    for j in range(8):
                        ins = pe.e.matmul(gbank[:, :], lhsT=wgs[ws][:, j, fc * 128:(fc + 1) * 128], rhs=xT[bs][:, j, :],
                                          start=(j == 0), stop=(j == 7))
                    tg_ = pe.inc(ins)
                    iu, ubank, ufree = ubk.get()
                    pe.wait(ufree)
                    for j in range(8):
                        ins = pe.e.matmul(ubank[:, :], lhsT=wus[ws][:, j, fc * 128:(fc + 1) * 128], rhs=xT[bs][:, j, :],
                                          start=(j == 0), stop=(j == 7))
                    tu_ = pe.inc(ins)
                    act.wait(tg_, sg_free[ssl])
                    tsg = act.inc(act.e.activation(out=sg[ssl][:], in_=gbank[:, :], func=AF.Silu))
                    gbk.rel(ig, tsg)
                    dve.wait(tsg, tu_)
                    if fc == 0:
                        dve.wait(aT_free[bs])
                    tac = dve.inc(dve.e.tensor_tensor(out=aT[bs][:, fc, :], in0=ubank[:, :], in1=sg[ssl][:], op=ALU.mult))
                    ubk.rel(iu, tac)
                    sg_free[ssl] = tac
                xT_free[bs] = tu_
                return tac

            def emit_DOWN(b, tac):
                bs = b % 2
                ws = b % 2
                ty = None
                for q in range(NQ):
                    sl = cnt["y"] % 4
                    cnt["y"] += 1
                    r0 = b * BLK + q * 128
                    tev = []
                    for half in range(2):
                        iy, ybank, yfree = ybk.get()
                        pe.wait(yfree, tac)
                        for fc in range(4):
                            ins = pe.e.matmul(ybank[:, :], lhsT=aT[bs][:, fc, q * 128:(q + 1) * 128],
                                              rhs=wds[ws][:, fc, half * 512:(half + 1) * 512], start=(fc == 0), stop=(fc == 3))
                        ty = pe.inc(ins)
                        hs_ = slice(half * 512, (half + 1) * 512)
                        dve.wait(ty, ys_free[sl], t_mod)
                        te_ = dve.inc(dve.e.tensor_tensor(out=yv[sl][:, hs_], in0=ybank[:, :], in1=gate2_b[:, hs_], op=ALU.mult))
                        ybk.rel(iy, te_)
                        tev.append(te_)
                    ys_free[sl] = sp.dma(ys_d[r0:r0 + 128, :], yv[sl][:], s_ys[sl], deps=tev)
                aT_free[bs] = ty
                wb_free[ws] = ty

            gather_w(0)
            emit_T(0)
            for b in range(NB):
                if b + 1 < NB:
                    gather_w(b + 1)
                tac = emit_GU(b)
                if b + 1 < NB:
                    emit_T(b + 1)
                emit_DOWN(b, tac)
            dma_toks.extend(v for v in ys_free if v is not None)
            barrier()
        if stop == "D3":
            return nc

        with ExitStack() as ph:
            y1 = [sb(f"y1_{i}", [128, D], F32, ph) for i in range(4)]
            y2 = [sb(f"y2_{i}", [128, D], F32, ph) for i in range(4)]
            xmt = [sb(f"xmt{i}", [128, D], F32, ph) for i in range(4)]
            yo = [sb(f"yo{i}", [128, D], F32, ph) for i in range(4)]
            fgb = sb("fgb", [128, D], F32, ph)
            ssf = sb("ssf", [128, 4], F32, ph)
            s_g = [sem(f"d_g{i}", ph) for i in range(4)]
            s_xmt = [sem(f"d_xmt{i}", ph) for i in range(4)]
            s_yo = [sem(f"d_yo{i}", ph) for i in range(4)]
            t_fg = sp.dma(fgb[:], cst2_d[:, 2 * D:3 * D], sem("d_fg", ph))
            g_free = [None] * 4
            xmt_free = [None] * 4
            yo_free = [None] * 4
            gtok = {}

            def issue_gather(t):
                sl = t % 4
                pool.wait(g_free[sl])
                c = pool.dcnt.get(id(s_g[sl]), 0)
                for k, dst in enumerate([y1[sl], y2[sl]]):
                    c += 16
                    pool.e.indirect_dma_start(out=dst[:, :], out_offset=None, in_=ys_d[:, :],
                                              in_offset=IOA(ap=dest_i[:, k, t:t + 1], axis=0)).then_inc(s_g[sl], 16)
                pool.dcnt[id(s_g[sl])] = c
                gtok[t] = (s_g[sl], c)
                ltok[t] = sp.dma(xmt[sl][:], y_d[t * 128:(t + 1) * 128, :], s_xmt[sl], deps=[xmt_free[sl]])

            ltok = {}
            for t in range(min(3, NT)):
                issue_gather(t)
            for t in range(NT):
                sl = t % 4
                rows = slice(t * 128, (t + 1) * 128)
                if t + 3 < NT:
                    issue_gather(t + 3)
                tg_ = gtok[t]
                tld = ltok[t]
                dve.wait(tg_, tld)
                ta = dve.inc(dve.e.scalar_tensor_tensor(out=xmt[sl][:], in0=y1[sl][:], scalar=w12_all[:, t, 0:1],
                                                        in1=xmt[sl][:], op0=ALU.mult, op1=ALU.add))
                dve.wait(ta)
                t2_ = dve.inc(dve.e.scalar_tensor_tensor(out=xmt[sl][:], in0=y2[sl][:], scalar=w12_all[:, t, 1:2],
                                                         in1=xmt[sl][:], op0=ALU.mult, op1=ALU.add))
                g_free[sl] = t2_
                act.wait(t2_, yo_free[sl])
                t3_ = act.inc(act.e.activation(out=yo[sl][:], in_=xmt[sl][:], func=AF.Square, accum_out=ssf[:, sl:sl + 1]))
                t4_ = rsqrt(ssf[:, sl:sl + 1], ssf[:, sl:sl + 1], 1.0 / D, 1, [t3_])
                dve.wait(t4_, t3_, t_fg)
                t5_ = dve.inc(dve.e.scalar_tensor_tensor(out=yo[sl][:], in0=xmt[sl][:], scalar=ssf[:, sl:sl + 1],
                                                         in1=fgb[:], op0=ALU.mult, op1=ALU.mult))
                xmt_free[sl] = t5_
                yo_free[sl] = sp.dma(y_d[rows, :], yo[sl][:], s_yo[sl], deps=[t5_])
            dma_toks.extend(v for v in yo_free if v is not None)
            barrier()
        return nc


def make_inputs(inputs, b, S):
    f32 = np.float32
    g = lambda k: np.asarray(inputs[k], dtype=f32)

    def col(v, n):
        return np.ascontiguousarray(v.reshape(n, 128).T)

    def rep(v):
        return np.ascontiguousarray(np.broadcast_to(v[None, :], (128, v.shape[0])))

    cst = np.zeros((128, NCST), f32)

    def put(name, arr):
        o, w = _off[name]
        assert arr.shape == (128, w), (name, arr.shape)
        cst[:, o:o + w] = arr

    b_ada = g('b_ada')[0]
    put("c", col(g('c')[b], 8))
    put("bada", np.concatenate([col(b_ada[i * D:(i + 1) * D], 8) for i in (0, 1, 3, 4)], 1))
    put("g1", col(g('norm1_g')[0], 8))
    put("g2", col(g('norm2_g')[0], 8))
    put("qg", col(g('q_a_norm_g')[0], 2))
    put("kvg", col(g('kv_a_norm_g')[0], 2))
    put("lq1", rep(g('lambda_q1')[0]))
    put("lk1", rep(g('lambda_k1')[0]))
    put("lq2", rep(g('lambda_q2')[0]))
    put("lk2", rep(g('lambda_k2')[0]))
    put("subln", rep(g('subln_g')[0]))
    put("brt", rep(np.concatenate([g('b_router_group')[0], g('b_router_expert')[0]])))
    put("ident", np.eye(128, dtype=f32))
    put("utri", np.triu(np.ones((128, 128), f32), 1))
    put("pcol", np.arange(128, dtype=f32)[:, None])
    cst2 = np.ascontiguousarray(np.concatenate([rep(b_ada[2 * D:3 * D]), rep(b_ada[5 * D:6 * D]), rep(g('final_norm_g'))], 1))

    w_in = g('w_in')[0]
    perm64 = np.concatenate([np.arange(32, 64), np.arange(0, 32)])

    def rot_cols(w, nblk):
        idx = np.concatenate([perm64 + 64 * i for i in range(nblk)])
        return w[:, idx]

    w_in_ext = np.ascontiguousarray(np.concatenate(
        [w_in, rot_cols(w_in[:, 512:576], 1), rot_cols(w_in[:, 576:1088], 8), rot_cols(w_in[:, 1088:1600], 8)], 1))
    wq = g('w_q_up')[0].reshape(256, 4, 192)
    wq_n = wq[:, :, :128].reshape(256, 512)
    wq_p = wq[:, :, 128:].reshape(256, 256)
    wq_ext = np.ascontiguousarray(np.concatenate([wq_n, wq_p, rot_cols(wq_p, 4)], 1))
    wkv = g('w_kv_up')[0].reshape(256, 4, 256)
    wkv_ext = np.ascontiguousarray(np.concatenate([wkv[:, :, :128].reshape(256, 512), wkv[:, :, 128:].reshape(256, 512)], 1))
    inv = (10000.0 ** (-np.arange(0, 64, 2, dtype=f32) / 64)).astype(f32)
    ang = np.arange(S, dtype=f32)[None, :] * inv[:, None]
    cos = np.cos(ang).astype(f32)
    sin = np.sin(ang).astype(f32)
    cos64 = np.concatenate([cos, cos], 0)
    sin64 = np.concatenate([-sin, sin], 0)
    cosT = np.ascontiguousarray(np.concatenate([cos64, cos64], 0))
    sinT = np.ascontiguousarray(np.concatenate([sin64, sin64], 0))
    return {
        "x": np.ascontiguousarray(g('x')[b, :S]),
        "cst": cst,
        "cst2": cst2,
        "w_ada": g('w_ada')[0],
        "w_in_ext": w_in_ext,
        "wq_ext": wq_ext,
        "wkv_ext": wkv_ext,
        "cosT": cosT,
        "sinT": sinT,
        "w_out": g('w_out')[0],
        "w_router": np.ascontiguousarray(np.concatenate([g('w_router_group')[0], g('w_router_expert')[0]], 1)),
        "w_eg": g('w_expert_gate')[0],
        "w_eu": g('w_expert_up')[0],
        "w_ed": g('w_expert_down')[0],
        "bthr": np.ascontiguousarray(np.broadcast_to(
            (512.0 * np.arange((2 * S + NE * 512) // 512, dtype=f32))[None, :, None],
            (128, (2 * S + NE * 512) // 512, 32)).reshape(128, -1)),
    }


def kernel(**inputs):
    S = inputs['x'].shape[1]
    B = inputs['x'].shape[0]
    nc = build(S)
    in_maps = [make_inputs(inputs, b, S) for b in range(B)]
    res = run_bass_kernel_spmd(nc, in_maps, core_ids=list(range(B)))
    return np.stack([np.asarray(r["y"], dtype=np.float32) for r in res.results], 0)
```
